# Optimizing a Trainium2 kernel written in Bass

```python
import math
import jax, jax.numpy as jnp
from jax import lax
import numpy as np

D_MODEL = 2048
BATCH = 8
SEQ = 2048
DEPTH = 1

SSM_WIDTH = D_MODEL // 2
SSM_GROUP_CH = 16
SSM_GROUPS = SSM_WIDTH // SSM_GROUP_CH
SSM_STATE = 64
N_HEADS = 8
HEAD_DIM = 128
N_KV_HEADS = 2
GQA_GROUP = N_HEADS // N_KV_HEADS
ATTN_WIDTH = N_HEADS * HEAD_DIM
KV_WIDTH = N_KV_HEADS * HEAD_DIM
MIX_WIDTH = SSM_WIDTH + ATTN_WIDTH
N_IDX_HEADS = 16
IDX_DIM = 64
INDEX_TOPK = 256
Q_BLOCK = 128
D_FF = 5632
CONV_WIDTH = 3
LN_EPS = 1e-5
DEEPNORM_ALPHA = (2.0 * DEPTH) ** 0.25
DEEPNORM_BETA = (8.0 * DEPTH) ** -0.25
IN_SPLITS = (SSM_WIDTH, ATTN_WIDTH, KV_WIDTH, KV_WIDTH, N_IDX_HEADS * IDX_DIM, IDX_DIM, N_IDX_HEADS)
N_IN = sum(IN_SPLITS)

kernel_name = "hymba_s5_dsa_alibi_convffn_deepnorm"


def _layer_norm(x, g, b):
    xf = x.astype(jnp.float32)
    mu = jnp.mean(xf, axis=-1, keepdims=True)
    xc = xf - mu
    var = jnp.mean(xc * xc, axis=-1, keepdims=True)
    y = xc * lax.rsqrt(var + LN_EPS) * g.astype(jnp.float32) + b.astype(jnp.float32)
    return y.astype(x.dtype)


def _complex_linear_combine(left, right):
    a1r, a1i, b1r, b1i = left
    a2r, a2i, b2r, b2i = right
    ar = a2r * a1r - a2i * a1i
    ai = a2r * a1i + a2i * a1r
    br = a2r * b1r - a2i * b1i + b2r
    bi = a2r * b1i + a2i * b1r + b2i
    return (ar, ai, br, bi)


def _s5_mixer(u, a_re, a_im, log_dt, b_re, b_im, c_re, c_im, d, w_glu, b_glu):
    bsz, seq, _ = u.shape
    f32 = jnp.float32
    uf = u.astype(f32).reshape(bsz, seq, SSM_GROUPS, SSM_GROUP_CH)
    a_re = a_re.astype(f32)
    a_im = a_im.astype(f32)
    dt = jnp.exp(log_dt.astype(f32))[:, None]
    mag = jnp.exp(dt * a_re)
    ang = dt * a_im
    abar_re = mag * jnp.cos(ang)
    abar_im = mag * jnp.sin(ang)
    num_re = abar_re - 1.0
    num_im = abar_im
    den = a_re * a_re + a_im * a_im
    f_re = (num_re * a_re + num_im * a_im) / den
    f_im = (num_im * a_re - num_re * a_im) / den
    b_re = b_re.astype(f32)
    b_im = b_im.astype(f32)
    bbar_re = f_re[..., None] * b_re - f_im[..., None] * b_im
    bbar_im = f_re[..., None] * b_im + f_im[..., None] * b_re
    bu_re = jnp.einsum('bsgh,gph->bsgp', uf, bbar_re)
    bu_im = jnp.einsum('bsgh,gph->bsgp', uf, bbar_im)
    a_full_re = jnp.broadcast_to(abar_re, bu_re.shape)
    a_full_im = jnp.broadcast_to(abar_im, bu_re.shape)
    _, _, h_re, h_im = lax.associative_scan(
        _complex_linear_combine, (a_full_re, a_full_im, bu_re, bu_im), axis=1)
    y = (jnp.einsum('bsgp,ghp->bsgh', h_re, c_re.astype(f32))
         - jnp.einsum('bsgp,ghp->bsgh', h_im, c_im.astype(f32))
         + d.astype(f32) * uf)
    y = jax.nn.gelu(y.reshape(bsz, seq, SSM_WIDTH), approximate=False).astype(u.dtype)
    return y * jax.nn.sigmoid(y @ w_glu + b_glu)


def _alibi_slopes():
    h = jnp.arange(1, N_HEADS + 1, dtype=jnp.float32)
    return jnp.exp2(-8.0 * h / N_HEADS)


def _dsa_attention(q, k, v, q_idx, k_idx, w_idx):
    bsz, seq = q.shape[0], q.shape[1]
    n_keep = min(INDEX_TOPK, seq // 4)
    n_blocks = seq // Q_BLOCK
    slopes = _alibi_slopes().reshape(N_KV_HEADS, GQA_GROUP)
    key_pos = jnp.arange(seq, dtype=jnp.int32)
    scale = HEAD_DIM ** -0.5

    def to_blocks(a):
        return a.reshape(bsz, n_blocks, Q_BLOCK, *a.shape[2:]).swapaxes(0, 1)

    def block_fn(args):
        qb, qib, wb, blk = args
        t = blk * Q_BLOCK + jnp.arange(Q_BLOCK, dtype=jnp.int32)
        rel = jax.nn.relu(jnp.einsum('bqhd,bsd->bqhs', qib, k_idx))
        idx_score = jnp.einsum('bqh,bqhs->bqs', wb, rel).astype(jnp.float32)
        causal = key_pos[None, :] <= t[:, None]
        idx_score = jnp.where(causal[None], idx_score, -jnp.inf)
        _, sel = lax.top_k(idx_score, n_keep)
        valid = sel <= t[None, :, None]
        kg = jax.vmap(lambda kb, ib: kb[ib])(k, sel)
        vg = jax.vmap(lambda vb, ib: vb[ib])(v, sel)
        qg = qb.reshape(bsz, Q_BLOCK, N_KV_HEADS, GQA_GROUP, HEAD_DIM)
        logits = jnp.einsum('bqcgd,bqkcd->bqcgk', qg, kg).astype(jnp.float32) * scale
        dist = (t[None, :, None] - sel).astype(jnp.float32)
        logits = logits - slopes[None, None, :, :, None] * dist[:, :, None, None, :]
        logits = jnp.where(valid[:, :, None, None, :], logits, -jnp.inf)
        probs = jax.nn.softmax(logits, axis=-1).astype(vg.dtype)
        o = jnp.einsum('bqcgk,bqkcd->bqcgd', probs, vg)
        return o.reshape(bsz, Q_BLOCK, ATTN_WIDTH)

    out = lax.map(block_fn, (to_blocks(q), to_blocks(q_idx), to_blocks(w_idx),
                             jnp.arange(n_blocks, dtype=jnp.int32)))
    return out.swapaxes(0, 1).reshape(bsz, seq, ATTN_WIDTH)


def _conv_ffn(x, w_up, w_gate, conv_w, conv_b, w_down):
    seq = x.shape[1]
    h = x @ w_up
    hp = jnp.pad(h, ((0, 0), (CONV_WIDTH - 1, 0), (0, 0)))
    hc = conv_b + sum(conv_w[i] * hp[:, i:i + seq] for i in range(CONV_WIDTH))
    return (jax.nn.gelu(hc, approximate=False) * (x @ w_gate)) @ w_down


def setup_inputs(seed: int = 0) -> dict:
    key = jax.random.key(seed)
    ks = jax.random.split(key, 24)
    f32 = jnp.float32
    L = DEPTH

    def nrm(k, shape, scale):
        return jax.random.normal(k, shape, f32) * scale

    n = jnp.arange(SSM_STATE, dtype=f32)
    G, P, HG = SSM_GROUPS, SSM_STATE, SSM_GROUP_CH
    return {
        "x": nrm(ks[0], (BATCH, SEQ, D_MODEL), 1.0),
        "w_in": nrm(ks[1], (L, D_MODEL, N_IN), D_MODEL ** -0.5),
        "ssm_a_re": -0.5 * jnp.exp(nrm(ks[2], (L, G, P), 0.02)),
        "ssm_a_im": math.pi * n + nrm(ks[3], (L, G, P), 0.02),
        "ssm_log_dt": jax.random.uniform(ks[4], (L, G), f32, math.log(1e-3), math.log(1e-1)),
        "ssm_b_re": nrm(ks[5], (L, G, P, HG), (2.0 * HG) ** -0.5),
        "ssm_b_im": nrm(ks[6], (L, G, P, HG), (2.0 * HG) ** -0.5),
        "ssm_c_re": nrm(ks[7], (L, G, HG, P), P ** -0.5),
        "ssm_c_im": nrm(ks[8], (L, G, HG, P), P ** -0.5),
        "ssm_d": nrm(ks[9], (L, G, HG), 1.0),
        "w_glu": nrm(ks[10], (L, SSM_WIDTH, SSM_WIDTH), SSM_WIDTH ** -0.5),
        "b_glu": nrm(ks[11], (L, SSM_WIDTH), 0.01),
        "w_out": nrm(ks[12], (L, MIX_WIDTH, D_MODEL), MIX_WIDTH ** -0.5 * DEEPNORM_BETA),
        "ln1_g": 1.0 + nrm(ks[13], (L, D_MODEL), 0.02),
        "ln1_b": nrm(ks[14], (L, D_MODEL), 0.02),
        "w_up": nrm(ks[15], (L, D_MODEL, D_FF), D_MODEL ** -0.5),
        "w_gate": nrm(ks[16], (L, D_MODEL, D_FF), D_MODEL ** -0.5),
        "conv_w": nrm(ks[17], (L, CONV_WIDTH, D_FF), CONV_WIDTH ** -0.5),
        "conv_b": nrm(ks[18], (L, D_FF), 0.01),
        "w_down": nrm(ks[19], (L, D_FF, D_MODEL), D_FF ** -0.5 * DEEPNORM_BETA),
        "ln2_g": 1.0 + nrm(ks[20], (L, D_MODEL), 0.02),
        "ln2_b": nrm(ks[21], (L, D_MODEL), 0.02),
    }


def reference(x, w_in, ssm_a_re, ssm_a_im, ssm_log_dt, ssm_b_re, ssm_b_im, ssm_c_re,
              ssm_c_im, ssm_d, w_glu, b_glu, w_out, ln1_g, ln1_b, w_up, w_gate,
              conv_w, conv_b, w_down, ln2_g, ln2_b):
    bsz, seq, _ = x.shape
    split_at = [int(s) for s in np.cumsum(IN_SPLITS)[:-1]]
    idx_w_scale = (N_IDX_HEADS ** -0.5) * (IDX_DIM ** -0.5)
    h = x
    for l in range(DEPTH):
        proj = h @ w_in[l]
        u_ssm, q, k, v, qi, ki, wi = jnp.split(proj, split_at, axis=-1)
        q = q.reshape(bsz, seq, N_HEADS, HEAD_DIM)
        k = k.reshape(bsz, seq, N_KV_HEADS, HEAD_DIM)
        v = v.reshape(bsz, seq, N_KV_HEADS, HEAD_DIM)
        qi = qi.reshape(bsz, seq, N_IDX_HEADS, IDX_DIM)
        wi = wi * idx_w_scale
        y_ssm = _s5_mixer(u_ssm, ssm_a_re[l], ssm_a_im[l], ssm_log_dt[l], ssm_b_re[l],
                          ssm_b_im[l], ssm_c_re[l], ssm_c_im[l], ssm_d[l], w_glu[l], b_glu[l])
        y_attn = _dsa_attention(q, k, v, qi, ki, wi)
        mix = jnp.concatenate([y_ssm.astype(h.dtype), y_attn.astype(h.dtype)], axis=-1) @ w_out[l]
        h = _layer_norm(DEEPNORM_ALPHA * h + mix, ln1_g[l], ln1_b[l])
        f = _conv_ffn(h, w_up[l], w_gate[l], conv_w[l], conv_b[l], w_down[l])
        h = _layer_norm(DEEPNORM_ALPHA * h + f, ln2_g[l], ln2_b[l])
    return h
```

```python
import math
from contextlib import ExitStack

import numpy as np
import concourse.bass as bass
import concourse.mybir as mybir
from concourse.bass_utils import run_bass_kernel_spmd

F32 = mybir.dt.float32
BF16 = mybir.dt.bfloat16
ALU = mybir.AluOpType
AF = mybir.ActivationFunctionType
AX = mybir.AxisListType

T = 2048
D = 2048
NIN = 3664
FF = 5632
NFC = FF // 128
ALPHA = 2.0 ** 0.25
EPS = 1e-5
MAGIC = 12582912.0
TWO_PI_S = 2.0 * math.pi * (1.0 - 1e-6)
NBIS = 16
TOPK = 256

ENGS = ("pe", "act", "dve", "pool", "sp")


class Op:
    __slots__ = ("eng", "fn", "deps", "is_dma", "signal", "count", "sem", "wres", "nop")

    def __init__(self, eng, fn, is_dma):
        self.eng = eng
        self.fn = fn
        self.is_dma = is_dma
        self.deps = []
        self.signal = False
        self.count = 0
        self.sem = None
        self.wres = None
        self.nop = False


class Prog:
    def __init__(self, nc, stack):
        self.nc = nc
        self.stack = stack
        self.ops = {e: [] for e in ENGS}
        self.res = {}
        self.dma_res = {}
        self.all_ops = []
        self.since_bar_dma = []

    def _st(self, r):
        s = self.res.get(r)
        if s is None:
            s = {"w": [], "r": []}
            self.res[r] = s
        return s

    def op(self, eng, fn, reads=(), writes=(), appends=(), dma=False):
        o = Op(eng, fn, dma)
        deps = []
        for r in reads:
            deps.extend(self._st(r)["w"])
        for r in writes:
            s = self._st(r)
            deps.extend(s["w"])
            deps.extend(s["r"])
        for r in appends:
            deps.extend(self._st(r)["r"])
        for r in reads:
            self._st(r)["r"].append(o)
        for r in writes:
            s = self._st(r)
            s["w"] = [o]
            s["r"] = []
        for r in appends:
            self._st(r)["w"].append(o)
        if dma:
            wr = list(writes) + list(appends)
            assert len(wr) == 1, "a DMA must write exactly one resource"
            o.wres = wr[0]
            self.since_bar_dma.append(o)
        seen = set()
        for d in deps:
            if d is o or id(d) in seen:
                continue
            seen.add(id(d))
            if (not d.is_dma) and (not dma) and d.eng == eng == "pe":
                continue
            o.deps.append(d)
        self.ops[eng].append(o)
        self.all_ops.append(o)
        return o

    def wait_all(self, eng, reads):
        o = self.op(eng, (lambda e: None), reads=reads)
        o.nop = True
        return o

    def barrier(self):
        deps = []
        for e in ENGS:
            for o in reversed(self.ops[e]):
                if not o.is_dma and not o.nop:
                    deps.append(o)
                    break
        deps.extend(self.since_bar_dma)
        self.since_bar_dma = []
        self.res = {}
        for e in ENGS:
            o = Op(e, (lambda eng: None), False)
            o.nop = True
            o.deps = [d for d in deps]
            self.ops[e].append(o)
            self.all_ops.append(o)

    def emit(self):
        nc = self.nc
        for o in self.all_ops:
            for d in o.deps:
                d.signal = True
        esem = {e: self.stack.enter_context(nc.semaphore("s_" + e)) for e in ENGS}
        cnt = {e: 0 for e in ENGS}
        for o in self.all_ops:
            if o.is_dma:
                ent = self.dma_res.get(o.wres)
                if ent is None:
                    ent = [self.stack.enter_context(nc.semaphore("d_" + o.wres)), 0]
                    self.dma_res[o.wres] = ent
                ent[1] += 16
                o.sem = ent[0]
                o.count = ent[1]
            else:
                if o.signal and not o.nop:
                    cnt[o.eng] += 1
                o.sem = esem[o.eng]
                o.count = cnt[o.eng]

        def run(eng_name):
            def body(eng):
                waited = {}
                for o in self.ops[eng_name]:
                    need = {}
                    for d in o.deps:
                        k = id(d.sem)
                        if k not in need or need[k][1] < d.count:
                            need[k] = (d.sem, d.count)
                    for k, (sem, val) in need.items():
                        if waited.get(k, 0) >= val:
                            continue
                        eng.wait_ge(sem, val)
                        waited[k] = val
                    inst = o.fn(eng)
                    if inst is None:
                        continue
                    if o.is_dma:
                        inst.then_inc(o.sem, 16)
                    elif o.signal and not o.nop:
                        inst.then_inc(o.sem, 1)
            return body

        with nc.Block() as block:
            block.tensor(run("pe"))
            block.scalar(run("act"))
            block.vector(run("dve"))
            block.gpsimd(run("pool"))
            block.sync(run("sp"))


class Phase:
    def __init__(self, nc):
        self.nc = nc
        self.st = ExitStack()

    def __enter__(self):
        self.st.__enter__()
        return self

    def __exit__(self, *a):
        return self.st.__exit__(*a)

    def sb(self, name, shape, dt):
        return self.st.enter_context(self.nc.sbuf_tensor("sb_" + name, list(shape), dt))

    def ps(self, name, shape, dt=F32):
        return self.st.enter_context(self.nc.psum_tensor("ps_" + name, list(shape), dt))


def build_program(debug=False, stop_after=99, skip=()):
    nc = bass.Bass("TRN2", target_bir_lowering=False)

    def din(name, shape):
        return nc.dram_tensor(name, list(shape), F32, kind="ExternalInput").ap()

    xT_d = din("xT", [D, T])
    x_d = din("x", [T, D])
    w_in_d = din("w_in", [D, NIN])
    w_glu_d = din("w_glu", [1024, 1024])
    w_out_d = din("w_out", [D, D])
    w_up_d = din("w_up", [D, FF])
    w_gate_d = din("w_gate", [D, FF])
    w_down_d = din("w_down", [FF, D])
    ssm_prm_d = din("ssm_prm", [128, 3 * 32])
    bpad_re_d = din("bpad_re", [128, 32 * 128])
    bpad_im_d = din("bpad_im", [128, 32 * 128])
    cpad_re_d = din("cpad_re", [128, 32 * 128])
    cpad_im_d = din("cpad_im", [128, 32 * 128])
    colp_d = din("colp", [128, 16])
    convp_d = din("convp", [128, NFC * 4])
    ln_d = din("ln", [4, 128, D])
    ident_d = din("ident", [128, 128])
    causal_d = din("causal", [128, 128])
    alibi_d = din("alibi", [128, 8 * 16])
    iota_d = din("iota", [128, T])

    out_d = nc.dram_tensor("out", [T, D], F32, kind="ExternalOutput").ap()
    skind = "ExternalOutput" if debug else "Internal"
    uT_s = nc.dram_tensor("s_uT", [1024, T], BF16, kind=skind).ap()
    yT_s = nc.dram_tensor("s_yT", [1024, T], BF16, kind=skind).ap()
    ycatT_s = nc.dram_tensor("s_ycatT", [2048, T], BF16, kind=skind).ap()
    h1_s = nc.dram_tensor("s_h1", [T, D], F32, kind=skind).ap()
    h1T_s = nc.dram_tensor("s_h1T", [D, T], BF16, kind=skind).ap()

    with ExitStack() as top:
        p = Prog(nc, top)
        G = Phase(nc)
        top.enter_context(G)
        ident_bf = G.sb("ident_bf", [128, 128], BF16)
        ident_f = G.sb("ident_f", [128, 128], F32)
        ones_bf = G.sb("ones_bf", [128, 128], BF16)
        p.op("pool", lambda e: e.dma_start(out=ident_bf[:], in_=ident_d), writes=["ident_bf"], dma=True)
        p.op("sp", lambda e: e.dma_start(out=ident_f[:], in_=ident_d), writes=["ident_f"], dma=True)
        p.op("dve", lambda e: e.memset(ones_bf[:], 1.0), writes=["ones_bf"])

        w_in_v = w_in_d.rearrange("(c p) n -> p c n", p=128)

        with Phase(nc) as A:
            QT = A.sb("QT", [128, 8, T], BF16)
            KT = A.sb("KT", [128, 2, T], BF16)
            qiT = A.sb("qiT", [128, 8, T], BF16)
            kiT2 = A.sb("kiT2", [128, T], BF16)
            Vt = A.sb("Vt", [128, 16, 256], BF16)
            wi_t = A.sb("wi_t", [128, 16, 16], F32)

            with Phase(nc) as P1:
                xTb = P1.sb("xTb", [128, 16, T], BF16)
                wb = [P1.sb(f"wb{i}", [128, 16, 256], BF16) for i in range(3)]
                wv = P1.sb("wv", [128, 16, 256], BF16)
                wvi = P1.sb("wvi", [128, 16, 80], BF16)
                ust = [P1.sb(f"ust{i}", [128, T], BF16) for i in range(2)]
                psm = [P1.ps(f"p1ps{i}", [128, T], F32) for i in range(2)]

                for c in range(16):
                    p.op("pool", lambda e, c=c: e.dma_start(out=xTb[:, c, :], in_=xT_d[c * 128:(c + 1) * 128, :]),
                         appends=["xTb"], dma=True)

                jobs = []
                for i in range(4):
                    jobs.append((i * 256, [("u", 2 * i), ("u", 2 * i + 1)]))
                for i in range(4):
                    jobs.append((1024 + i * 256, [("q", 2 * i), ("q", 2 * i + 1)]))
                jobs.append((2048, [("k", 0), ("k", 1)]))
                for i in range(4):
                    jobs.append((2560 + i * 256, [("qi", 2 * i), ("qi", 2 * i + 1)]))
                jobs.append((None, [("ki", 0)]))

                tile_no = 0
                for jn, (col0, dests) in enumerate(jobs):
                    if stop_after < 1 and jn >= int(stop_after * 10):
                        break
                    if col0 is None and "ki" in skip:
                        continue
                    wbuf = wb[jn % 3]
                    wname = f"wb{jn % 3}"
                    if col0 is None:
                        p.op("pool", lambda e, wbuf=wbuf: e.dma_start(out=wbuf[:, :, 0:64], in_=w_in_v[:, :, 3584:3648]),
                             writes=[wname], dma=True)
                        p.op("pool", lambda e, wbuf=wbuf: e.dma_start(out=wbuf[:, :, 64:128], in_=w_in_v[:, :, 3584:3648]),
                             appends=[wname], dma=True)
                    else:
                        p.op("pool", lambda e, wbuf=wbuf, col0=col0: e.dma_start(out=wbuf[:, :, :], in_=w_in_v[:, :, col0:col0 + 256]),
                             writes=[wname], dma=True)
                    for sub, (kind, idx) in enumerate(dests):
                        ps = psm[tile_no % 2]
                        psn = [f"p1ps{tile_no % 2}_{n}" for n in range(4)]
                        for kc in range(16):
                            for n in range(4):
                                p.op("pe", lambda e, ps=ps, wbuf=wbuf, kc=kc, n=n, sub=sub: e.matmul(
                                    ps[:, n * 512:(n + 1) * 512], lhsT=wbuf[:, kc, sub * 128:(sub + 1) * 128],
                                    rhs=xTb[:, kc, n * 512:(n + 1) * 512], start=(kc == 0), stop=(kc == 15)),
                                    reads=[wname, "xTb"],
                                    writes=[psn[n]] if kc == 0 else (), appends=[psn[n]] if kc > 0 else ())
                        if kind == "u":
                            dst = ust[idx % 2]
                            dname = f"ust{idx % 2}"
                            dst_ap = dst[:, :]
                        elif kind == "q":
                            dst_ap, dname = QT[:, idx, :], f"QT{idx}"
                        elif kind == "k":
                            dst_ap, dname = KT[:, idx, :], f"KT{idx}"
                        elif kind == "qi":
                            dst_ap, dname = qiT[:, idx, :], f"qiT{idx}"
                        else:
                            dst_ap, dname = kiT2[:, :], "kiT2"
                        if tile_no % 2 == 0:
                            p.op("act", lambda e, ps=ps, dst_ap=dst_ap: e.activation(out=dst_ap, in_=ps[:, :], func=AF.Identity),
                                 reads=psn, writes=[dname])
                        else:
                            p.op("dve", lambda e, ps=ps, dst_ap=dst_ap: e.tensor_copy(out=dst_ap, in_=ps[:, :]),
                                 reads=psn, writes=[dname])
                        if kind == "u":
                            p.op("sp", lambda e, dst=dst, idx=idx: e.dma_start(out=uT_s[idx * 128:(idx + 1) * 128, :], in_=dst[:, :]),
                                 reads=[dname], writes=[f"s_uT{idx}"], dma=True)
                        tile_no += 1

                p.op("pool", lambda e: e.dma_start(out=wv[:, :, 0:256], in_=w_in_v[:, :, 2304:2560]), writes=["wv"], dma=True)
                p.op("pool", lambda e: e.dma_start(out=wvi[:, :, :], in_=w_in_v[:, :, 3584:3664]), writes=["wvi"], dma=True)
                for tt in range(16 if (stop_after >= 1 and 'tm' not in skip) else 0):
                    ps = psm[tt % 2]
                    pn = f"p1ps{tt % 2}_0"
                    pn1 = f"p1ps{tt % 2}_1"
                    for kc in range(16):
                        p.op("pe", lambda e, ps=ps, kc=kc, tt=tt: e.matmul(
                            ps[:, 0:256], lhsT=xTb[:, kc, tt * 128:(tt + 1) * 128], rhs=wv[:, kc, 0:256],
                            start=(kc == 0), stop=(kc == 15)),
                            reads=["wv", "xTb"], writes=[pn] if kc == 0 else (), appends=[pn] if kc > 0 else ())
                    for kc in range(16):
                        p.op("pe", lambda e, ps=ps, kc=kc, tt=tt: e.matmul(
                            ps[:, 512:528], lhsT=xTb[:, kc, tt * 128:(tt + 1) * 128], rhs=wvi[:, kc, 64:80],
                            start=(kc == 0), stop=(kc == 15)),
                            reads=["wvi", "xTb"], writes=[pn1] if kc == 0 else (), appends=[pn1] if kc > 0 else ())
                    p.op("act", lambda e, ps=ps, tt=tt: e.activation(out=Vt[:, tt, :], in_=ps[:, 0:256], func=AF.Identity),
                         reads=[pn], writes=[f"Vt{tt}"])
                    p.op("dve", lambda e, ps=ps, tt=tt: e.tensor_copy(out=wi_t[:, tt, :], in_=ps[:, 512:528]),
                         reads=[pn1], writes=[f"wi{tt}"])
            p.barrier()
            if stop_after <= 1:
                p.wait_all("sp", [])
                p.emit()
                return nc

            with Phase(nc) as P2:
                causal = P2.sb("causal", [128, 128], F32)
                alibi = P2.sb("alibi", [128, 8, 16], F32)
                sc = [P2.sb(f"sc{i}", [128, T], F32) for i in range(2)]
                rl = [P2.sb(f"rl{i}", [128, 512], BF16) for i in range(3)]
                dgw = P2.sb("dg", [128, 16, 128], BF16)
                junk = P2.sb("junk", [128, T], BF16)
                mask = P2.sb("mask", [128, T], BF16)
                maskT = [P2.sb(f"maskT{i}", [128, 16, 128], BF16) for i in range(2)]
                PTb = [P2.sb(f"PT{i}", [128, 4, 128], BF16) for i in range(2)]
                PTm = [P2.sb(f"PTm{i}", [128, 4, 128], BF16) for i in range(2)]
                rden = P2.sb("rden", [128, 512], F32)
                yst = [P2.sb(f"yst{i}", [128, 4, 128], BF16) for i in range(2)]
                bs = P2.sb("bs", [128, 16], F32)
                psI = [P2.ps(f"psI{i}", [128, 512], F32) for i in range(2)]
                psAccI = P2.ps("psAcc", [128, 512], F32)
                psS = [P2.ps(f"psS{i}", [128, 512], F32) for i in range(2)]
                psO = P2.ps("psO", [128, 512], F32)
                psD = P2.ps("psD", [128, 512], F32)
                psT = P2.ps("psT", [128, 1024], BF16)

                p.op("sp", lambda e: e.dma_start(out=causal[:], in_=causal_d), writes=["causal"], dma=True)
                p.op("sp", lambda e: e.dma_start(out=alibi[:].rearrange("p h d -> p (h d)"), in_=alibi_d), writes=["alibi"], dma=True)

                HI, LO, RW, NLO, MID, CNT, TTS, THR = range(8)
                rot = {"ii": 0, "ri": 0, "si": 0}

                def gen_front(j):
                    nk = 128 * (j + 1)
                    nch = (nk + 511) // 512
                    scj = sc[j % 2]
                    scn = f"sc{j % 2}"
                    q0 = j * 128
                    mT = maskT[j % 2]
                    mTn = f"maskT{j % 2}"
                    p.op("dve", lambda e, j=j: e.tensor_tensor(
                        out=dgw[:, :, :], in0=ident_f[:, :].unsqueeze(1).to_broadcast([128, 16, 128]),
                        in1=wi_t[:, j, :].unsqueeze(2).to_broadcast([128, 16, 128]), op=ALU.mult),
                        reads=["ident_f", f"wi{j}"], writes=["dg"])
                    for n in range(nch):
                        c0 = n * 512
                        cw = min(512, nk - c0)
                        for h in range(16):
                            m, half = h // 2, h % 2
                            pl, ph = half * 64, half * 64 + 64
                            pI = psI[rot["ii"] % 2]
                            pIn = f"psI{rot['ii'] % 2}"
                            rot["ii"] += 1
                            r_ = rl[rot["ri"] % 3]
                            rn = f"rl{rot['ri'] % 3}"
                            rot["ri"] += 1
                            p.op("pe", lambda e, pI=pI, m=m, pl=pl, ph=ph, q0=q0, c0=c0, cw=cw: e.matmul(
                                pI[:, 0:cw], lhsT=qiT[pl:ph, m, q0:q0 + 128], rhs=kiT2[pl:ph, c0:c0 + cw],
                                start=True, stop=True), reads=[f"qiT{m}", "kiT2"], writes=[pIn])
                            p.op("act", lambda e, pI=pI, r_=r_, cw=cw: e.activation(out=r_[:, 0:cw], in_=pI[:, 0:cw], func=AF.Relu),
                                 reads=[pIn], writes=[rn])
                            p.op("pe", lambda e, r_=r_, h=h, cw=cw: e.matmul(
                                psAccI[:, 0:cw], lhsT=dgw[:, h, :], rhs=r_[:, 0:cw], start=(h == 0), stop=(h == 15)),
                                reads=[rn, "dg"], writes=["psAcc"] if h == 0 else (), appends=["psAcc"] if h > 0 else ())
                            yield
                        p.op("act", lambda e, scj=scj, c0=c0, cw=cw: e.activation(out=scj[:, c0:c0 + cw], in_=psAccI[:, 0:cw], func=AF.Identity),
                             reads=["psAcc"], writes=[f"{scn}_{n}"])
                    scall = [f"{scn}_{n}" for n in range(nch)]
                    p.op("dve", lambda e, scj=scj, q0=q0: e.tensor_tensor(out=scj[:, q0:q0 + 128], in0=scj[:, q0:q0 + 128], in1=causal[:, :], op=ALU.add),
                         reads=scall + ["causal"], writes=scall)
                    yield "BIS"
                    if j < 2:
                        p.op("dve", lambda e: e.memset(bs[:, THR:THR + 1], -1.0e29), writes=["bs"])
                    else:
                        p.op("dve", lambda e, scj=scj, nk=nk: e.tensor_reduce(out=bs[:, HI:HI + 1], in_=scj[:, 0:nk], axis=AX.X, op=ALU.max),
                             reads=scall + ["bs"], writes=["bs"])
                        p.op("dve", lambda e, scj=scj, q0=q0: e.tensor_reduce(out=bs[:, LO:LO + 1], in_=scj[:, 0:q0], axis=AX.X, op=ALU.min),
                             reads=scall + ["bs"], writes=["bs"])
                        yield
                        p.op("dve", lambda e: e.tensor_tensor(out=bs[:, RW:RW + 1], in0=bs[:, HI:HI + 1], in1=bs[:, LO:LO + 1], op=ALU.subtract),
                             reads=["bs"], writes=["bs"])
                        p.op("dve", lambda e: e.reciprocal(out=bs[:, RW:RW + 1], in_=bs[:, RW:RW + 1]), reads=["bs"], writes=["bs"])
                        p.op("dve", lambda e: e.scalar_tensor_tensor(out=bs[:, NLO:NLO + 1], in0=bs[:, LO:LO + 1], scalar=-1.0, in1=bs[:, RW:RW + 1],
                                                                    op0=ALU.mult, op1=ALU.mult), reads=["bs"], writes=["bs"])
                        yield
                        p.op("dve", lambda e, scj=scj, nk=nk: e.tensor_scalar(out=scj[:, 0:nk], in0=scj[:, 0:nk], scalar1=bs[:, RW:RW + 1], scalar2=bs[:, NLO:NLO + 1],
                                                                       op0=ALU.mult, op1=ALU.add), reads=scall + ["bs"], writes=scall)
                        p.op("dve", lambda e: e.memset(bs[:, MID:MID + 1], 0.5), reads=["bs"], writes=["bs"])
                        yield
                        for it in range(NBIS):
                            p.op("dve", lambda e, scj=scj, nk=nk: e.tensor_scalar(
                                out=junk[:, 0:nk], in0=scj[:, 0:nk], scalar1=bs[:, MID:MID + 1], scalar2=0.0,
                                op0=ALU.is_ge, op1=ALU.add, accum_out=bs[:, CNT:CNT + 1]),
                                reads=scall + ["bs"], writes=["bs", "junk"])
                            yield
                            p.op("dve", lambda e, it=it: e.tensor_scalar(
                                out=bs[:, TTS:TTS + 1], in0=bs[:, CNT:CNT + 1], scalar1=TOPK - 0.5, scalar2=0.5 ** (it + 1),
                                op0=ALU.is_ge, op1=ALU.mult), reads=["bs"], writes=["bs"])
                            yield
                            p.op("dve", lambda e, it=it: e.scalar_tensor_tensor(
                                out=bs[:, MID:MID + 1], in0=bs[:, TTS:TTS + 1], scalar=-(0.5 ** (it + 2)), in1=bs[:, MID:MID + 1],
                                op0=ALU.add, op1=ALU.add), reads=["bs"], writes=["bs"])
                            yield
                        p.op("dve", lambda e: e.tensor_scalar(out=bs[:, THR:THR + 1], in0=bs[:, MID:MID + 1], scalar1=-(0.5 ** (NBIS + 1)), scalar2=None, op0=ALU.add),
                             reads=["bs"], writes=["bs"])
                    p.op("dve", lambda e, scj=scj, nk=nk: e.tensor_scalar(
                        out=mask[:, 0:nk], in0=scj[:, 0:nk], scalar1=bs[:, THR:THR + 1], scalar2=None, op0=ALU.is_ge),
                        reads=scall + ["bs"], writes=["mask"])
                    yield
                    for g0 in range(0, j + 1, 8):
                        g1 = min(j + 1, g0 + 8)
                        for i in range(g0, g1):
                            p.op("pe", lambda e, i=i, g0=g0: e.transpose(psT[:, (i - g0) * 128:(i - g0 + 1) * 128], mask[:, i * 128:(i + 1) * 128], ident_bf[:]),
                                 reads=["mask", "ident_bf"], writes=["psT"] if i == g0 else (), appends=["psT"] if i > g0 else ())
                        p.op("act", lambda e, g0=g0, g1=g1, mT=mT: e.activation(
                            out=mT[:, g0:g1, :], in_=psT[:, 0:(g1 - g0) * 128].rearrange("p (g t) -> p g t", t=128), func=AF.Identity),
                            reads=["psT"], writes=[mTn] if g0 == 0 else (), appends=[mTn] if g0 > 0 else ())
                        yield

                def gen_attn(j):
                    q0 = j * 128
                    mT = maskT[j % 2]
                    mTn = f"maskT{j % 2}"
                    steps = [(c, i) for c in range(2) for i in range(j + 1)]
                    par = []

                    def emit_st(k):
                        c, i = steps[k]
                        k_ = rot["si"] % 2
                        rot["si"] += 1
                        par.append(k_)
                        pS, pSn = psS[k_], f"psS{k_}"
                        for hh in range(4):
                            p.op("pe", lambda e, pS=pS, c=c, i=i, hh=hh, q0=q0: e.matmul(
                                pS[:, hh * 128:(hh + 1) * 128], lhsT=KT[:, c, i * 128:(i + 1) * 128],
                                rhs=QT[:, 4 * c + hh, q0:q0 + 128], start=True, stop=True),
                                reads=[f"KT{c}", f"QT{4 * c + hh}"], writes=[pSn] if hh == 0 else (), appends=[pSn] if hh > 0 else ())

                    emit_st(0)
                    for k, (c, i) in enumerate(steps):
                        if k + 1 < len(steps):
                            emit_st(k + 1)
                        k_ = par[k]
                        pS, pSn = psS[k_], f"psS{k_}"
                        PT_, PTn = PTb[k_], f"PT{k_}"
                        PM_, PMn = PTm[k_], f"PTm{k_}"
                        for hh in range(4):
                            p.op("act", lambda e, pS=pS, PT_=PT_, c=c, i=i, hh=hh, j=j: e.activation(
                                out=PT_[:, hh, :], in_=pS[:, hh * 128:(hh + 1) * 128], func=AF.Exp,
                                bias=alibi[:, 4 * c + hh, (j - i):(j - i) + 1], scale=128.0 ** -0.5),
                                reads=[pSn, "alibi"], writes=[PTn] if hh == 0 else (), appends=[PTn] if hh > 0 else ())
                        p.op("dve", lambda e, PT_=PT_, PM_=PM_, i=i, mT=mT: e.tensor_tensor(
                            out=PM_[:, :, :], in0=PT_[:, :, :], in1=mT[:, i:i + 1, :].to_broadcast([128, 4, 128]), op=ALU.mult),
                            reads=[PTn, mTn], writes=[PMn])
                        p.op("pe", lambda e, PM_=PM_, c=c, i=i, j=j: e.matmul(
                            psO[:, :], lhsT=Vt[:, i, c * 128:(c + 1) * 128], rhs=PM_[:, :, :].rearrange("p h t -> p (h t)"),
                            start=(i == 0), stop=(i == j)),
                            reads=[PMn, f"Vt{i}"], writes=["psO"] if i == 0 else (), appends=["psO"] if i > 0 else ())
                        p.op("pe", lambda e, PM_=PM_, i=i, j=j: e.matmul(
                            psD[:, :], lhsT=ones_bf[:, :], rhs=PM_[:, :, :].rearrange("p h t -> p (h t)"),
                            start=(i == 0), stop=(i == j)),
                            reads=[PMn, "ones_bf"], writes=["psD"] if i == 0 else (), appends=["psD"] if i > 0 else ())
                        if i == j:
                            ys = yst[c]
                            ysn = f"yst{c}"
                            p.op("dve", lambda e: e.reciprocal(out=rden[:, :], in_=psD[:, :]), reads=["psD"], writes=["rden"])
                            p.op("dve", lambda e, ys=ys: e.tensor_tensor(out=ys[:, :, :].rearrange("p h t -> p (h t)"), in0=psO[:, :], in1=rden[:, :], op=ALU.mult),
                                 reads=["psO", "rden"], writes=[ysn])
                            p.op("sp", lambda e, ys=ys, c=c, q0=q0: e.dma_start(
                                out=ycatT_s[1024 + c * 512:1024 + (c + 1) * 512, q0:q0 + 128].rearrange("(h p) t -> p h t", p=128), in_=ys[:, :, :]),
                                reads=[ysn], writes=[f"s_yat{c}"], dma=True)
                        yield

                for _ in gen_front(0):
                    pass
                for j in range(16):
                    fa = gen_attn(j)
                    ff = gen_front(j + 1) if j + 1 < 16 else None
                    if ff is not None:
                        for tok in ff:
                            if tok == "BIS":
                                break
                        else:
                            ff = None
                    gens = [fa] + ([ff] if ff is not None else [])
                    while gens:
                        for g_ in list(gens):
                            try:
                                next(g_)
                            except StopIteration:
                                gens.remove(g_)
        p.barrier()
        if stop_after <= 2:
            p.wait_all("sp", [])
            p.emit()
            return nc

        with Phase(nc) as P3:
            prm = P3.sb("prm", [128, 96], F32)
            sv = P3.sb("sv", [128, 16, 32], F32)
            Bre = P3.sb("Bre", [128, 32, 128], BF16)
            Bim = P3.sb("Bim", [128, 32, 128], BF16)
            CR = P3.sb("CR", [128, 32, 128], BF16)
            NCR = P3.sb("NCR", [128, 32, 128], BF16)
            NCI = P3.sb("NCI", [128, 32, 128], BF16)
            colp = P3.sb("colp", [128, 16], F32)
            diagD = P3.sb("diagD", [128, 8, 128], BF16)
            iot = P3.sb("iot", [128, T], F32)
            hpi = P3.sb("hpi", [128, 1], F32)
            p.op("dve", lambda e: e.memset(hpi[:], 0.5 * math.pi), writes=["prm"])
            p.op("sp", lambda e: e.dma_start(out=prm[:], in_=ssm_prm_d), appends=["prm"], dma=True)
            p.op("sp", lambda e: e.dma_start(out=colp[:], in_=colp_d), writes=["colp"], dma=True)
            p.op("sp", lambda e: e.dma_start(out=iot[:], in_=iota_d), writes=["iot"], dma=True)
            p.op("pool", lambda e: e.dma_start(out=Bre[:].rearrange("p a b -> p (a b)"), in_=bpad_re_d), writes=["Bre"], dma=True)
            p.op("pool", lambda e: e.dma_start(out=Bim[:].rearrange("p a b -> p (a b)"), in_=bpad_im_d), writes=["Bim"], dma=True)

            A_RE, A_IM, LDT = prm[:, 0:32], prm[:, 32:64], prm[:, 64:96]
            (DT, RR, TH, CO, SI, FRE, FIM, T0, T1, T2, T3, DEN) = range(12)

            def svv(k):
                return sv[:, k, :]

            def d1(fn, **kw):
                p.op("dve", fn, reads=["prm", "sv"], writes=["sv"])

            def a1(fn):
                p.op("act", fn, reads=["prm", "sv"], writes=["sv"])

            a1(lambda e: e.activation(out=svv(DT), in_=LDT, func=AF.Exp))
            d1(lambda e: e.tensor_tensor(out=svv(T0), in0=svv(DT), in1=A_RE, op=ALU.mult))
            a1(lambda e: e.activation(out=svv(RR), in_=svv(T0), func=AF.Exp))
            d1(lambda e: e.tensor_tensor(out=svv(TH), in0=svv(DT), in1=A_IM, op=ALU.mult))
            d1(lambda e: e.tensor_scalar(out=svv(TH), in0=svv(TH), scalar1=1.0 / (2.0 * math.pi), scalar2=None, op0=ALU.mult))
            d1(lambda e: e.tensor_scalar(out=svv(T0), in0=svv(TH), scalar1=MAGIC, scalar2=None, op0=ALU.add))
            d1(lambda e: e.tensor_scalar(out=svv(T0), in0=svv(T0), scalar1=-MAGIC, scalar2=None, op0=ALU.add))
            d1(lambda e: e.tensor_tensor(out=svv(T1), in0=svv(TH), in1=svv(T0), op=ALU.subtract))
            a1(lambda e: e.activation(out=svv(SI), in_=svv(T1), func=AF.Sin, scale=TWO_PI_S))
            a1(lambda e: e.activation(out=svv(T2), in_=svv(T1), func=AF.Abs))
            a1(lambda e: e.activation(out=svv(CO), in_=svv(T2), func=AF.Sin, bias=hpi[:, 0:1], scale=-TWO_PI_S))
            d1(lambda e: e.tensor_tensor(out=svv(T0), in0=svv(RR), in1=svv(CO), op=ALU.mult))
            d1(lambda e: e.tensor_scalar(out=svv(T0), in0=svv(T0), scalar1=-1.0, scalar2=None, op0=ALU.add))
            d1(lambda e: e.tensor_tensor(out=svv(T1), in0=svv(RR), in1=svv(SI), op=ALU.mult))
            d1(lambda e: e.tensor_tensor(out=svv(DEN), in0=A_RE, in1=A_RE, op=ALU.mult))
            d1(lambda e: e.tensor_tensor(out=svv(T2), in0=A_IM, in1=A_IM, op=ALU.mult))
            d1(lambda e: e.tensor_tensor(out=svv(DEN), in0=svv(DEN), in1=svv(T2), op=ALU.add))
            d1(lambda e: e.reciprocal(out=svv(DEN), in_=svv(DEN)))
            d1(lambda e: e.tensor_tensor(out=svv(T2), in0=svv(T0), in1=A_RE, op=ALU.mult))
            d1(lambda e: e.tensor_tensor(out=svv(T3), in0=svv(T1), in1=A_IM, op=ALU.mult))
            d1(lambda e: e.tensor_tensor(out=svv(T2), in0=svv(T2), in1=svv(T3), op=ALU.add))
            d1(lambda e: e.tensor_tensor(out=svv(FRE), in0=svv(T2), in1=svv(DEN), op=ALU.mult))
            d1(lambda e: e.tensor_tensor(out=svv(T2), in0=svv(T1), in1=A_RE, op=ALU.mult))
            d1(lambda e: e.tensor_tensor(out=svv(T3), in0=svv(T0), in1=A_IM, op=ALU.mult))
            d1(lambda e: e.tensor_tensor(out=svv(T2), in0=svv(T2), in1=svv(T3), op=ALU.subtract))
            d1(lambda e: e.tensor_tensor(out=svv(FIM), in0=svv(T2), in1=svv(DEN), op=ALU.mult))

            with Phase(nc) as P3c:
                cre = P3c.sb("cre", [128, 32, 128], F32)
                cim = P3c.sb("cim", [128, 32, 128], F32)
                ct1 = P3c.sb("ct1", [128, 32, 128], F32)
                ct2 = P3c.sb("ct2", [128, 32, 128], F32)
                p.op("sp", lambda e: e.dma_start(out=cre[:].rearrange("p a b -> p (a b)"), in_=cpad_re_d), writes=["cre"], dma=True)
                p.op("sp", lambda e: e.dma_start(out=cim[:].rearrange("p a b -> p (a b)"), in_=cpad_im_d), writes=["cim"], dma=True)

                def bc(k):
                    return sv[:, k, :].unsqueeze(2).to_broadcast([128, 32, 128])
                p.op("dve", lambda e: e.tensor_tensor(out=ct1[:], in0=cre[:], in1=bc(FRE), op=ALU.mult), reads=["cre", "sv"], writes=["ct1"])
                p.op("dve", lambda e: e.tensor_tensor(out=ct2[:], in0=cim[:], in1=bc(FIM), op=ALU.mult), reads=["cim", "sv"], writes=["ct2"])
                p.op("dve", lambda e: e.tensor_tensor(out=CR[:], in0=ct1[:], in1=ct2[:], op=ALU.subtract), reads=["ct1", "ct2"], writes=["CR"])
                p.op("dve", lambda e: e.tensor_tensor(out=NCR[:], in0=ct2[:], in1=ct1[:], op=ALU.subtract), reads=["ct1", "ct2"], writes=["NCR"])
                p.op("dve", lambda e: e.tensor_tensor(out=ct1[:], in0=cre[:], in1=bc(FIM), op=ALU.mult), reads=["cre", "sv", "CR", "NCR"], writes=["ct1"])
                p.op("dve", lambda e: e.tensor_tensor(out=ct2[:], in0=cim[:], in1=bc(FRE), op=ALU.mult), reads=["cim", "sv", "CR", "NCR"], writes=["ct2"])
                p.op("dve", lambda e: e.tensor_tensor(out=ct1[:], in0=ct1[:], in1=ct2[:], op=ALU.add), reads=["ct1", "ct2"], writes=["ct1"])
                p.op("dve", lambda e: e.tensor_scalar(out=NCI[:], in0=ct1[:], scalar1=-1.0, scalar2=None, op0=ALU.mult), reads=["ct1"], writes=["NCI"])
            for ct in range(8):
                p.op("dve", lambda e, ct=ct: e.tensor_scalar(out=diagD[:, ct, :], in0=ident_f[:, :], scalar1=colp[:, ct:ct + 1], scalar2=None, op0=ALU.mult),
                     reads=["ident_f", "colp"], appends=["diagD"])
            p.barrier()

            with Phase(nc) as P3m:
                uTt = [P3m.sb(f"uTt{i}", [128, T], BF16) for i in range(2)]
                angA = P3m.sb("angA", [128, T], F32)
                angB = P3m.sb("angB", [128, T], F32)
                negM = P3m.sb("negM", [128, 1], F32)
                cosb = [P3m.sb(f"cosb{i}", [128, T], BF16) for i in range(2)]
                sinb = [P3m.sb(f"sinb{i}", [128, T], BF16) for i in range(2)]
                breb = [P3m.sb(f"breb{i}", [128, T], BF16) for i in range(2)]
                bimb = [P3m.sb(f"bimb{i}", [128, T], BF16) for i in range(2)]
                tmb = [P3m.sb(f"tmb{i}", [128, T], BF16) for i in range(4)]
                xreb = P3m.sb("xreb", [128, T], BF16)
                ximb = P3m.sb("ximb", [128, T], BF16)
                greb = P3m.sb("greb", [128, T], BF16)
                gimb = P3m.sb("gimb", [128, T], BF16)
                PP = [P3m.sb(f"PP{i}", [128, T], BF16) for i in range(4)]
                ygs = [P3m.sb(f"ygs{i}", [128, T], BF16) for i in range(2)]
                psB = P3m.ps("psBu", [128, T], F32)
                psY = P3m.ps("psY", [128, T], F32)
                psYn = [f"psY{n}" for n in range(4)]
                p.op("dve", lambda e: e.memset(negM[:], -MAGIC), writes=["negM"])

                def tab_A(q):
                    p.op("dve", lambda e, q=q: e.tensor_scalar(out=angA[:, :], in0=iot[:, :], scalar1=sv[:, TH, q:q + 1], scalar2=MAGIC, op0=ALU.mult, op1=ALU.add),
                         reads=["iot", "sv"], writes=["angA"])

                def tab_B(q):
                    p.op("act", lambda e: e.activation(out=angB[:, :], in_=angA[:, :], func=AF.Identity, bias=negM[:, 0:1], scale=1.0),
                         reads=["angA", "negM"], writes=["angB"])

                def tab_C(q):
                    p.op("dve", lambda e, q=q: e.scalar_tensor_tensor(out=angA[:, :], in0=iot[:, :], scalar=sv[:, TH, q:q + 1], in1=angB[:, :], op0=ALU.mult, op1=ALU.subtract),
                         reads=["iot", "sv", "angB"], writes=["angA"])

                def tab_D(q):
                    cb, cbn = cosb[q % 2], f"cosb{q % 2}"
                    sb_, sbn = sinb[q % 2], f"sinb{q % 2}"
                    p.op("act", lambda e, sb_=sb_: e.activation(out=sb_[:, :], in_=angA[:, :], func=AF.Sin, scale=TWO_PI_S), reads=["angA"], writes=[sbn])
                    p.op("act", lambda e: e.activation(out=angB[:, :], in_=angA[:, :], func=AF.Abs), reads=["angA"], writes=["angB"])
                    p.op("act", lambda e, cb=cb: e.activation(out=cb[:, :], in_=angB[:, :], func=AF.Sin, bias=hpi[:, 0:1], scale=-TWO_PI_S), reads=["angB", "hpi"], writes=[cbn])

                def load_u(ct):
                    p.op("sp", lambda e, ct=ct: e.dma_start(out=uTt[ct % 2][:, :], in_=uT_s[ct * 128:(ct + 1) * 128, :]),
                         reads=[f"s_uT{ct}"], writes=[f"uTt{ct % 2}"], dma=True)

                def emit_dterm(ct):
                    ut, utn = uTt[ct % 2], f"uTt{ct % 2}"
                    for n in range(4):
                        p.op("pe", lambda e, ut=ut, ct=ct, n=n: e.matmul(psY[:, n * 512:(n + 1) * 512], lhsT=diagD[:, ct, :],
                                                                     rhs=ut[:, n * 512:(n + 1) * 512], start=True, stop=False),
                             reads=[utn, "diagD"], writes=[psYn[n]])

                def emit_bu(q):
                    ct = q // 4
                    ut, utn = uTt[ct % 2], f"uTt{ct % 2}"
                    br, brn = breb[q % 2], f"breb{q % 2}"
                    bi, bin_ = bimb[q % 2], f"bimb{q % 2}"
                    for hf in range(2):
                        t0 = hf * 1024
                        pbn = ["psB0", "psB1", "psB2", "psB3"]
                        for part, Bw, bwn in ((0, Bre, "Bre"), (1, Bim, "Bim")):
                            for n in range(2):
                                p.op("pe", lambda e, Bw=Bw, q=q, ut=ut, part=part, n=n, t0=t0: e.matmul(
                                    psB[:, part * 1024 + n * 512:part * 1024 + (n + 1) * 512], lhsT=Bw[:, q, :],
                                    rhs=ut[:, t0 + n * 512:t0 + (n + 1) * 512], start=True, stop=True),
                                    reads=[bwn, utn], writes=[pbn[part * 2 + n]])
                        sl = slice(t0, t0 + 1024)
                        p.op("act", lambda e, sl=sl, br=br: e.activation(out=br[:, sl], in_=psB[:, 0:1024], func=AF.Identity),
                             reads=["psB0", "psB1"], writes=[brn] if hf == 0 else (), appends=[brn] if hf else ())
                        p.op("act", lambda e, sl=sl, bi=bi: e.activation(out=bi[:, sl], in_=psB[:, 1024:2048], func=AF.Identity),
                             reads=["psB2", "psB3"], writes=[bin_] if hf == 0 else (), appends=[bin_] if hf else ())

                load_u(0)
                tab_A(0); tab_B(0); tab_C(0); tab_D(0)
                emit_bu(0)
                for q in range(32):
                    ct, pq = divmod(q, 4)
                    cb, cbn = cosb[q % 2], f"cosb{q % 2}"
                    sb_, sbn = sinb[q % 2], f"sinb{q % 2}"
                    br, brn = breb[q % 2], f"breb{q % 2}"
                    bi, bin_ = bimb[q % 2], f"bimb{q % 2}"
                    if pq == 0:
                        if ct + 1 < 8:
                            load_u(ct + 1)
                        emit_dterm(ct)
                    if q + 1 < 32:
                        tab_A(q + 1)
                        tab_B(q + 1)
                        emit_bu(q + 1)
                    p.op("dve", lambda e, cb=cb, br=br: e.tensor_tensor(out=tmb[0][:, :], in0=br[:, :], in1=cb[:, :], op=ALU.mult), reads=[brn, cbn], writes=["tmb0"])
                    p.op("dve", lambda e, sb_=sb_, bi=bi: e.tensor_tensor(out=tmb[1][:, :], in0=bi[:, :], in1=sb_[:, :], op=ALU.mult), reads=[bin_, sbn], writes=["tmb1"])
                    p.op("dve", lambda e, cb=cb, bi=bi: e.tensor_tensor(out=tmb[2][:, :], in0=bi[:, :], in1=cb[:, :], op=ALU.mult), reads=[bin_, cbn], writes=["tmb2"])
                    p.op("dve", lambda e, sb_=sb_, br=br: e.tensor_tensor(out=tmb[3][:, :], in0=br[:, :], in1=sb_[:, :], op=ALU.mult), reads=[brn, sbn], writes=["tmb3"])
                    p.op("dve", lambda e: e.tensor_tensor(out=xreb[:, :], in0=tmb[0][:, :], in1=tmb[1][:, :], op=ALU.add), reads=["tmb0", "tmb1"], writes=["xreb"])
                    p.op("dve", lambda e: e.tensor_tensor(out=ximb[:, :], in0=tmb[2][:, :], in1=tmb[3][:, :], op=ALU.subtract), reads=["tmb2", "tmb3"], writes=["ximb"])
                    if q + 1 < 32:
                        tab_C(q + 1)
                        tab_D(q + 1)
                    p.op("dve", lambda e, q=q: e.tensor_tensor_scan(out=greb[:, :], data0=sv[:, RR, q:q + 1].to_broadcast([128, T]), data1=xreb[:, :], initial=0.0, op0=ALU.mult, op1=ALU.add),
                         reads=["sv", "xreb"], writes=["greb"])
                    p.op("dve", lambda e, q=q: e.tensor_tensor_scan(out=gimb[:, :], data0=sv[:, RR, q:q + 1].to_broadcast([128, T]), data1=ximb[:, :], initial=0.0, op0=ALU.mult, op1=ALU.add),
                         reads=["sv", "ximb"], writes=["gimb"])
                    p.op("dve", lambda e, cb=cb: e.tensor_tensor(out=PP[0][:, :], in0=greb[:, :], in1=cb[:, :], op=ALU.mult), reads=["greb", cbn], writes=["PP0"])
                    p.op("dve", lambda e, sb_=sb_: e.tensor_tensor(out=PP[1][:, :], in0=gimb[:, :], in1=sb_[:, :], op=ALU.mult), reads=["gimb", sbn], writes=["PP1"])
                    p.op("dve", lambda e, sb_=sb_: e.tensor_tensor(out=PP[2][:, :], in0=greb[:, :], in1=sb_[:, :], op=ALU.mult), reads=["greb", sbn], writes=["PP2"])
                    p.op("dve", lambda e, cb=cb: e.tensor_tensor(out=PP[3][:, :], in0=gimb[:, :], in1=cb[:, :], op=ALU.mult), reads=["gimb", cbn], writes=["PP3"])
                    for k, (Cw, cwn) in enumerate(((CR, "CR"), (NCR, "NCR"), (NCI, "NCI"), (NCI, "NCI"))):
                        for n in range(4):
                            last = (pq == 3 and k == 3)
                            p.op("pe", lambda e, Cw=Cw, q=q, k=k, n=n, last=last: e.matmul(
                                psY[:, n * 512:(n + 1) * 512], lhsT=Cw[:, q, :], rhs=PP[k][:, n * 512:(n + 1) * 512],
                                start=False, stop=last), reads=[cwn, f"PP{k}"], appends=[psYn[n]])
                    if pq == 3:
                        yg = ygs[ct % 2]
                        ygn = f"ygs{ct % 2}"
                        p.op("act", lambda e, yg=yg: e.activation(out=yg[:, :], in_=psY[:, :], func=AF.Gelu), reads=psYn, writes=[ygn])
                        p.op("sp", lambda e, yg=yg, ct=ct: e.dma_start(out=yT_s[ct * 128:(ct + 1) * 128, :], in_=yg[:, :]),
                             reads=[ygn], writes=["s_yT"] if ct == 0 else (), appends=["s_yT"] if ct > 0 else (), dma=True)
        p.barrier()
        if stop_after <= 3:
            p.wait_all("sp", [])
            p.emit()
            return nc

        P34 = Phase(nc)
        P34.__enter__()
        wo = P34.sb("wo", [128, 16, D], BF16)
        for kc in range(16):
            p.op("pool", lambda e, kc=kc: e.dma_start(out=wo[:, kc, :], in_=w_out_d[kc * 128:(kc + 1) * 128, :]), appends=["wo"], dma=True)
        with Phase(nc) as P3b:
            yTs = P3b.sb("yTs", [128, 8, T], BF16)
            wgl = P3b.sb("wgl", [128, 8, 1024], BF16)
            colp2 = P3b.sb("colp2", [128, 16], F32)
            sg = [P3b.sb(f"sg{i}", [128, T], F32) for i in range(2)]
            yso = [P3b.sb(f"yso{i}", [128, T], BF16) for i in range(2)]
            psZ = [P3b.ps(f"psZ{i}", [128, T], F32) for i in range(2)]
            p.op("sp", lambda e: e.dma_start(out=colp2[:], in_=colp_d), writes=["colp2"], dma=True)
            p.op("sp", lambda e: e.dma_start(out=yTs[:, :, :], in_=yT_s.rearrange("(c p) t -> p c t", p=128)), writes=["yTs"], dma=True)
            p.op("pool", lambda e: e.dma_start(out=wgl[:, :, :], in_=w_glu_d.rearrange("(c p) n -> p c n", p=128)), writes=["wgl"], dma=True)
            for mo in range(8):
                pz = psZ[mo % 2]
                pzn = [f"psZ{mo % 2}_{n}" for n in range(4)]
                for kc in range(8):
                    for n in range(4):
                        p.op("pe", lambda e, pz=pz, mo=mo, kc=kc, n=n: e.matmul(
                            pz[:, n * 512:(n + 1) * 512], lhsT=wgl[:, kc, mo * 128:(mo + 1) * 128], rhs=yTs[:, kc, n * 512:(n + 1) * 512],
                            start=(kc == 0), stop=(kc == 7)), reads=["wgl", "yTs"],
                            writes=[pzn[n]] if kc == 0 else (), appends=[pzn[n]] if kc > 0 else ())
                s_ = sg[mo % 2]
                sn = f"sg{mo % 2}"
                yo = yso[mo % 2]
                yon = f"yso{mo % 2}"
                p.op("act", lambda e, pz=pz, s_=s_, mo=mo: e.activation(out=s_[:, :], in_=pz[:, :], func=AF.Sigmoid, bias=colp2[:, 8 + mo:9 + mo], scale=1.0),
                     reads=pzn + ["colp2"], writes=[sn])
                p.op("dve", lambda e, s_=s_, yo=yo, mo=mo: e.tensor_tensor(out=yo[:, :], in0=yTs[:, mo, :], in1=s_[:, :], op=ALU.mult),
                     reads=[sn, "yTs"], writes=[yon])
                p.op("sp", lambda e, yo=yo, mo=mo: e.dma_start(out=ycatT_s[mo * 128:(mo + 1) * 128, :], in_=yo[:, :]),
                     reads=[yon], writes=["s_yssm"] if mo == 0 else (), appends=["s_yssm"] if mo > 0 else (), dma=True)
        p.barrier()
        if stop_after <= 4:
            p.wait_all("sp", [])
            p.emit()
            return nc

        def layer_norm(pre_ap, prn, gam, bet, gnames, stt, sttn, lnj):
            p.op("dve", lambda e: e.memset(stt[:, 0:2], 0.0), writes=[sttn])
            p.op("act", lambda e: e.activation(out=lnj[:, :], in_=pre_ap, func=AF.Identity, accum_out=stt[:, 0:1]),
                 reads=[prn, sttn], writes=["lnj", sttn])
            p.op("act", lambda e: e.activation(out=lnj[:, :], in_=pre_ap, func=AF.Square, accum_out=stt[:, 1:2]),
                 reads=[prn, sttn], writes=["lnj", sttn])
            p.op("dve", lambda e: e.tensor_scalar(out=stt[:, 2:4], in0=stt[:, 0:2], scalar1=1.0 / D, scalar2=None, op0=ALU.mult), reads=[sttn], writes=[sttn])
            p.op("dve", lambda e: e.tensor_tensor(out=stt[:, 4:5], in0=stt[:, 2:3], in1=stt[:, 2:3], op=ALU.mult), reads=[sttn], writes=[sttn])
            p.op("dve", lambda e: e.tensor_tensor(out=stt[:, 5:6], in0=stt[:, 3:4], in1=stt[:, 4:5], op=ALU.subtract), reads=[sttn], writes=[sttn])
            p.op("dve", lambda e: e.tensor_scalar(out=stt[:, 5:6], in0=stt[:, 5:6], scalar1=EPS, scalar2=None, op0=ALU.add), reads=[sttn], writes=[sttn])
            p.op("act", lambda e: e.activation(out=stt[:, 7:8], in_=stt[:, 5:6], func=AF.Sqrt), reads=[sttn], writes=[sttn])
            p.op("dve", lambda e: e.reciprocal(out=stt[:, 5:6], in_=stt[:, 7:8]), reads=[sttn], writes=[sttn])
            p.op("dve", lambda e: e.scalar_tensor_tensor(out=stt[:, 6:7], in0=stt[:, 2:3], scalar=-1.0, in1=stt[:, 5:6], op0=ALU.mult, op1=ALU.mult), reads=[sttn], writes=[sttn])
            p.op("dve", lambda e: e.tensor_scalar(out=pre_ap, in0=pre_ap, scalar1=stt[:, 5:6], scalar2=stt[:, 6:7], op0=ALU.mult, op1=ALU.add),
                 reads=[prn, sttn], writes=[prn])
            p.op("dve", lambda e: e.tensor_tensor(out=pre_ap, in0=pre_ap, in1=gam[:, :], op=ALU.mult), reads=[prn] + gnames, writes=[prn])
            p.op("dve", lambda e: e.tensor_tensor(out=pre_ap, in0=pre_ap, in1=bet[:, :], op=ALU.add), reads=[prn] + gnames, writes=[prn])

        with Phase(nc) as P4:
            yct = [P4.sb(f"yct{i}", [128, 16, 128], BF16) for i in range(2)]
            xt = [P4.sb(f"xt{i}", [128, D], F32) for i in range(2)]
            lnj = P4.sb("lnj", [128, D], BF16)
            gam = P4.sb("gam", [128, D], F32)
            bet = P4.sb("bet", [128, D], F32)
            stt = P4.sb("stt", [128, 8], F32)
            h1b = P4.sb("h1b", [128, D], BF16)
            hTs = [P4.sb(f"hTs{i}", [128, 16, 128], BF16) for i in range(2)]
            psM = P4.ps("psM", [128, D], F32)
            psTr = P4.ps("psTr", [128, D], BF16)
            psMn = [f"psM{n}" for n in range(4)]
            p.op("sp", lambda e: e.dma_start(out=gam[:], in_=ln_d[0]), writes=["gam"], dma=True)
            p.op("sp", lambda e: e.dma_start(out=bet[:], in_=ln_d[1]), writes=["bet"], dma=True)
            ycat_v = ycatT_s.rearrange("(c p) t -> p c t", p=128)
            h1T_v = h1T_s.rearrange("(c p) t -> p c t", p=128)

            def p4_load(tt):
                p.op("sp", lambda e, tt=tt: e.dma_start(out=yct[tt % 2][:, :, :], in_=ycat_v[:, :, tt * 128:(tt + 1) * 128]),
                     reads=["s_yssm", "s_yat0", "s_yat1"], writes=[f"yct{tt % 2}"], dma=True)
                p.op("sp", lambda e, tt=tt: e.dma_start(out=xt[tt % 2][:, :], in_=x_d[tt * 128:(tt + 1) * 128, :]),
                     writes=[f"xt{tt % 2}"], dma=True)
            def p4_mm(tt):
                yc, ycn = yct[tt % 2], f"yct{tt % 2}"
                for kc in range(16):
                    for n in range(4):
                        p.op("pe", lambda e, yc=yc, kc=kc, n=n: e.matmul(psM[:, n * 512:(n + 1) * 512], lhsT=yc[:, kc, :],
                                                                      rhs=wo[:, kc, n * 512:(n + 1) * 512], start=(kc == 0), stop=(kc == 15)),
                             reads=[ycn, "wo"], writes=[psMn[n]] if kc == 0 else (), appends=[psMn[n]] if kc > 0 else ())
            p4_load(0)
            p4_load(1)
            p4_mm(0)
            for tt in range(16):
                x_, xn = xt[tt % 2], f"xt{tt % 2}"
                p.op("dve", lambda e, x_=x_: e.scalar_tensor_tensor(out=x_[:, :], in0=x_[:, :], scalar=ALPHA, in1=psM[:, :], op0=ALU.mult, op1=ALU.add),
                     reads=psMn + [xn], writes=[xn])
                if tt + 1 < 16:
                    p4_mm(tt + 1)
                if debug:
                    p.op("sp", lambda e, x_=x_, tt=tt: e.dma_start(out=out_d[tt * 128:(tt + 1) * 128, :], in_=x_[:, :]),
                         reads=[xn], writes=["out_dbg"], dma=True)
                layer_norm(x_[:, :], xn, gam, bet, ["gam", "bet"], stt, "stt", lnj)
                p.op("sp", lambda e, x_=x_, tt=tt: e.dma_start(out=h1_s[tt * 128:(tt + 1) * 128, :], in_=x_[:, :]),
                     reads=[xn], writes=["s_h1"] if tt == 0 else (), appends=["s_h1"] if tt > 0 else (), dma=True)
                p.op("act", lambda e, x_=x_: e.activation(out=h1b[:, :], in_=x_[:, :], func=AF.Identity), reads=[xn], writes=["h1b"])
                for dc in range(16):
                    p.op("pe", lambda e, dc=dc: e.transpose(psTr[:, dc * 128:(dc + 1) * 128], h1b[:, dc * 128:(dc + 1) * 128], ident_bf[:]),
                         reads=["h1b", "ident_bf"], writes=["psTr"] if dc == 0 else (), appends=["psTr"] if dc > 0 else ())
                hs, hsn = hTs[tt % 2], f"hTs{tt % 2}"
                p.op("dve", lambda e, hs=hs: e.tensor_copy(out=hs[:, :, :].rearrange("p c t -> p (c t)"), in_=psTr[:, :]), reads=["psTr"], writes=[hsn])
                p.op("sp", lambda e, hs=hs, tt=tt: e.dma_start(out=h1T_v[:, :, tt * 128:(tt + 1) * 128], in_=hs[:, :, :]),
                     reads=[hsn], writes=["s_h1T"] if tt == 0 else (), appends=["s_h1T"] if tt > 0 else (), dma=True)
                if tt + 2 < 16:
                    p4_load(tt + 2)
        p.barrier()
        P34.__exit__(None, None, None)
        if stop_after <= 5:
            p.wait_all("sp", [])
            p.emit()
            return nc

        with Phase(nc) as P5:
            hT = [P5.sb(f"hT{i}", [128, 16, 512], BF16) for i in range(2)]
            gT = P5.sb("gT", [128, NFC, 512], BF16)
            wu = [P5.sb(f"wu{i}", [128, 16, 256], BF16) for i in range(2)]
            wg = [P5.sb(f"wg{i}", [128, 16, 256], BF16) for i in range(2)]
            wd = [P5.sb(f"wd{i}", [128, 4, 512], BF16) for i in range(2)]
            pre2 = P5.sb("pre2", [128, 4, D], F32)
            lnj5 = P5.sb("lnj5", [128, D], BF16)
            gam5 = P5.sb("gam5", [128, D], F32)
            bet5 = P5.sb("bet5", [128, D], F32)
            stt5 = P5.sb("stt5", [128, 8], F32)
            cvp = P5.sb("cvp", [128, NFC, 4], F32)
            hprev = P5.sb("hprev", [128, NFC, 2], F32)
            hup = [P5.sb(f"hup{i}", [128, 514], F32) for i in range(2)]
            cv = [P5.sb(f"cv{i}", [128, 512], F32) for i in range(2)]
            ge = [P5.sb(f"ge{i}", [128, 512], F32) for i in range(2)]
            psU = [P5.ps(f"psU{i}", [128, 512], F32) for i in range(2)]
            psG = [P5.ps(f"psG{i}", [128, 512], F32) for i in range(2)]
            psA = [P5.ps(f"psAcc{i}", [128, 512], F32) for i in range(4)]
            p.op("sp", lambda e: e.dma_start(out=gam5[:], in_=ln_d[2]), writes=["gam5"], dma=True)
            p.op("sp", lambda e: e.dma_start(out=bet5[:], in_=ln_d[3]), writes=["bet5"], dma=True)
            p.op("sp", lambda e: e.dma_start(out=cvp[:].rearrange("p a b -> p (a b)"), in_=convp_d), writes=["cvp"], dma=True)
            p.op("dve", lambda e: e.memset(hprev[:], 0.0), writes=["hprev"])
            h1T_v5 = h1T_s.rearrange("(c p) t -> p c t", p=128)
            wup_v = w_up_d.rearrange("(c p) f -> p c f", p=128)
            wgt_v = w_gate_d.rearrange("(c p) f -> p c f", p=128)
            wdn_v = w_down_d.rearrange("(k p) d -> p k d", p=128)

            def lnj_fix():
                return None
            wl = 0
            dl = 0
            ev = 0
            for ti in range(4):
                tk0 = ti * 512
                hT_, hTn = hT[ti % 2], f"hT{ti % 2}"
                p.op("sp", lambda e, hT_=hT_, tk0=tk0: e.dma_start(out=hT_[:, :, :], in_=h1T_v5[:, :, tk0:tk0 + 512]),
                     reads=["s_h1T"], writes=[hTn], dma=True)
                p.op("sp", lambda e, tk0=tk0: e.dma_start(out=pre2[:, :, :], in_=h1_s[tk0:tk0 + 512, :].rearrange("(s p) d -> p s d", p=128)),
                     reads=["s_h1"], writes=["pre2"], dma=True)
                for f2 in range(NFC // 2):
                    wu_, wun = wu[wl % 2], f"wu{wl % 2}"
                    wg_, wgn = wg[wl % 2], f"wg{wl % 2}"
                    wl += 1
                    p.op("pool", lambda e, wu_=wu_, f2=f2: e.dma_start(out=wu_[:, :, :], in_=wup_v[:, :, f2 * 256:(f2 + 1) * 256]), writes=[wun], dma=True)
                    p.op("pool", lambda e, wg_=wg_, f2=f2: e.dma_start(out=wg_[:, :, :], in_=wgt_v[:, :, f2 * 256:(f2 + 1) * 256]), writes=[wgn], dma=True)
                    for sub in range(2):
                        fc = f2 * 2 + sub
                        pU, pUn = psU[ev % 2], f"psU{ev % 2}"
                        pG, pGn = psG[ev % 2], f"psG{ev % 2}"
                        hu, hun = hup[ev % 2], f"hup{ev % 2}"
                        cv_, cvn = cv[ev % 2], f"cv{ev % 2}"
                        ge_, gen = ge[ev % 2], f"ge{ev % 2}"
                        ev += 1
                        for kc in range(16):
                            p.op("pe", lambda e, pU=pU, wu_=wu_, kc=kc, sub=sub, hT_=hT_: e.matmul(
                                pU[:, :], lhsT=wu_[:, kc, sub * 128:(sub + 1) * 128], rhs=hT_[:, kc, :], start=(kc == 0), stop=(kc == 15)),
                                reads=[wun, hTn], writes=[pUn] if kc == 0 else (), appends=[pUn] if kc > 0 else ())
                        for kc in range(16):
                            p.op("pe", lambda e, pG=pG, wg_=wg_, kc=kc, sub=sub, hT_=hT_: e.matmul(
                                pG[:, :], lhsT=wg_[:, kc, sub * 128:(sub + 1) * 128], rhs=hT_[:, kc, :], start=(kc == 0), stop=(kc == 15)),
                                reads=[wgn, hTn], writes=[pGn] if kc == 0 else (), appends=[pGn] if kc > 0 else ())
                        p.op("act", lambda e, hu=hu, pU=pU: e.activation(out=hu[:, 2:514], in_=pU[:, :], func=AF.Identity), reads=[pUn], writes=[hun])
                        p.op("act", lambda e, hu=hu, fc=fc: e.activation(out=hu[:, 0:2], in_=hprev[:, fc, :], func=AF.Identity), reads=["hprev"], appends=[hun])
                        p.op("act", lambda e, hu=hu, fc=fc: e.activation(out=hprev[:, fc, :], in_=hu[:, 512:514], func=AF.Identity), reads=[hun], writes=["hprev"])
                        p.op("dve", lambda e, hu=hu, cv_=cv_, fc=fc: e.tensor_scalar(out=cv_[:, :], in0=hu[:, 2:514], scalar1=cvp[:, fc, 2:3], scalar2=cvp[:, fc, 3:4], op0=ALU.mult, op1=ALU.add),
                             reads=[hun, "cvp"], writes=[cvn])
                        p.op("dve", lambda e, hu=hu, cv_=cv_, fc=fc: e.scalar_tensor_tensor(out=cv_[:, :], in0=hu[:, 1:513], scalar=cvp[:, fc, 1:2], in1=cv_[:, :], op0=ALU.mult, op1=ALU.add),
                             reads=[hun, "cvp", cvn], writes=[cvn])
                        p.op("dve", lambda e, hu=hu, cv_=cv_, fc=fc: e.scalar_tensor_tensor(out=cv_[:, :], in0=hu[:, 0:512], scalar=cvp[:, fc, 0:1], in1=cv_[:, :], op0=ALU.mult, op1=ALU.add),
                             reads=[hun, "cvp", cvn], writes=[cvn])
                        p.op("act", lambda e, cv_=cv_, ge_=ge_: e.activation(out=ge_[:, :], in_=cv_[:, :], func=AF.Gelu), reads=[cvn], writes=[gen])
                        p.op("dve", lambda e, ge_=ge_, pG=pG, fc=fc: e.tensor_tensor(out=gT[:, fc, :], in0=ge_[:, :], in1=pG[:, :], op=ALU.mult),
                             reads=[gen, pGn], writes=[f"gT{fc}"])
                for dg in range(4):
                    for k4 in range(NFC // 4):
                        wd_, wdn = wd[dl % 2], f"wd{dl % 2}"
                        dl += 1
                        p.op("pool", lambda e, wd_=wd_, k4=k4, dg=dg: e.dma_start(out=wd_[:, :, :], in_=wdn_v[:, k4 * 4:(k4 + 1) * 4, dg * 512:(dg + 1) * 512]),
                             writes=[wdn], dma=True)
                        for kk in range(4):
                            k = k4 * 4 + kk
                            for st_ in range(4):
                                p.op("pe", lambda e, wd_=wd_, kk=kk, k=k, st_=st_: e.matmul(
                                    psA[st_][:, :], lhsT=gT[:, k, st_ * 128:(st_ + 1) * 128], rhs=wd_[:, kk, :], start=(k == 0), stop=(k == NFC - 1)),
                                    reads=[wdn, f"gT{k}"], writes=[f"psAcc{st_}"] if k == 0 else (), appends=[f"psAcc{st_}"] if k > 0 else ())
                    for st_ in range(4):
                        p.op("dve", lambda e, st_=st_, dg=dg: e.scalar_tensor_tensor(
                            out=pre2[:, st_, dg * 512:(dg + 1) * 512], in0=pre2[:, st_, dg * 512:(dg + 1) * 512], scalar=ALPHA, in1=psA[st_][:, :], op0=ALU.mult, op1=ALU.add),
                            reads=[f"psAcc{st_}", "pre2"], writes=["pre2"])
                if debug and ti == 0:
                    for fc in range(8):
                        p.op("sp", lambda e, fc=fc: e.dma_start(out=uT_s[fc * 128:(fc + 1) * 128, 0:512], in_=gT[:, fc, :]),
                             reads=[f"gT{fc}"], writes=[f"dbg_g{fc}"], dma=True)
                for st_ in range(4):
                    if not debug:
                        layer_norm(pre2[:, st_, :], "pre2", gam5, bet5, ["gam5", "bet5"], stt5, "stt5", lnj5)
                    p.op("sp", lambda e, st_=st_, tk0=tk0: e.dma_start(out=out_d[tk0 + st_ * 128:tk0 + (st_ + 1) * 128, :], in_=pre2[:, st_, :]),
                         reads=["pre2"], writes=["out"] if (ti == 0 and st_ == 0) else (), appends=["out"] if not (ti == 0 and st_ == 0) else (), dma=True)
            p.wait_all("sp", ["out"])
        p.emit()
    return nc


def _consts():
    ident = np.eye(128, dtype=np.float32)
    pp = np.arange(128)[:, None]
    jj = np.arange(128)[None, :]
    causal = np.where(jj <= pp, 0.0, -1.0e30).astype(np.float32)
    slopes = np.exp2(-8.0 * np.arange(1, 9, dtype=np.float64) / 8.0)
    dd = np.arange(16, dtype=np.float64)
    alibi = (slopes[None, :, None] * (np.arange(128, dtype=np.float64)[:, None, None] - 128.0 * dd[None, None, :])).astype(np.float32)
    iota = np.ascontiguousarray(np.broadcast_to(np.arange(T, dtype=np.float32)[None, :], (128, T)))
    return ident, causal, np.ascontiguousarray(alibi.reshape(128, 128)), iota


def _shared_inputs(inp):
    f32 = np.float32
    G, P, HG = 64, 64, 16
    g = np.arange(G)

    def state_major(a):
        return np.ascontiguousarray(a.reshape(32, 2, P).transpose(1, 2, 0).reshape(128, 32))
    a_re = state_major(inp["ssm_a_re"][0])
    a_im = state_major(inp["ssm_a_im"][0])
    ldt = state_major(np.repeat(inp["ssm_log_dt"][0][:, None], P, axis=1))
    ssm_prm = np.ascontiguousarray(np.concatenate([a_re, a_im, ldt], axis=1).astype(f32))

    def bpad(b):
        o = np.zeros((128, 32, 128), f32)
        for gi in range(G):
            q, g2 = gi // 2, gi % 2
            r0 = (q % 4) * 32 + g2 * 16
            o[r0:r0 + 16, q, g2 * 64:(g2 + 1) * 64] = b[gi].T
        return o.reshape(128, 32 * 128)

    def cpad(c):
        o = np.zeros((128, 32, 128), f32)
        for gi in range(G):
            q, g2 = gi // 2, gi % 2
            c0 = (q % 4) * 32 + g2 * 16
            o[g2 * 64:(g2 + 1) * 64, q, c0:c0 + 16] = c[gi].T
        return o.reshape(128, 32 * 128)

    colp = np.concatenate([inp["ssm_d"][0].reshape(8, 128).T, inp["b_glu"][0].reshape(8, 128).T], axis=1).astype(f32)
    cw = inp["conv_w"][0]
    cb = inp["conv_b"][0]
    convp = np.stack([cw[0], cw[1], cw[2], cb], axis=1).reshape(NFC, 128, 4).transpose(1, 0, 2).reshape(128, NFC * 4)
    ln = np.stack([np.broadcast_to(inp[k][0][None, :], (128, D)) for k in ("ln1_g", "ln1_b", "ln2_g", "ln2_b")], axis=0)
    ident, causal, alibi, iota = _consts()
    return {
        "w_in": np.ascontiguousarray(inp["w_in"][0]),
        "w_glu": np.ascontiguousarray(inp["w_glu"][0]),
        "w_out": np.ascontiguousarray(inp["w_out"][0]),
        "w_up": np.ascontiguousarray(inp["w_up"][0]),
        "w_gate": np.ascontiguousarray(inp["w_gate"][0]),
        "w_down": np.ascontiguousarray(inp["w_down"][0]),
        "ssm_prm": ssm_prm,
        "bpad_re": bpad(inp["ssm_b_re"][0]), "bpad_im": bpad(inp["ssm_b_im"][0]),
        "cpad_re": cpad(inp["ssm_c_re"][0]), "cpad_im": cpad(inp["ssm_c_im"][0]),
        "colp": np.ascontiguousarray(colp),
        "convp": np.ascontiguousarray(convp.astype(f32)),
        "ln": np.ascontiguousarray(ln.astype(f32)),
        "ident": ident, "causal": causal, "alibi": alibi, "iota": iota,
    }


def kernel(**inputs):
    inp = {k: np.asarray(v, dtype=np.float32) for k, v in inputs.items()}
    x = inp["x"]
    nb = x.shape[0]
    shared = _shared_inputs(inp)
    in_maps = []
    for b in range(nb):
        m = dict(shared)
        m["x"] = np.ascontiguousarray(x[b])
        m["xT"] = np.ascontiguousarray(x[b].T)
        in_maps.append(m)
    nc = build_program()
    res = run_bass_kernel_spmd(nc, in_maps, core_ids=list(range(nb)))
    out = np.stack([np.asarray(r["out"], dtype=np.float32) for r in res.results], axis=0)
    return out
```

```python
import math
from contextlib import ExitStack

import numpy as np
import concourse.bass as bass
import concourse.mybir as mybir
from concourse.bass_utils import run_bass_kernel_spmd

F32 = mybir.dt.float32
BF16 = mybir.dt.bfloat16
ALU = mybir.AluOpType
AF = mybir.ActivationFunctionType
AX = mybir.AxisListType

T = 2048
D = 2048
NIN = 3664
FF = 5632
NFC = FF // 128
ALPHA = 2.0 ** 0.25
EPS = 1e-5
MAGIC = 12582912.0
TWO_PI_S = 2.0 * math.pi * (1.0 - 1e-6)
NBIS = 16
TOPK = 256

ENGS = ("pe", "act", "dve", "pool", "sp")


class Op:
    __slots__ = ("eng", "fn", "deps", "is_dma", "signal", "count", "sem", "wres", "nop")

    def __init__(self, eng, fn, is_dma):
        self.eng = eng
        self.fn = fn
        self.is_dma = is_dma
        self.deps = []
        self.signal = False
        self.count = 0
        self.sem = None
        self.wres = None
        self.nop = False


class Prog:
    def __init__(self, nc, stack):
        self.nc = nc
        self.stack = stack
        self.ops = {e: [] for e in ENGS}
        self.res = {}
        self.dma_res = {}
        self.all_ops = []
        self.since_bar_dma = []

    def _st(self, r):
        s = self.res.get(r)
        if s is None:
            s = {"w": [], "r": []}
            self.res[r] = s
        return s

    def op(self, eng, fn, reads=(), writes=(), appends=(), dma=False):
        o = Op(eng, fn, dma)
        deps = []
        for r in reads:
            deps.extend(self._st(r)["w"])
        for r in writes:
            s = self._st(r)
            deps.extend(s["w"])
            deps.extend(s["r"])
        for r in appends:
            deps.extend(self._st(r)["r"])
        for r in reads:
            self._st(r)["r"].append(o)
        for r in writes:
            s = self._st(r)
            s["w"] = [o]
            s["r"] = []
        for r in appends:
            self._st(r)["w"].append(o)
        if dma:
            wr = list(writes) + list(appends)
            assert len(wr) == 1, "a DMA must write exactly one resource"
            o.wres = wr[0]
            self.since_bar_dma.append(o)
        seen = set()
        for d in deps:
            if d is o or id(d) in seen:
                continue
            seen.add(id(d))
            if (not d.is_dma) and (not dma) and d.eng == eng == "pe":
                continue
            o.deps.append(d)
        self.ops[eng].append(o)
        self.all_ops.append(o)
        return o

    def wait_all(self, eng, reads):
        o = self.op(eng, (lambda e: None), reads=reads)
        o.nop = True
        return o

    def barrier(self):
        deps = []
        for e in ENGS:
            for o in reversed(self.ops[e]):
                if not o.is_dma and not o.nop:
                    deps.append(o)
                    break
        deps.extend(self.since_bar_dma)
        self.since_bar_dma = []
        self.res = {}
        for e in ENGS:
            o = Op(e, (lambda eng: None), False)
            o.nop = True
            o.deps = [d for d in deps]
            self.ops[e].append(o)
            self.all_ops.append(o)

    def emit(self):
        nc = self.nc
        for o in self.all_ops:
            for d in o.deps:
                d.signal = True
        esem = {e: self.stack.enter_context(nc.semaphore("s_" + e)) for e in ENGS}
        cnt = {e: 0 for e in ENGS}
        for o in self.all_ops:
            if o.is_dma:
                ent = self.dma_res.get(o.wres)
                if ent is None:
                    ent = [self.stack.enter_context(nc.semaphore("d_" + o.wres)), 0]
                    self.dma_res[o.wres] = ent
                ent[1] += 16
                o.sem = ent[0]
                o.count = ent[1]
            else:
                if o.signal and not o.nop:
                    cnt[o.eng] += 1
                o.sem = esem[o.eng]
                o.count = cnt[o.eng]

        def run(eng_name):
            def body(eng):
                waited = {}
                for o in self.ops[eng_name]:
                    need = {}
                    for d in o.deps:
                        k = id(d.sem)
                        if k not in need or need[k][1] < d.count:
                            need[k] = (d.sem, d.count)
                    for k, (sem, val) in need.items():
                        if waited.get(k, 0) >= val:
                            continue
                        eng.wait_ge(sem, val)
                        waited[k] = val
                    inst = o.fn(eng)
                    if inst is None:
                        continue
                    if o.is_dma:
                        inst.then_inc(o.sem, 16)
                    elif o.signal and not o.nop:
                        inst.then_inc(o.sem, 1)
            return body

        with nc.Block() as block:
            block.tensor(run("pe"))
            block.scalar(run("act"))
            block.vector(run("dve"))
            block.gpsimd(run("pool"))
            block.sync(run("sp"))


class Phase:
    def __init__(self, nc):
        self.nc = nc
        self.st = ExitStack()

    def __enter__(self):
        self.st.__enter__()
        return self

    def __exit__(self, *a):
        return self.st.__exit__(*a)

    def sb(self, name, shape, dt):
        return self.st.enter_context(self.nc.sbuf_tensor("sb_" + name, list(shape), dt))

    def ps(self, name, shape, dt=F32):
        return self.st.enter_context(self.nc.psum_tensor("ps_" + name, list(shape), dt))


def build_program(debug=False, stop_after=99, skip=()):
    nc = bass.Bass("TRN2", target_bir_lowering=False)

    def din(name, shape):
        return nc.dram_tensor(name, list(shape), F32, kind="ExternalInput").ap()

    xT_d = din("xT", [D, T])
    x_d = din("x", [T, D])
    w_in_d = din("w_in", [D, NIN])
    w_glu_d = din("w_glu", [1024, 1024])
    w_out_d = din("w_out", [D, D])
    w_up_d = din("w_up", [D, FF])
    w_gate_d = din("w_gate", [D, FF])
    w_down_d = din("w_down", [FF, D])
    ssm_prm_d = din("ssm_prm", [128, 3 * 32])
    bpad_re_d = din("bpad_re", [128, 32 * 128])
    bpad_im_d = din("bpad_im", [128, 32 * 128])
    cpad_re_d = din("cpad_re", [128, 32 * 128])
    cpad_im_d = din("cpad_im", [128, 32 * 128])
    colp_d = din("colp", [128, 16])
    convp_d = din("convp", [128, NFC * 4])
    ln_d = din("ln", [4, 128, D])
    ident_d = din("ident", [128, 128])
    causal_d = din("causal", [128, 128])
    alibi_d = din("alibi", [128, 8 * 16])
    iota_d = din("iota", [128, T])

    out_d = nc.dram_tensor("out", [T, D], F32, kind="ExternalOutput").ap()
    skind = "ExternalOutput" if debug else "Internal"
    uT_s = nc.dram_tensor("s_uT", [1024, T], BF16, kind=skind).ap()
    yT_s = nc.dram_tensor("s_yT", [1024, T], BF16, kind=skind).ap()
    ycatT_s = nc.dram_tensor("s_ycatT", [2048, T], BF16, kind=skind).ap()
    h1_s = nc.dram_tensor("s_h1", [T, D], F32, kind=skind).ap()
    h1T_s = nc.dram_tensor("s_h1T", [D, T], BF16, kind=skind).ap()

    with ExitStack() as top:
        p = Prog(nc, top)
        G = Phase(nc)
        top.enter_context(G)
        ident_bf = G.sb("ident_bf", [128, 128], BF16)
        ident_f = G.sb("ident_f", [128, 128], F32)
        ones_bf = G.sb("ones_bf", [128, 128], BF16)
        p.op("pool", lambda e: e.dma_start(out=ident_bf[:], in_=ident_d), writes=["ident_bf"], dma=True)
        p.op("sp", lambda e: e.dma_start(out=ident_f[:], in_=ident_d), writes=["ident_f"], dma=True)
        p.op("dve", lambda e: e.memset(ones_bf[:], 1.0), writes=["ones_bf"])

        w_in_v = w_in_d.rearrange("(c p) n -> p c n", p=128)

        with Phase(nc) as A:
            QT = A.sb("QT", [128, 8, T], BF16)
            KT = A.sb("KT", [128, 2, T], BF16)
            qiT = A.sb("qiT", [128, 8, T], BF16)
            kiT2 = A.sb("kiT2", [128, T], BF16)
            Vt = A.sb("Vt", [128, 16, 256], BF16)
            wi_t = A.sb("wi_t", [128, 16, 16], F32)

            with Phase(nc) as P1:
                xTb = P1.sb("xTb", [128, 16, T], BF16)
                wb = [P1.sb(f"wb{i}", [128, 16, 256], BF16) for i in range(3)]
                wv = P1.sb("wv", [128, 16, 256], BF16)
                wvi = P1.sb("wvi", [128, 16, 80], BF16)
                ust = [P1.sb(f"ust{i}", [128, T], BF16) for i in range(2)]
                psm = [P1.ps(f"p1ps{i}", [128, T], F32) for i in range(2)]

                for c in range(16):
                    p.op("pool", lambda e, c=c: e.dma_start(out=xTb[:, c, :], in_=xT_d[c * 128:(c + 1) * 128, :]),
                         appends=["xTb"], dma=True)

                jobs = []
                for i in range(4):
                    jobs.append((i * 256, [("u", 2 * i), ("u", 2 * i + 1)]))
                for i in range(4):
                    jobs.append((1024 + i * 256, [("q", 2 * i), ("q", 2 * i + 1)]))
                jobs.append((2048, [("k", 0), ("k", 1)]))
                for i in range(4):
                    jobs.append((2560 + i * 256, [("qi", 2 * i), ("qi", 2 * i + 1)]))
                jobs.append((None, [("ki", 0)]))

                tile_no = 0
                for jn, (col0, dests) in enumerate(jobs):
                    if stop_after < 1 and jn >= int(stop_after * 10):
                        break
                    if col0 is None and "ki" in skip:
                        continue
                    wbuf = wb[jn % 3]
                    wname = f"wb{jn % 3}"
                    if col0 is None:
                        p.op("pool", lambda e, wbuf=wbuf: e.dma_start(out=wbuf[:, :, 0:64], in_=w_in_v[:, :, 3584:3648]),
                             writes=[wname], dma=True)
                        p.op("pool", lambda e, wbuf=wbuf: e.dma_start(out=wbuf[:, :, 64:128], in_=w_in_v[:, :, 3584:3648]),
                             appends=[wname], dma=True)
                    else:
                        p.op("pool", lambda e, wbuf=wbuf, col0=col0: e.dma_start(out=wbuf[:, :, :], in_=w_in_v[:, :, col0:col0 + 256]),
                             writes=[wname], dma=True)
                    for sub, (kind, idx) in enumerate(dests):
                        ps = psm[tile_no % 2]
                        psn = [f"p1ps{tile_no % 2}_{n}" for n in range(4)]
                        for kc in range(16):
                            for n in range(4):
                                p.op("pe", lambda e, ps=ps, wbuf=wbuf, kc=kc, n=n, sub=sub: e.matmul(
                                    ps[:, n * 512:(n + 1) * 512], lhsT=wbuf[:, kc, sub * 128:(sub + 1) * 128],
                                    rhs=xTb[:, kc, n * 512:(n + 1) * 512], start=(kc == 0), stop=(kc == 15)),
                                    reads=[wname, "xTb"],
                                    writes=[psn[n]] if kc == 0 else (), appends=[psn[n]] if kc > 0 else ())
                        if kind == "u":
                            dst = ust[idx % 2]
                            dname = f"ust{idx % 2}"
                            dst_ap = dst[:, :]
                        elif kind == "q":
                            dst_ap, dname = QT[:, idx, :], f"QT{idx}"
                        elif kind == "k":
                            dst_ap, dname = KT[:, idx, :], f"KT{idx}"
                        elif kind == "qi":
                            dst_ap, dname = qiT[:, idx, :], f"qiT{idx}"
                        else:
                            dst_ap, dname = kiT2[:, :], "kiT2"
                        if tile_no % 2 == 0:
                            p.op("act", lambda e, ps=ps, dst_ap=dst_ap: e.activation(out=dst_ap, in_=ps[:, :], func=AF.Identity),
                                 reads=psn, writes=[dname])
                        else:
                            p.op("dve", lambda e, ps=ps, dst_ap=dst_ap: e.tensor_copy(out=dst_ap, in_=ps[:, :]),
                                 reads=psn, writes=[dname])
                        if kind == "u":
                            p.op("sp", lambda e, dst=dst, idx=idx: e.dma_start(out=uT_s[idx * 128:(idx + 1) * 128, :], in_=dst[:, :]),
                                 reads=[dname], writes=[f"s_uT{idx}"], dma=True)
                        tile_no += 1

                p.op("pool", lambda e: e.dma_start(out=wv[:, :, 0:256], in_=w_in_v[:, :, 2304:2560]), writes=["wv"], dma=True)
                p.op("pool", lambda e: e.dma_start(out=wvi[:, :, :], in_=w_in_v[:, :, 3584:3664]), writes=["wvi"], dma=True)
                for tt in range(16 if (stop_after >= 1 and 'tm' not in skip) else 0):
                    ps = psm[tt % 2]
                    pn = f"p1ps{tt % 2}_0"
                    pn1 = f"p1ps{tt % 2}_1"
                    for kc in range(16):
                        p.op("pe", lambda e, ps=ps, kc=kc, tt=tt: e.matmul(
                            ps[:, 0:256], lhsT=xTb[:, kc, tt * 128:(tt + 1) * 128], rhs=wv[:, kc, 0:256],
                            start=(kc == 0), stop=(kc == 15)),
                            reads=["wv", "xTb"], writes=[pn] if kc == 0 else (), appends=[pn] if kc > 0 else ())
                    for kc in range(16):
                        p.op("pe", lambda e, ps=ps, kc=kc, tt=tt: e.matmul(
                            ps[:, 512:528], lhsT=xTb[:, kc, tt * 128:(tt + 1) * 128], rhs=wvi[:, kc, 64:80],
                            start=(kc == 0), stop=(kc == 15)),
                            reads=["wvi", "xTb"], writes=[pn1] if kc == 0 else (), appends=[pn1] if kc > 0 else ())
                    p.op("act", lambda e, ps=ps, tt=tt: e.activation(out=Vt[:, tt, :], in_=ps[:, 0:256], func=AF.Identity),
                         reads=[pn], writes=[f"Vt{tt}"])
                    p.op("dve", lambda e, ps=ps, tt=tt: e.tensor_copy(out=wi_t[:, tt, :], in_=ps[:, 512:528]),
                         reads=[pn1], writes=[f"wi{tt}"])
            p.barrier()
            if stop_after <= 1:
                p.wait_all("sp", [])
                p.emit()
                return nc

            with Phase(nc) as P2:
                causal = P2.sb("causal", [128, 128], F32)
                alibi = P2.sb("alibi", [128, 8, 16], F32)
                sc = [P2.sb(f"sc{i}", [128, T], F32) for i in range(2)]
                rl = [P2.sb(f"rl{i}", [128, 512], F32) for i in range(3)]
                junk = P2.sb("junk", [128, T], BF16)
                mask = P2.sb("mask", [128, T], BF16)
                maskT = [P2.sb(f"maskT{i}", [128, 16, 128], BF16) for i in range(2)]
                PTb = [P2.sb(f"PT{i}", [128, 4, 128], BF16) for i in range(2)]
                PTm = [P2.sb(f"PTm{i}", [128, 4, 128], BF16) for i in range(2)]
                rden = P2.sb("rden", [128, 512], F32)
                yst = [P2.sb(f"yst{i}", [128, 4, 128], BF16) for i in range(2)]
                bs = P2.sb("bs", [128, 16], F32)
                psI = [P2.ps(f"psI{i}", [128, 512], F32) for i in range(3)]
                psS = [P2.ps(f"psS{i}", [128, 512], F32) for i in range(2)]
                psO = P2.ps("psO", [128, 512], F32)
                psD = P2.ps("psD", [128, 512], F32)
                psT = P2.ps("psT", [128, 1024], BF16)

                p.op("sp", lambda e: e.dma_start(out=causal[:], in_=causal_d), writes=["causal"], dma=True)
                p.op("sp", lambda e: e.dma_start(out=alibi[:].rearrange("p h d -> p (h d)"), in_=alibi_d), writes=["alibi"], dma=True)

                HI, LO, RW, NLO, MID, CNT, TTS, THR = range(8)
                rot = {"ii": 0, "ri": 0, "si": 0}

                def gen_front(j):
                    nk = 128 * (j + 1)
                    nch = (nk + 511) // 512
                    scj = sc[j % 2]
                    scn = f"sc{j % 2}"
                    q0 = j * 128
                    mT = maskT[j % 2]
                    mTn = f"maskT{j % 2}"
                    for h in range(16):
                        m, half = h // 2, h % 2
                        pl, ph = half * 64, half * 64 + 64
                        for n in range(nch):
                            c0 = n * 512
                            cw = min(512, nk - c0)
                            pI = psI[rot["ii"] % 3]
                            pIn = f"psI{rot['ii'] % 3}"
                            rot["ii"] += 1
                            r_ = rl[rot["ri"] % 3]
                            rn = f"rl{rot['ri'] % 3}"
                            rot["ri"] += 1
                            p.op("pe", lambda e, pI=pI, m=m, pl=pl, ph=ph, q0=q0, c0=c0, cw=cw: e.matmul(
                                pI[:, 0:cw], lhsT=qiT[pl:ph, m, q0:q0 + 128], rhs=kiT2[pl:ph, c0:c0 + cw],
                                start=True, stop=True), reads=[f"qiT{m}", "kiT2"], writes=[pIn])
                            p.op("act", lambda e, pI=pI, r_=r_, cw=cw: e.activation(out=r_[:, 0:cw], in_=pI[:, 0:cw], func=AF.Relu),
                                 reads=[pIn], writes=[rn])
                            if h == 0:
                                p.op("dve", lambda e, r_=r_, scj=scj, c0=c0, cw=cw, j=j, h=h: e.tensor_scalar(
                                    out=scj[:, c0:c0 + cw], in0=r_[:, 0:cw], scalar1=wi_t[:, j, h:h + 1], scalar2=None, op0=ALU.mult),
                                    reads=[rn, f"wi{j}"], writes=[f"{scn}_{n}"])
                            else:
                                p.op("dve", lambda e, r_=r_, scj=scj, c0=c0, cw=cw, j=j, h=h: e.scalar_tensor_tensor(
                                    out=scj[:, c0:c0 + cw], in0=r_[:, 0:cw], scalar=wi_t[:, j, h:h + 1], in1=scj[:, c0:c0 + cw],
                                    op0=ALU.mult, op1=ALU.add),
                                    reads=[rn, f"wi{j}", f"{scn}_{n}"], writes=[f"{scn}_{n}"])
                            yield
                    scall = [f"{scn}_{n}" for n in range(nch)]
                    p.op("dve", lambda e, scj=scj, q0=q0: e.tensor_tensor(out=scj[:, q0:q0 + 128], in0=scj[:, q0:q0 + 128], in1=causal[:, :], op=ALU.add),
                         reads=scall + ["causal"], writes=scall)
                    yield "BIS"
                    if j < 2:
                        p.op("dve", lambda e: e.memset(bs[:, THR:THR + 1], -1.0e29), writes=["bs"])
                    else:
                        p.op("dve", lambda e, scj=scj, nk=nk: e.tensor_reduce(out=bs[:, HI:HI + 1], in_=scj[:, 0:nk], axis=AX.X, op=ALU.max),
                             reads=scall + ["bs"], writes=["bs"])
                        p.op("dve", lambda e, scj=scj, q0=q0: e.tensor_reduce(out=bs[:, LO:LO + 1], in_=scj[:, 0:q0], axis=AX.X, op=ALU.min),
                             reads=scall + ["bs"], writes=["bs"])
                        yield
                        p.op("dve", lambda e: e.tensor_tensor(out=bs[:, RW:RW + 1], in0=bs[:, HI:HI + 1], in1=bs[:, LO:LO + 1], op=ALU.subtract),
                             reads=["bs"], writes=["bs"])
                        p.op("dve", lambda e: e.reciprocal(out=bs[:, RW:RW + 1], in_=bs[:, RW:RW + 1]), reads=["bs"], writes=["bs"])
                        p.op("dve", lambda e: e.scalar_tensor_tensor(out=bs[:, NLO:NLO + 1], in0=bs[:, LO:LO + 1], scalar=-1.0, in1=bs[:, RW:RW + 1],
                                                                    op0=ALU.mult, op1=ALU.mult), reads=["bs"], writes=["bs"])
                        yield
                        p.op("dve", lambda e, scj=scj, nk=nk: e.tensor_scalar(out=scj[:, 0:nk], in0=scj[:, 0:nk], scalar1=bs[:, RW:RW + 1], scalar2=bs[:, NLO:NLO + 1],
                                                                       op0=ALU.mult, op1=ALU.add), reads=scall + ["bs"], writes=scall)
                        p.op("dve", lambda e: e.memset(bs[:, MID:MID + 1], 0.5), reads=["bs"], writes=["bs"])
                        yield
                        for it in range(NBIS):
                            p.op("dve", lambda e, scj=scj, nk=nk: e.tensor_scalar(
                                out=junk[:, 0:nk], in0=scj[:, 0:nk], scalar1=bs[:, MID:MID + 1], scalar2=0.0,
                                op0=ALU.is_ge, op1=ALU.add, accum_out=bs[:, CNT:CNT + 1]),
                                reads=scall + ["bs"], writes=["bs", "junk"])
                            yield
                            p.op("dve", lambda e, it=it: e.tensor_scalar(
                                out=bs[:, TTS:TTS + 1], in0=bs[:, CNT:CNT + 1], scalar1=TOPK - 0.5, scalar2=0.5 ** (it + 1),
                                op0=ALU.is_ge, op1=ALU.mult), reads=["bs"], writes=["bs"])
                            yield
                            p.op("dve", lambda e, it=it: e.scalar_tensor_tensor(
                                out=bs[:, MID:MID + 1], in0=bs[:, TTS:TTS + 1], scalar=-(0.5 ** (it + 2)), in1=bs[:, MID:MID + 1],
                                op0=ALU.add, op1=ALU.add), reads=["bs"], writes=["bs"])
                            yield
                        p.op("dve", lambda e: e.tensor_scalar(out=bs[:, THR:THR + 1], in0=bs[:, MID:MID + 1], scalar1=-(0.5 ** (NBIS + 1)), scalar2=None, op0=ALU.add),
                             reads=["bs"], writes=["bs"])
                    p.op("dve", lambda e, scj=scj, nk=nk: e.tensor_scalar(
                        out=mask[:, 0:nk], in0=scj[:, 0:nk], scalar1=bs[:, THR:THR + 1], scalar2=None, op0=ALU.is_ge),
                        reads=scall + ["bs"], writes=["mask"])
                    yield
                    for g0 in range(0, j + 1, 8):
                        g1 = min(j + 1, g0 + 8)
                        for i in range(g0, g1):
                            p.op("pe", lambda e, i=i, g0=g0: e.transpose(psT[:, (i - g0) * 128:(i - g0 + 1) * 128], mask[:, i * 128:(i + 1) * 128], ident_bf[:]),
                                 reads=["mask", "ident_bf"], writes=["psT"] if i == g0 else (), appends=["psT"] if i > g0 else ())
                        p.op("act", lambda e, g0=g0, g1=g1, mT=mT: e.activation(
                            out=mT[:, g0:g1, :], in_=psT[:, 0:(g1 - g0) * 128].rearrange("p (g t) -> p g t", t=128), func=AF.Identity),
                            reads=["psT"], writes=[mTn] if g0 == 0 else (), appends=[mTn] if g0 > 0 else ())
                        yield

                def gen_attn(j):
                    q0 = j * 128
                    mT = maskT[j % 2]
                    mTn = f"maskT{j % 2}"
                    steps = [(c, i) for c in range(2) for i in range(j + 1)]
                    par = []

                    def emit_st(k):
                        c, i = steps[k]
                        k_ = rot["si"] % 2
                        rot["si"] += 1
                        par.append(k_)
                        pS, pSn = psS[k_], f"psS{k_}"
                        for hh in range(4):
                            p.op("pe", lambda e, pS=pS, c=c, i=i, hh=hh, q0=q0: e.matmul(
                                pS[:, hh * 128:(hh + 1) * 128], lhsT=KT[:, c, i * 128:(i + 1) * 128],
                                rhs=QT[:, 4 * c + hh, q0:q0 + 128], start=True, stop=True),
                                reads=[f"KT{c}", f"QT{4 * c + hh}"], writes=[pSn] if hh == 0 else (), appends=[pSn] if hh > 0 else ())

                    emit_st(0)
                    for k, (c, i) in enumerate(steps):
                        if k + 1 < len(steps):
                            emit_st(k + 1)
                        k_ = par[k]
                        pS, pSn = psS[k_], f"psS{k_}"
                        PT_, PTn = PTb[k_], f"PT{k_}"
                        PM_, PMn = PTm[k_], f"PTm{k_}"
                        for hh in range(4):
                            p.op("act", lambda e, pS=pS, PT_=PT_, c=c, i=i, hh=hh, j=j: e.activation(
                                out=PT_[:, hh, :], in_=pS[:, hh * 128:(hh + 1) * 128], func=AF.Exp,
                                bias=alibi[:, 4 * c + hh, (j - i):(j - i) + 1], scale=128.0 ** -0.5),
                                reads=[pSn, "alibi"], writes=[PTn] if hh == 0 else (), appends=[PTn] if hh > 0 else ())
                        p.op("dve", lambda e, PT_=PT_, PM_=PM_, i=i, mT=mT: e.tensor_tensor(
                            out=PM_[:, :, :], in0=PT_[:, :, :], in1=mT[:, i:i + 1, :].to_broadcast([128, 4, 128]), op=ALU.mult),
                            reads=[PTn, mTn], writes=[PMn])
                        p.op("pe", lambda e, PM_=PM_, c=c, i=i, j=j: e.matmul(
                            psO[:, :], lhsT=Vt[:, i, c * 128:(c + 1) * 128], rhs=PM_[:, :, :].rearrange("p h t -> p (h t)"),
                            start=(i == 0), stop=(i == j)),
                            reads=[PMn, f"Vt{i}"], writes=["psO"] if i == 0 else (), appends=["psO"] if i > 0 else ())
                        p.op("pe", lambda e, PM_=PM_, i=i, j=j: e.matmul(
                            psD[:, :], lhsT=ones_bf[:, :], rhs=PM_[:, :, :].rearrange("p h t -> p (h t)"),
                            start=(i == 0), stop=(i == j)),
                            reads=[PMn, "ones_bf"], writes=["psD"] if i == 0 else (), appends=["psD"] if i > 0 else ())
                        if i == j:
                            ys = yst[c]
                            ysn = f"yst{c}"
                            p.op("dve", lambda e: e.reciprocal(out=rden[:, :], in_=psD[:, :]), reads=["psD"], writes=["rden"])
                            p.op("dve", lambda e, ys=ys: e.tensor_tensor(out=ys[:, :, :].rearrange("p h t -> p (h t)"), in0=psO[:, :], in1=rden[:, :], op=ALU.mult),
                                 reads=["psO", "rden"], writes=[ysn])
                            p.op("sp", lambda e, ys=ys, c=c, q0=q0: e.dma_start(
                                out=ycatT_s[1024 + c * 512:1024 + (c + 1) * 512, q0:q0 + 128].rearrange("(h p) t -> p h t", p=128), in_=ys[:, :, :]),
                                reads=[ysn], writes=[f"s_yat{c}"], dma=True)
                        yield

                for _ in gen_front(0):
                    pass
                for j in range(16):
                    fa = gen_attn(j)
                    ff = gen_front(j + 1) if j + 1 < 16 else None
                    if ff is not None:
                        for tok in ff:
                            if tok == "BIS":
                                break
                        else:
                            ff = None
                    gens = [fa] + ([ff] if ff is not None else [])
                    while gens:
                        for g_ in list(gens):
                            try:
                                next(g_)
                            except StopIteration:
                                gens.remove(g_)
        p.barrier()
        if stop_after <= 2:
            p.wait_all("sp", [])
            p.emit()
            return nc

        with Phase(nc) as P3:
            prm = P3.sb("prm", [128, 96], F32)
            sv = P3.sb("sv", [128, 16, 32], F32)
            Bre = P3.sb("Bre", [128, 32, 128], BF16)
            Bim = P3.sb("Bim", [128, 32, 128], BF16)
            CR = P3.sb("CR", [128, 32, 128], BF16)
            NCR = P3.sb("NCR", [128, 32, 128], BF16)
            NCI = P3.sb("NCI", [128, 32, 128], BF16)
            colp = P3.sb("colp", [128, 16], F32)
            diagD = P3.sb("diagD", [128, 8, 128], BF16)
            iot = P3.sb("iot", [128, T], F32)
            hpi = P3.sb("hpi", [128, 1], F32)
            p.op("dve", lambda e: e.memset(hpi[:], 0.5 * math.pi), writes=["prm"])
            p.op("sp", lambda e: e.dma_start(out=prm[:], in_=ssm_prm_d), appends=["prm"], dma=True)
            p.op("sp", lambda e: e.dma_start(out=colp[:], in_=colp_d), writes=["colp"], dma=True)
            p.op("sp", lambda e: e.dma_start(out=iot[:], in_=iota_d), writes=["iot"], dma=True)
            p.op("pool", lambda e: e.dma_start(out=Bre[:].rearrange("p a b -> p (a b)"), in_=bpad_re_d), writes=["Bre"], dma=True)
            p.op("pool", lambda e: e.dma_start(out=Bim[:].rearrange("p a b -> p (a b)"), in_=bpad_im_d), writes=["Bim"], dma=True)

            A_RE, A_IM, LDT = prm[:, 0:32], prm[:, 32:64], prm[:, 64:96]
            (DT, RR, TH, CO, SI, FRE, FIM, T0, T1, T2, T3, DEN) = range(12)

            def svv(k):
                return sv[:, k, :]

            def d1(fn, **kw):
                p.op("dve", fn, reads=["prm", "sv"], writes=["sv"])

            def a1(fn):
                p.op("act", fn, reads=["prm", "sv"], writes=["sv"])

            a1(lambda e: e.activation(out=svv(DT), in_=LDT, func=AF.Exp))
            d1(lambda e: e.tensor_tensor(out=svv(T0), in0=svv(DT), in1=A_RE, op=ALU.mult))
            a1(lambda e: e.activation(out=svv(RR), in_=svv(T0), func=AF.Exp))
            d1(lambda e: e.tensor_tensor(out=svv(TH), in0=svv(DT), in1=A_IM, op=ALU.mult))
            d1(lambda e: e.tensor_scalar(out=svv(TH), in0=svv(TH), scalar1=1.0 / (2.0 * math.pi), scalar2=None, op0=ALU.mult))
            d1(lambda e: e.tensor_scalar(out=svv(T0), in0=svv(TH), scalar1=MAGIC, scalar2=None, op0=ALU.add))
            d1(lambda e: e.tensor_scalar(out=svv(T0), in0=svv(T0), scalar1=-MAGIC, scalar2=None, op0=ALU.add))
            d1(lambda e: e.tensor_tensor(out=svv(T1), in0=svv(TH), in1=svv(T0), op=ALU.subtract))
            a1(lambda e: e.activation(out=svv(SI), in_=svv(T1), func=AF.Sin, scale=TWO_PI_S))
            a1(lambda e: e.activation(out=svv(T2), in_=svv(T1), func=AF.Abs))
            a1(lambda e: e.activation(out=svv(CO), in_=svv(T2), func=AF.Sin, bias=hpi[:, 0:1], scale=-TWO_PI_S))
            d1(lambda e: e.tensor_tensor(out=svv(T0), in0=svv(RR), in1=svv(CO), op=ALU.mult))
            d1(lambda e: e.tensor_scalar(out=svv(T0), in0=svv(T0), scalar1=-1.0, scalar2=None, op0=ALU.add))
            d1(lambda e: e.tensor_tensor(out=svv(T1), in0=svv(RR), in1=svv(SI), op=ALU.mult))
            d1(lambda e: e.tensor_tensor(out=svv(DEN), in0=A_RE, in1=A_RE, op=ALU.mult))
            d1(lambda e: e.tensor_tensor(out=svv(T2), in0=A_IM, in1=A_IM, op=ALU.mult))
            d1(lambda e: e.tensor_tensor(out=svv(DEN), in0=svv(DEN), in1=svv(T2), op=ALU.add))
            d1(lambda e: e.reciprocal(out=svv(DEN), in_=svv(DEN)))
            d1(lambda e: e.tensor_tensor(out=svv(T2), in0=svv(T0), in1=A_RE, op=ALU.mult))
            d1(lambda e: e.tensor_tensor(out=svv(T3), in0=svv(T1), in1=A_IM, op=ALU.mult))
            d1(lambda e: e.tensor_tensor(out=svv(T2), in0=svv(T2), in1=svv(T3), op=ALU.add))
            d1(lambda e: e.tensor_tensor(out=svv(FRE), in0=svv(T2), in1=svv(DEN), op=ALU.mult))
            d1(lambda e: e.tensor_tensor(out=svv(T2), in0=svv(T1), in1=A_RE, op=ALU.mult))
            d1(lambda e: e.tensor_tensor(out=svv(T3), in0=svv(T0), in1=A_IM, op=ALU.mult))
            d1(lambda e: e.tensor_tensor(out=svv(T2), in0=svv(T2), in1=svv(T3), op=ALU.subtract))
            d1(lambda e: e.tensor_tensor(out=svv(FIM), in0=svv(T2), in1=svv(DEN), op=ALU.mult))

            with Phase(nc) as P3c:
                cre = P3c.sb("cre", [128, 32, 128], F32)
                cim = P3c.sb("cim", [128, 32, 128], F32)
                ct1 = P3c.sb("ct1", [128, 32, 128], F32)
                ct2 = P3c.sb("ct2", [128, 32, 128], F32)
                p.op("sp", lambda e: e.dma_start(out=cre[:].rearrange("p a b -> p (a b)"), in_=cpad_re_d), writes=["cre"], dma=True)
                p.op("sp", lambda e: e.dma_start(out=cim[:].rearrange("p a b -> p (a b)"), in_=cpad_im_d), writes=["cim"], dma=True)

                def bc(k):
                    return sv[:, k, :].unsqueeze(2).to_broadcast([128, 32, 128])
                p.op("dve", lambda e: e.tensor_tensor(out=ct1[:], in0=cre[:], in1=bc(FRE), op=ALU.mult), reads=["cre", "sv"], writes=["ct1"])
                p.op("dve", lambda e: e.tensor_tensor(out=ct2[:], in0=cim[:], in1=bc(FIM), op=ALU.mult), reads=["cim", "sv"], writes=["ct2"])
                p.op("dve", lambda e: e.tensor_tensor(out=CR[:], in0=ct1[:], in1=ct2[:], op=ALU.subtract), reads=["ct1", "ct2"], writes=["CR"])
                p.op("dve", lambda e: e.tensor_tensor(out=NCR[:], in0=ct2[:], in1=ct1[:], op=ALU.subtract), reads=["ct1", "ct2"], writes=["NCR"])
                p.op("dve", lambda e: e.tensor_tensor(out=ct1[:], in0=cre[:], in1=bc(FIM), op=ALU.mult), reads=["cre", "sv", "CR", "NCR"], writes=["ct1"])
                p.op("dve", lambda e: e.tensor_tensor(out=ct2[:], in0=cim[:], in1=bc(FRE), op=ALU.mult), reads=["cim", "sv", "CR", "NCR"], writes=["ct2"])
                p.op("dve", lambda e: e.tensor_tensor(out=ct1[:], in0=ct1[:], in1=ct2[:], op=ALU.add), reads=["ct1", "ct2"], writes=["ct1"])
                p.op("dve", lambda e: e.tensor_scalar(out=NCI[:], in0=ct1[:], scalar1=-1.0, scalar2=None, op0=ALU.mult), reads=["ct1"], writes=["NCI"])
            for ct in range(8):
                p.op("dve", lambda e, ct=ct: e.tensor_scalar(out=diagD[:, ct, :], in0=ident_f[:, :], scalar1=colp[:, ct:ct + 1], scalar2=None, op0=ALU.mult),
                     reads=["ident_f", "colp"], appends=["diagD"])
            p.barrier()

            with Phase(nc) as P3m:
                uTt = [P3m.sb(f"uTt{i}", [128, T], BF16) for i in range(2)]
                angA = P3m.sb("angA", [128, T], F32)
                angB = P3m.sb("angB", [128, T], F32)
                negM = P3m.sb("negM", [128, 1], F32)
                cosb = [P3m.sb(f"cosb{i}", [128, T], BF16) for i in range(2)]
                sinb = [P3m.sb(f"sinb{i}", [128, T], BF16) for i in range(2)]
                breb = [P3m.sb(f"breb{i}", [128, T], BF16) for i in range(2)]
                bimb = [P3m.sb(f"bimb{i}", [128, T], BF16) for i in range(2)]
                tmb = [P3m.sb(f"tmb{i}", [128, T], BF16) for i in range(4)]
                xreb = P3m.sb("xreb", [128, T], BF16)
                ximb = P3m.sb("ximb", [128, T], BF16)
                greb = P3m.sb("greb", [128, T], BF16)
                gimb = P3m.sb("gimb", [128, T], BF16)
                PP = [P3m.sb(f"PP{i}", [128, T], BF16) for i in range(4)]
                ygs = [P3m.sb(f"ygs{i}", [128, T], BF16) for i in range(2)]
                psB = P3m.ps("psBu", [128, T], F32)
                psY = P3m.ps("psY", [128, T], F32)
                psYn = [f"psY{n}" for n in range(4)]
                p.op("dve", lambda e: e.memset(negM[:], -MAGIC), writes=["negM"])

                def tab_A(q):
                    p.op("dve", lambda e, q=q: e.tensor_scalar(out=angA[:, :], in0=iot[:, :], scalar1=sv[:, TH, q:q + 1], scalar2=MAGIC, op0=ALU.mult, op1=ALU.add),
                         reads=["iot", "sv"], writes=["angA"])

                def tab_B(q):
                    p.op("act", lambda e: e.activation(out=angB[:, :], in_=angA[:, :], func=AF.Identity, bias=negM[:, 0:1], scale=1.0),
                         reads=["angA", "negM"], writes=["angB"])

                def tab_C(q):
                    p.op("dve", lambda e, q=q: e.scalar_tensor_tensor(out=angA[:, :], in0=iot[:, :], scalar=sv[:, TH, q:q + 1], in1=angB[:, :], op0=ALU.mult, op1=ALU.subtract),
                         reads=["iot", "sv", "angB"], writes=["angA"])

                def tab_D(q):
                    cb, cbn = cosb[q % 2], f"cosb{q % 2}"
                    sb_, sbn = sinb[q % 2], f"sinb{q % 2}"
                    p.op("act", lambda e, sb_=sb_: e.activation(out=sb_[:, :], in_=angA[:, :], func=AF.Sin, scale=TWO_PI_S), reads=["angA"], writes=[sbn])
                    p.op("act", lambda e: e.activation(out=angB[:, :], in_=angA[:, :], func=AF.Abs), reads=["angA"], writes=["angB"])
                    p.op("act", lambda e, cb=cb: e.activation(out=cb[:, :], in_=angB[:, :], func=AF.Sin, bias=hpi[:, 0:1], scale=-TWO_PI_S), reads=["angB", "hpi"], writes=[cbn])

                def load_u(ct):
                    p.op("sp", lambda e, ct=ct: e.dma_start(out=uTt[ct % 2][:, :], in_=uT_s[ct * 128:(ct + 1) * 128, :]),
                         reads=[f"s_uT{ct}"], writes=[f"uTt{ct % 2}"], dma=True)

                def emit_dterm(ct):
                    ut, utn = uTt[ct % 2], f"uTt{ct % 2}"
                    for n in range(4):
                        p.op("pe", lambda e, ut=ut, ct=ct, n=n: e.matmul(psY[:, n * 512:(n + 1) * 512], lhsT=diagD[:, ct, :],
                                                                     rhs=ut[:, n * 512:(n + 1) * 512], start=True, stop=False),
                             reads=[utn, "diagD"], writes=[psYn[n]])

                def emit_bu(q):
                    ct = q // 4
                    ut, utn = uTt[ct % 2], f"uTt{ct % 2}"
                    br, brn = breb[q % 2], f"breb{q % 2}"
                    bi, bin_ = bimb[q % 2], f"bimb{q % 2}"
                    for hf in range(2):
                        t0 = hf * 1024
                        pbn = ["psB0", "psB1", "psB2", "psB3"]
                        for part, Bw, bwn in ((0, Bre, "Bre"), (1, Bim, "Bim")):
                            for n in range(2):
                                p.op("pe", lambda e, Bw=Bw, q=q, ut=ut, part=part, n=n, t0=t0: e.matmul(
                                    psB[:, part * 1024 + n * 512:part * 1024 + (n + 1) * 512], lhsT=Bw[:, q, :],
                                    rhs=ut[:, t0 + n * 512:t0 + (n + 1) * 512], start=True, stop=True),
                                    reads=[bwn, utn], writes=[pbn[part * 2 + n]])
                        sl = slice(t0, t0 + 1024)
                        p.op("act", lambda e, sl=sl, br=br: e.activation(out=br[:, sl], in_=psB[:, 0:1024], func=AF.Identity),
                             reads=["psB0", "psB1"], writes=[brn] if hf == 0 else (), appends=[brn] if hf else ())
                        p.op("act", lambda e, sl=sl, bi=bi: e.activation(out=bi[:, sl], in_=psB[:, 1024:2048], func=AF.Identity),
                             reads=["psB2", "psB3"], writes=[bin_] if hf == 0 else (), appends=[bin_] if hf else ())

                load_u(0)
                tab_A(0); tab_B(0); tab_C(0); tab_D(0)
                emit_bu(0)
                for q in range(32):
                    ct, pq = divmod(q, 4)
                    cb, cbn = cosb[q % 2], f"cosb{q % 2}"
                    sb_, sbn = sinb[q % 2], f"sinb{q % 2}"
                    br, brn = breb[q % 2], f"breb{q % 2}"
                    bi, bin_ = bimb[q % 2], f"bimb{q % 2}"
                    if pq == 0:
                        if ct + 1 < 8:
                            load_u(ct + 1)
                        emit_dterm(ct)
                    if q + 1 < 32:
                        tab_A(q + 1)
                        tab_B(q + 1)
                        emit_bu(q + 1)
                    p.op("dve", lambda e, cb=cb, br=br: e.tensor_tensor(out=tmb[0][:, :], in0=br[:, :], in1=cb[:, :], op=ALU.mult), reads=[brn, cbn], writes=["tmb0"])
                    p.op("dve", lambda e, sb_=sb_, bi=bi: e.tensor_tensor(out=tmb[1][:, :], in0=bi[:, :], in1=sb_[:, :], op=ALU.mult), reads=[bin_, sbn], writes=["tmb1"])
                    p.op("dve", lambda e, cb=cb, bi=bi: e.tensor_tensor(out=tmb[2][:, :], in0=bi[:, :], in1=cb[:, :], op=ALU.mult), reads=[bin_, cbn], writes=["tmb2"])
                    p.op("dve", lambda e, sb_=sb_, br=br: e.tensor_tensor(out=tmb[3][:, :], in0=br[:, :], in1=sb_[:, :], op=ALU.mult), reads=[brn, sbn], writes=["tmb3"])
                    p.op("dve", lambda e: e.tensor_tensor(out=xreb[:, :], in0=tmb[0][:, :], in1=tmb[1][:, :], op=ALU.add), reads=["tmb0", "tmb1"], writes=["xreb"])
                    p.op("dve", lambda e: e.tensor_tensor(out=ximb[:, :], in0=tmb[2][:, :], in1=tmb[3][:, :], op=ALU.subtract), reads=["tmb2", "tmb3"], writes=["ximb"])
                    if q + 1 < 32:
                        tab_C(q + 1)
                        tab_D(q + 1)
                    p.op("dve", lambda e, q=q: e.tensor_tensor_scan(out=greb[:, :], data0=sv[:, RR, q:q + 1].to_broadcast([128, T]), data1=xreb[:, :], initial=0.0, op0=ALU.mult, op1=ALU.add),
                         reads=["sv", "xreb"], writes=["greb"])
                    p.op("dve", lambda e, q=q: e.tensor_tensor_scan(out=gimb[:, :], data0=sv[:, RR, q:q + 1].to_broadcast([128, T]), data1=ximb[:, :], initial=0.0, op0=ALU.mult, op1=ALU.add),
                         reads=["sv", "ximb"], writes=["gimb"])
                    p.op("dve", lambda e, cb=cb: e.tensor_tensor(out=PP[0][:, :], in0=greb[:, :], in1=cb[:, :], op=ALU.mult), reads=["greb", cbn], writes=["PP0"])
                    p.op("dve", lambda e, sb_=sb_: e.tensor_tensor(out=PP[1][:, :], in0=gimb[:, :], in1=sb_[:, :], op=ALU.mult), reads=["gimb", sbn], writes=["PP1"])
                    p.op("dve", lambda e, sb_=sb_: e.tensor_tensor(out=PP[2][:, :], in0=greb[:, :], in1=sb_[:, :], op=ALU.mult), reads=["greb", sbn], writes=["PP2"])
                    p.op("dve", lambda e, cb=cb: e.tensor_tensor(out=PP[3][:, :], in0=gimb[:, :], in1=cb[:, :], op=ALU.mult), reads=["gimb", cbn], writes=["PP3"])
                    for k, (Cw, cwn) in enumerate(((CR, "CR"), (NCR, "NCR"), (NCI, "NCI"), (NCI, "NCI"))):
                        for n in range(4):
                            last = (pq == 3 and k == 3)
                            p.op("pe", lambda e, Cw=Cw, q=q, k=k, n=n, last=last: e.matmul(
                                psY[:, n * 512:(n + 1) * 512], lhsT=Cw[:, q, :], rhs=PP[k][:, n * 512:(n + 1) * 512],
                                start=False, stop=last), reads=[cwn, f"PP{k}"], appends=[psYn[n]])
                    if pq == 3:
                        yg = ygs[ct % 2]
                        ygn = f"ygs{ct % 2}"
                        p.op("act", lambda e, yg=yg: e.activation(out=yg[:, :], in_=psY[:, :], func=AF.Gelu), reads=psYn, writes=[ygn])
                        p.op("sp", lambda e, yg=yg, ct=ct: e.dma_start(out=yT_s[ct * 128:(ct + 1) * 128, :], in_=yg[:, :]),
                             reads=[ygn], writes=["s_yT"] if ct == 0 else (), appends=["s_yT"] if ct > 0 else (), dma=True)
        p.barrier()
        if stop_after <= 3:
            p.wait_all("sp", [])
            p.emit()
            return nc

        P34 = Phase(nc)
        P34.__enter__()
        wo = P34.sb("wo", [128, 16, D], BF16)
        for kc in range(16):
            p.op("pool", lambda e, kc=kc: e.dma_start(out=wo[:, kc, :], in_=w_out_d[kc * 128:(kc + 1) * 128, :]), appends=["wo"], dma=True)
        with Phase(nc) as P3b:
            yTs = P3b.sb("yTs", [128, 8, T], BF16)
            wgl = P3b.sb("wgl", [128, 8, 1024], BF16)
            colp2 = P3b.sb("colp2", [128, 16], F32)
            sg = [P3b.sb(f"sg{i}", [128, T], F32) for i in range(2)]
            yso = [P3b.sb(f"yso{i}", [128, T], BF16) for i in range(2)]
            psZ = [P3b.ps(f"psZ{i}", [128, T], F32) for i in range(2)]
            p.op("sp", lambda e: e.dma_start(out=colp2[:], in_=colp_d), writes=["colp2"], dma=True)
            p.op("sp", lambda e: e.dma_start(out=yTs[:, :, :], in_=yT_s.rearrange("(c p) t -> p c t", p=128)), writes=["yTs"], dma=True)
            p.op("pool", lambda e: e.dma_start(out=wgl[:, :, :], in_=w_glu_d.rearrange("(c p) n -> p c n", p=128)), writes=["wgl"], dma=True)
            for mo in range(8):
                pz = psZ[mo % 2]
                pzn = [f"psZ{mo % 2}_{n}" for n in range(4)]
                for kc in range(8):
                    for n in range(4):
                        p.op("pe", lambda e, pz=pz, mo=mo, kc=kc, n=n: e.matmul(
                            pz[:, n * 512:(n + 1) * 512], lhsT=wgl[:, kc, mo * 128:(mo + 1) * 128], rhs=yTs[:, kc, n * 512:(n + 1) * 512],
                            start=(kc == 0), stop=(kc == 7)), reads=["wgl", "yTs"],
                            writes=[pzn[n]] if kc == 0 else (), appends=[pzn[n]] if kc > 0 else ())
                s_ = sg[mo % 2]
                sn = f"sg{mo % 2}"
                yo = yso[mo % 2]
                yon = f"yso{mo % 2}"
                p.op("act", lambda e, pz=pz, s_=s_, mo=mo: e.activation(out=s_[:, :], in_=pz[:, :], func=AF.Sigmoid, bias=colp2[:, 8 + mo:9 + mo], scale=1.0),
                     reads=pzn + ["colp2"], writes=[sn])
                p.op("dve", lambda e, s_=s_, yo=yo, mo=mo: e.tensor_tensor(out=yo[:, :], in0=yTs[:, mo, :], in1=s_[:, :], op=ALU.mult),
                     reads=[sn, "yTs"], writes=[yon])
                p.op("sp", lambda e, yo=yo, mo=mo: e.dma_start(out=ycatT_s[mo * 128:(mo + 1) * 128, :], in_=yo[:, :]),
                     reads=[yon], writes=["s_yssm"] if mo == 0 else (), appends=["s_yssm"] if mo > 0 else (), dma=True)
        p.barrier()
        if stop_after <= 4:
            p.wait_all("sp", [])
            p.emit()
            return nc

        def layer_norm(pre_ap, prn, gam, bet, gnames, stt, sttn, lnj):
            p.op("dve", lambda e: e.memset(stt[:, 0:2], 0.0), writes=[sttn])
            p.op("act", lambda e: e.activation(out=lnj[:, :], in_=pre_ap, func=AF.Identity, accum_out=stt[:, 0:1]),
                 reads=[prn, sttn], writes=["lnj", sttn])
            p.op("act", lambda e: e.activation(out=lnj[:, :], in_=pre_ap, func=AF.Square, accum_out=stt[:, 1:2]),
                 reads=[prn, sttn], writes=["lnj", sttn])
            p.op("dve", lambda e: e.tensor_scalar(out=stt[:, 2:4], in0=stt[:, 0:2], scalar1=1.0 / D, scalar2=None, op0=ALU.mult), reads=[sttn], writes=[sttn])
            p.op("dve", lambda e: e.tensor_tensor(out=stt[:, 4:5], in0=stt[:, 2:3], in1=stt[:, 2:3], op=ALU.mult), reads=[sttn], writes=[sttn])
            p.op("dve", lambda e: e.tensor_tensor(out=stt[:, 5:6], in0=stt[:, 3:4], in1=stt[:, 4:5], op=ALU.subtract), reads=[sttn], writes=[sttn])
            p.op("dve", lambda e: e.tensor_scalar(out=stt[:, 5:6], in0=stt[:, 5:6], scalar1=EPS, scalar2=None, op0=ALU.add), reads=[sttn], writes=[sttn])
            p.op("act", lambda e: e.activation(out=stt[:, 7:8], in_=stt[:, 5:6], func=AF.Sqrt), reads=[sttn], writes=[sttn])
            p.op("dve", lambda e: e.reciprocal(out=stt[:, 5:6], in_=stt[:, 7:8]), reads=[sttn], writes=[sttn])
            p.op("dve", lambda e: e.scalar_tensor_tensor(out=stt[:, 6:7], in0=stt[:, 2:3], scalar=-1.0, in1=stt[:, 5:6], op0=ALU.mult, op1=ALU.mult), reads=[sttn], writes=[sttn])
            p.op("dve", lambda e: e.tensor_scalar(out=pre_ap, in0=pre_ap, scalar1=stt[:, 5:6], scalar2=stt[:, 6:7], op0=ALU.mult, op1=ALU.add),
                 reads=[prn, sttn], writes=[prn])
            p.op("dve", lambda e: e.tensor_tensor(out=pre_ap, in0=pre_ap, in1=gam[:, :], op=ALU.mult), reads=[prn] + gnames, writes=[prn])
            p.op("dve", lambda e: e.tensor_tensor(out=pre_ap, in0=pre_ap, in1=bet[:, :], op=ALU.add), reads=[prn] + gnames, writes=[prn])

        with Phase(nc) as P4:
            yct = [P4.sb(f"yct{i}", [128, 16, 128], BF16) for i in range(2)]
            xt = [P4.sb(f"xt{i}", [128, D], F32) for i in range(2)]
            lnj = P4.sb("lnj", [128, D], BF16)
            gam = P4.sb("gam", [128, D], F32)
            bet = P4.sb("bet", [128, D], F32)
            stt = P4.sb("stt", [128, 8], F32)
            h1b = P4.sb("h1b", [128, D], BF16)
            hTs = [P4.sb(f"hTs{i}", [128, 16, 128], BF16) for i in range(2)]
            psM = P4.ps("psM", [128, D], F32)
            psTr = P4.ps("psTr", [128, D], BF16)
            psMn = [f"psM{n}" for n in range(4)]
            p.op("sp", lambda e: e.dma_start(out=gam[:], in_=ln_d[0]), writes=["gam"], dma=True)
            p.op("sp", lambda e: e.dma_start(out=bet[:], in_=ln_d[1]), writes=["bet"], dma=True)
            ycat_v = ycatT_s.rearrange("(c p) t -> p c t", p=128)
            h1T_v = h1T_s.rearrange("(c p) t -> p c t", p=128)

            def p4_load(tt):
                p.op("sp", lambda e, tt=tt: e.dma_start(out=yct[tt % 2][:, :, :], in_=ycat_v[:, :, tt * 128:(tt + 1) * 128]),
                     reads=["s_yssm", "s_yat0", "s_yat1"], writes=[f"yct{tt % 2}"], dma=True)
                p.op("sp", lambda e, tt=tt: e.dma_start(out=xt[tt % 2][:, :], in_=x_d[tt * 128:(tt + 1) * 128, :]),
                     writes=[f"xt{tt % 2}"], dma=True)
            def p4_mm(tt):
                yc, ycn = yct[tt % 2], f"yct{tt % 2}"
                for kc in range(16):
                    for n in range(4):
                        p.op("pe", lambda e, yc=yc, kc=kc, n=n: e.matmul(psM[:, n * 512:(n + 1) * 512], lhsT=yc[:, kc, :],
                                                                      rhs=wo[:, kc, n * 512:(n + 1) * 512], start=(kc == 0), stop=(kc == 15)),
                             reads=[ycn, "wo"], writes=[psMn[n]] if kc == 0 else (), appends=[psMn[n]] if kc > 0 else ())
            p4_load(0)
            p4_load(1)
            p4_mm(0)
            for tt in range(16):
                x_, xn = xt[tt % 2], f"xt{tt % 2}"
                p.op("dve", lambda e, x_=x_: e.scalar_tensor_tensor(out=x_[:, :], in0=x_[:, :], scalar=ALPHA, in1=psM[:, :], op0=ALU.mult, op1=ALU.add),
                     reads=psMn + [xn], writes=[xn])
                if tt + 1 < 16:
                    p4_mm(tt + 1)
                if debug:
                    p.op("sp", lambda e, x_=x_, tt=tt: e.dma_start(out=out_d[tt * 128:(tt + 1) * 128, :], in_=x_[:, :]),
                         reads=[xn], writes=["out_dbg"], dma=True)
                layer_norm(x_[:, :], xn, gam, bet, ["gam", "bet"], stt, "stt", lnj)
                p.op("sp", lambda e, x_=x_, tt=tt: e.dma_start(out=h1_s[tt * 128:(tt + 1) * 128, :], in_=x_[:, :]),
                     reads=[xn], writes=["s_h1"] if tt == 0 else (), appends=["s_h1"] if tt > 0 else (), dma=True)
                p.op("act", lambda e, x_=x_: e.activation(out=h1b[:, :], in_=x_[:, :], func=AF.Identity), reads=[xn], writes=["h1b"])
                for dc in range(16):
                    p.op("pe", lambda e, dc=dc: e.transpose(psTr[:, dc * 128:(dc + 1) * 128], h1b[:, dc * 128:(dc + 1) * 128], ident_bf[:]),
                         reads=["h1b", "ident_bf"], writes=["psTr"] if dc == 0 else (), appends=["psTr"] if dc > 0 else ())
                hs, hsn = hTs[tt % 2], f"hTs{tt % 2}"
                p.op("dve", lambda e, hs=hs: e.tensor_copy(out=hs[:, :, :].rearrange("p c t -> p (c t)"), in_=psTr[:, :]), reads=["psTr"], writes=[hsn])
                p.op("sp", lambda e, hs=hs, tt=tt: e.dma_start(out=h1T_v[:, :, tt * 128:(tt + 1) * 128], in_=hs[:, :, :]),
                     reads=[hsn], writes=["s_h1T"] if tt == 0 else (), appends=["s_h1T"] if tt > 0 else (), dma=True)
                if tt + 2 < 16:
                    p4_load(tt + 2)
        p.barrier()
        P34.__exit__(None, None, None)
        if stop_after <= 5:
            p.wait_all("sp", [])
            p.emit()
            return nc

        with Phase(nc) as P5:
            hT = [P5.sb(f"hT{i}", [128, 16, 512], BF16) for i in range(2)]
            gT = P5.sb("gT", [128, NFC, 512], BF16)
            wu = [P5.sb(f"wu{i}", [128, 16, 256], BF16) for i in range(3)]
            wg = [P5.sb(f"wg{i}", [128, 16, 256], BF16) for i in range(3)]
            wd = [P5.sb(f"wd{i}", [128, 4, 512], BF16) for i in range(4)]
            pre2 = P5.sb("pre2", [128, 4, D], F32)
            lnj5 = P5.sb("lnj5", [128, D], BF16)
            gam5 = P5.sb("gam5", [128, D], F32)
            bet5 = P5.sb("bet5", [128, D], F32)
            stt5 = P5.sb("stt5", [128, 8], F32)
            cvp = P5.sb("cvp", [128, NFC, 4], F32)
            hprev = P5.sb("hprev", [128, NFC, 2], F32)
            hup = [P5.sb(f"hup{i}", [128, 514], F32) for i in range(2)]
            cv = [P5.sb(f"cv{i}", [128, 512], F32) for i in range(2)]
            ge = [P5.sb(f"ge{i}", [128, 512], F32) for i in range(2)]
            psU = [P5.ps(f"psU{i}", [128, 512], F32) for i in range(2)]
            psG = [P5.ps(f"psG{i}", [128, 512], F32) for i in range(2)]
            psA = [P5.ps(f"psAcc{i}", [128, 512], F32) for i in range(4)]
            p.op("sp", lambda e: e.dma_start(out=gam5[:], in_=ln_d[2]), writes=["gam5"], dma=True)
            p.op("sp", lambda e: e.dma_start(out=bet5[:], in_=ln_d[3]), writes=["bet5"], dma=True)
            p.op("sp", lambda e: e.dma_start(out=cvp[:].rearrange("p a b -> p (a b)"), in_=convp_d), writes=["cvp"], dma=True)
            p.op("dve", lambda e: e.memset(hprev[:], 0.0), writes=["hprev"])
            h1T_v5 = h1T_s.rearrange("(c p) t -> p c t", p=128)
            wup_v = w_up_d.rearrange("(c p) f -> p c f", p=128)
            wgt_v = w_gate_d.rearrange("(c p) f -> p c f", p=128)
            wdn_v = w_down_d.rearrange("(k p) d -> p k d", p=128)

            def lnj_fix():
                return None
            wl = 0
            dl = 0
            ev = 0
            for ti in range(4):
                tk0 = ti * 512
                hT_, hTn = hT[ti % 2], f"hT{ti % 2}"
                if ti == 0:
                    p.op("sp", lambda e, hT_=hT_, tk0=tk0: e.dma_start(out=hT_[:, :, :], in_=h1T_v5[:, :, tk0:tk0 + 512]),
                         reads=["s_h1T"], writes=[hTn], dma=True)
                p.op("sp", lambda e, tk0=tk0: e.dma_start(out=pre2[:, :, :], in_=h1_s[tk0:tk0 + 512, :].rearrange("(s p) d -> p s d", p=128)),
                     reads=["s_h1"], writes=["pre2"], dma=True)
                for f2 in range(NFC // 2):
                    wu_, wun = wu[wl % 3], f"wu{wl % 3}"
                    wg_, wgn = wg[wl % 3], f"wg{wl % 3}"
                    wl += 1
                    p.op("pool", lambda e, wu_=wu_, f2=f2: e.dma_start(out=wu_[:, :, :], in_=wup_v[:, :, f2 * 256:(f2 + 1) * 256]), writes=[wun], dma=True)
                    p.op("pool", lambda e, wg_=wg_, f2=f2: e.dma_start(out=wg_[:, :, :], in_=wgt_v[:, :, f2 * 256:(f2 + 1) * 256]), writes=[wgn], dma=True)
                    for sub in range(2):
                        fc = f2 * 2 + sub
                        pU, pUn = psU[ev % 2], f"psU{ev % 2}"
                        pG, pGn = psG[ev % 2], f"psG{ev % 2}"
                        hu, hun = hup[ev % 2], f"hup{ev % 2}"
                        cv_, cvn = cv[ev % 2], f"cv{ev % 2}"
                        ge_, gen = ge[ev % 2], f"ge{ev % 2}"
                        ev += 1
                        for kc in range(16):
                            p.op("pe", lambda e, pU=pU, wu_=wu_, kc=kc, sub=sub, hT_=hT_: e.matmul(
                                pU[:, :], lhsT=wu_[:, kc, sub * 128:(sub + 1) * 128], rhs=hT_[:, kc, :], start=(kc == 0), stop=(kc == 15)),
                                reads=[wun, hTn], writes=[pUn] if kc == 0 else (), appends=[pUn] if kc > 0 else ())
                        for kc in range(16):
                            p.op("pe", lambda e, pG=pG, wg_=wg_, kc=kc, sub=sub, hT_=hT_: e.matmul(
                                pG[:, :], lhsT=wg_[:, kc, sub * 128:(sub + 1) * 128], rhs=hT_[:, kc, :], start=(kc == 0), stop=(kc == 15)),
                                reads=[wgn, hTn], writes=[pGn] if kc == 0 else (), appends=[pGn] if kc > 0 else ())
                        p.op("act", lambda e, hu=hu, pU=pU: e.activation(out=hu[:, 2:514], in_=pU[:, :], func=AF.Identity), reads=[pUn], writes=[hun])
                        p.op("act", lambda e, hu=hu, fc=fc: e.activation(out=hu[:, 0:2], in_=hprev[:, fc, :], func=AF.Identity), reads=["hprev"], appends=[hun])
                        p.op("act", lambda e, hu=hu, fc=fc: e.activation(out=hprev[:, fc, :], in_=hu[:, 512:514], func=AF.Identity), reads=[hun], writes=["hprev"])
                        p.op("dve", lambda e, hu=hu, cv_=cv_, fc=fc: e.tensor_scalar(out=cv_[:, :], in0=hu[:, 2:514], scalar1=cvp[:, fc, 2:3], scalar2=cvp[:, fc, 3:4], op0=ALU.mult, op1=ALU.add),
                             reads=[hun, "cvp"], writes=[cvn])
                        p.op("dve", lambda e, hu=hu, cv_=cv_, fc=fc: e.scalar_tensor_tensor(out=cv_[:, :], in0=hu[:, 1:513], scalar=cvp[:, fc, 1:2], in1=cv_[:, :], op0=ALU.mult, op1=ALU.add),
                             reads=[hun, "cvp", cvn], writes=[cvn])
                        p.op("dve", lambda e, hu=hu, cv_=cv_, fc=fc: e.scalar_tensor_tensor(out=cv_[:, :], in0=hu[:, 0:512], scalar=cvp[:, fc, 0:1], in1=cv_[:, :], op0=ALU.mult, op1=ALU.add),
                             reads=[hun, "cvp", cvn], writes=[cvn])
                        p.op("act", lambda e, cv_=cv_, ge_=ge_: e.activation(out=ge_[:, :], in_=cv_[:, :], func=AF.Gelu), reads=[cvn], writes=[gen])
                        p.op("dve", lambda e, ge_=ge_, pG=pG, fc=fc: e.tensor_tensor(out=gT[:, fc, :], in0=ge_[:, :], in1=pG[:, :], op=ALU.mult),
                             reads=[gen, pGn], writes=[f"gT{fc}"])
                if ti + 1 < 4:
                    p.op("sp", lambda e, ti=ti: e.dma_start(out=hT[(ti + 1) % 2][:, :, :], in_=h1T_v5[:, :, (ti + 1) * 512:(ti + 2) * 512]),
                         reads=["s_h1T"], writes=[f"hT{(ti + 1) % 2}"], dma=True)
                for dg in range(4):
                    for k4 in range(NFC // 4):
                        wd_, wdn = wd[dl % 4], f"wd{dl % 4}"
                        dl += 1
                        p.op("pool", lambda e, wd_=wd_, k4=k4, dg=dg: e.dma_start(out=wd_[:, :, :], in_=wdn_v[:, k4 * 4:(k4 + 1) * 4, dg * 512:(dg + 1) * 512]),
                             writes=[wdn], dma=True)
                        for kk in range(4):
                            k = k4 * 4 + kk
                            for st_ in range(4):
                                p.op("pe", lambda e, wd_=wd_, kk=kk, k=k, st_=st_: e.matmul(
                                    psA[st_][:, :], lhsT=gT[:, k, st_ * 128:(st_ + 1) * 128], rhs=wd_[:, kk, :], start=(k == 0), stop=(k == NFC - 1)),
                                    reads=[wdn, f"gT{k}"], writes=[f"psAcc{st_}"] if k == 0 else (), appends=[f"psAcc{st_}"] if k > 0 else ())
                    for st_ in range(4):
                        p.op("dve", lambda e, st_=st_, dg=dg: e.scalar_tensor_tensor(
                            out=pre2[:, st_, dg * 512:(dg + 1) * 512], in0=pre2[:, st_, dg * 512:(dg + 1) * 512], scalar=ALPHA, in1=psA[st_][:, :], op0=ALU.mult, op1=ALU.add),
                            reads=[f"psAcc{st_}", "pre2"], writes=["pre2"])
                if debug and ti == 0:
                    for fc in range(8):
                        p.op("sp", lambda e, fc=fc: e.dma_start(out=uT_s[fc * 128:(fc + 1) * 128, 0:512], in_=gT[:, fc, :]),
                             reads=[f"gT{fc}"], writes=[f"dbg_g{fc}"], dma=True)
                for st_ in range(4):
                    if not debug:
                        layer_norm(pre2[:, st_, :], "pre2", gam5, bet5, ["gam5", "bet5"], stt5, "stt5", lnj5)
                    p.op("sp", lambda e, st_=st_, tk0=tk0: e.dma_start(out=out_d[tk0 + st_ * 128:tk0 + (st_ + 1) * 128, :], in_=pre2[:, st_, :]),
                         reads=["pre2"], writes=["out"] if (ti == 0 and st_ == 0) else (), appends=["out"] if not (ti == 0 and st_ == 0) else (), dma=True)
            p.wait_all("sp", ["out"])
        p.emit()
    return nc


def _consts():
    ident = np.eye(128, dtype=np.float32)
    pp = np.arange(128)[:, None]
    jj = np.arange(128)[None, :]
    causal = np.where(jj <= pp, 0.0, -1.0e30).astype(np.float32)
    slopes = np.exp2(-8.0 * np.arange(1, 9, dtype=np.float64) / 8.0)
    dd = np.arange(16, dtype=np.float64)
    alibi = (slopes[None, :, None] * (np.arange(128, dtype=np.float64)[:, None, None] - 128.0 * dd[None, None, :])).astype(np.float32)
    iota = np.ascontiguousarray(np.broadcast_to(np.arange(T, dtype=np.float32)[None, :], (128, T)))
    return ident, causal, np.ascontiguousarray(alibi.reshape(128, 128)), iota


def _shared_inputs(inp):
    f32 = np.float32
    G, P, HG = 64, 64, 16
    g = np.arange(G)

    def state_major(a):
        return np.ascontiguousarray(a.reshape(32, 2, P).transpose(1, 2, 0).reshape(128, 32))
    a_re = state_major(inp["ssm_a_re"][0])
    a_im = state_major(inp["ssm_a_im"][0])
    ldt = state_major(np.repeat(inp["ssm_log_dt"][0][:, None], P, axis=1))
    ssm_prm = np.ascontiguousarray(np.concatenate([a_re, a_im, ldt], axis=1).astype(f32))

    def bpad(b):
        o = np.zeros((128, 32, 128), f32)
        for gi in range(G):
            q, g2 = gi // 2, gi % 2
            r0 = (q % 4) * 32 + g2 * 16
            o[r0:r0 + 16, q, g2 * 64:(g2 + 1) * 64] = b[gi].T
        return o.reshape(128, 32 * 128)

    def cpad(c):
        o = np.zeros((128, 32, 128), f32)
        for gi in range(G):
            q, g2 = gi // 2, gi % 2
            c0 = (q % 4) * 32 + g2 * 16
            o[g2 * 64:(g2 + 1) * 64, q, c0:c0 + 16] = c[gi].T
        return o.reshape(128, 32 * 128)

    colp = np.concatenate([inp["ssm_d"][0].reshape(8, 128).T, inp["b_glu"][0].reshape(8, 128).T], axis=1).astype(f32)
    cw = inp["conv_w"][0]
    cb = inp["conv_b"][0]
    convp = np.stack([cw[0], cw[1], cw[2], cb], axis=1).reshape(NFC, 128, 4).transpose(1, 0, 2).reshape(128, NFC * 4)
    ln = np.stack([np.broadcast_to(inp[k][0][None, :], (128, D)) for k in ("ln1_g", "ln1_b", "ln2_g", "ln2_b")], axis=0)
    ident, causal, alibi, iota = _consts()
    return {
        "w_in": np.ascontiguousarray(inp["w_in"][0]),
        "w_glu": np.ascontiguousarray(inp["w_glu"][0]),
        "w_out": np.ascontiguousarray(inp["w_out"][0]),
        "w_up": np.ascontiguousarray(inp["w_up"][0]),
        "w_gate": np.ascontiguousarray(inp["w_gate"][0]),
        "w_down": np.ascontiguousarray(inp["w_down"][0]),
        "ssm_prm": ssm_prm,
        "bpad_re": bpad(inp["ssm_b_re"][0]), "bpad_im": bpad(inp["ssm_b_im"][0]),
        "cpad_re": cpad(inp["ssm_c_re"][0]), "cpad_im": cpad(inp["ssm_c_im"][0]),
        "colp": np.ascontiguousarray(colp),
        "convp": np.ascontiguousarray(convp.astype(f32)),
        "ln": np.ascontiguousarray(ln.astype(f32)),
        "ident": ident, "causal": causal, "alibi": alibi, "iota": iota,
    }


def kernel(**inputs):
    inp = {k: np.asarray(v, dtype=np.float32) for k, v in inputs.items()}
    x = inp["x"]
    nb = x.shape[0]
    shared = _shared_inputs(inp)
    in_maps = []
    for b in range(nb):
        m = dict(shared)
        m["x"] = np.ascontiguousarray(x[b])
        m["xT"] = np.ascontiguousarray(x[b].T)
        in_maps.append(m)
    nc = build_program()
    res = run_bass_kernel_spmd(nc, in_maps, core_ids=list(range(nb)))
    out = np.stack([np.asarray(r["out"], dtype=np.float32) for r in res.results], axis=0)
    return out
```

```python
import math
from contextlib import ExitStack

import numpy as np
import concourse.bass as bass
import concourse.mybir as mybir
from concourse.bass_utils import run_bass_kernel_spmd

F32 = mybir.dt.float32
BF16 = mybir.dt.bfloat16
ALU = mybir.AluOpType
AF = mybir.ActivationFunctionType
AX = mybir.AxisListType

T = 2048
D = 2048
NIN = 3664
FF = 5632
NFC = FF // 128
ALPHA = 2.0 ** 0.25
EPS = 1e-5
MAGIC = 12582912.0
TWO_PI_S = 2.0 * math.pi * (1.0 - 1e-6)
NBIS = 16
TOPK = 256

ENGS = ("pe", "act", "dve", "pool", "sp")


class Op:
    __slots__ = ("eng", "fn", "deps", "is_dma", "signal", "count", "sem", "wres", "nop")

    def __init__(self, eng, fn, is_dma):
        self.eng = eng
        self.fn = fn
        self.is_dma = is_dma
        self.deps = []
        self.signal = False
        self.count = 0
        self.sem = None
        self.wres = None
        self.nop = False


class Prog:
    def __init__(self, nc, stack):
        self.nc = nc
        self.stack = stack
        self.ops = {e: [] for e in ENGS}
        self.res = {}
        self.dma_res = {}
        self.all_ops = []
        self.since_bar_dma = []

    def _st(self, r):
        s = self.res.get(r)
        if s is None:
            s = {"w": [], "r": []}
            self.res[r] = s
        return s

    def op(self, eng, fn, reads=(), writes=(), appends=(), dma=False):
        o = Op(eng, fn, dma)
        deps = []
        for r in reads:
            deps.extend(self._st(r)["w"])
        for r in writes:
            s = self._st(r)
            deps.extend(s["w"])
            deps.extend(s["r"])
        for r in appends:
            deps.extend(self._st(r)["r"])
        for r in reads:
            self._st(r)["r"].append(o)
        for r in writes:
            s = self._st(r)
            s["w"] = [o]
            s["r"] = []
        for r in appends:
            self._st(r)["w"].append(o)
        if dma:
            wr = list(writes) + list(appends)
            assert len(wr) == 1, "a DMA must write exactly one resource"
            o.wres = wr[0]
            self.since_bar_dma.append(o)
        seen = set()
        for d in deps:
            if d is o or id(d) in seen:
                continue
            seen.add(id(d))
            if (not d.is_dma) and (not dma) and d.eng == eng == "pe":
                continue
            o.deps.append(d)
        self.ops[eng].append(o)
        self.all_ops.append(o)
        return o

    def wait_all(self, eng, reads):
        o = self.op(eng, (lambda e: None), reads=reads)
        o.nop = True
        return o

    def barrier(self):
        deps = []
        for e in ENGS:
            for o in reversed(self.ops[e]):
                if not o.is_dma and not o.nop:
                    deps.append(o)
                    break
        deps.extend(self.since_bar_dma)
        self.since_bar_dma = []
        self.res = {}
        for e in ENGS:
            o = Op(e, (lambda eng: None), False)
            o.nop = True
            o.deps = [d for d in deps]
            self.ops[e].append(o)
            self.all_ops.append(o)

    def emit(self):
        nc = self.nc
        for o in self.all_ops:
            for d in o.deps:
                d.signal = True
        esem = {e: self.stack.enter_context(nc.semaphore("s_" + e)) for e in ENGS}
        cnt = {e: 0 for e in ENGS}
        for o in self.all_ops:
            if o.is_dma:
                ent = self.dma_res.get(o.wres)
                if ent is None:
                    ent = [self.stack.enter_context(nc.semaphore("d_" + o.wres)), 0]
                    self.dma_res[o.wres] = ent
                ent[1] += 16
                o.sem = ent[0]
                o.count = ent[1]
            else:
                if o.signal and not o.nop:
                    cnt[o.eng] += 1
                o.sem = esem[o.eng]
                o.count = cnt[o.eng]

        def run(eng_name):
            def body(eng):
                waited = {}
                for o in self.ops[eng_name]:
                    need = {}
                    for d in o.deps:
                        k = id(d.sem)
                        if k not in need or need[k][1] < d.count:
                            need[k] = (d.sem, d.count)
                    for k, (sem, val) in need.items():
                        if waited.get(k, 0) >= val:
                            continue
                        eng.wait_ge(sem, val)
                        waited[k] = val
                    inst = o.fn(eng)
                    if inst is None:
                        continue
                    if o.is_dma:
                        inst.then_inc(o.sem, 16)
                    elif o.signal and not o.nop:
                        inst.then_inc(o.sem, 1)
            return body

        with nc.Block() as block:
            block.tensor(run("pe"))
            block.scalar(run("act"))
            block.vector(run("dve"))
            block.gpsimd(run("pool"))
            block.sync(run("sp"))


class Phase:
    def __init__(self, nc):
        self.nc = nc
        self.st = ExitStack()

    def __enter__(self):
        self.st.__enter__()
        return self

    def __exit__(self, *a):
        return self.st.__exit__(*a)

    def sb(self, name, shape, dt):
        return self.st.enter_context(self.nc.sbuf_tensor("sb_" + name, list(shape), dt))

    def ps(self, name, shape, dt=F32):
        return self.st.enter_context(self.nc.psum_tensor("ps_" + name, list(shape), dt))


def build_program(debug=False, stop_after=99, skip=()):
    nc = bass.Bass("TRN2", target_bir_lowering=False)

    def din(name, shape):
        return nc.dram_tensor(name, list(shape), F32, kind="ExternalInput").ap()

    xT_d = din("xT", [D, T])
    x_d = din("x", [T, D])
    w_in_d = din("w_in", [D, NIN])
    w_glu_d = din("w_glu", [1024, 1024])
    w_out_d = din("w_out", [D, D])
    w_up_d = din("w_up", [D, FF])
    w_gate_d = din("w_gate", [D, FF])
    w_down_d = din("w_down", [FF, D])
    ssm_prm_d = din("ssm_prm", [128, 3 * 32])
    bpad_re_d = din("bpad_re", [128, 32 * 128])
    bpad_im_d = din("bpad_im", [128, 32 * 128])
    cpad_re_d = din("cpad_re", [128, 32 * 128])
    cpad_im_d = din("cpad_im", [128, 32 * 128])
    colp_d = din("colp", [128, 16])
    convp_d = din("convp", [128, NFC * 4])
    ln_d = din("ln", [4, 128, D])
    ident_d = din("ident", [128, 128])
    causal_d = din("causal", [128, 128])
    alibi_d = din("alibi", [128, 8 * 16])
    iota_d = din("iota", [128, T])

    out_d = nc.dram_tensor("out", [T, D], F32, kind="ExternalOutput").ap()
    skind = "ExternalOutput" if debug else "Internal"
    uT_s = nc.dram_tensor("s_uT", [1024, T], BF16, kind=skind).ap()
    yT_s = nc.dram_tensor("s_yT", [1024, T], BF16, kind=skind).ap()
    ycatT_s = nc.dram_tensor("s_ycatT", [2048, T], BF16, kind=skind).ap()
    h1_s = nc.dram_tensor("s_h1", [T, D], F32, kind=skind).ap()
    h1T_s = nc.dram_tensor("s_h1T", [D, T], BF16, kind=skind).ap()

    with ExitStack() as top:
        p = Prog(nc, top)
        G = Phase(nc)
        top.enter_context(G)
        ident_bf = G.sb("ident_bf", [128, 128], BF16)
        ident_f = G.sb("ident_f", [128, 128], F32)
        ones_bf = G.sb("ones_bf", [128, 128], BF16)
        p.op("pool", lambda e: e.dma_start(out=ident_bf[:], in_=ident_d), writes=["ident_bf"], dma=True)
        p.op("sp", lambda e: e.dma_start(out=ident_f[:], in_=ident_d), writes=["ident_f"], dma=True)
        p.op("dve", lambda e: e.memset(ones_bf[:], 1.0), writes=["ones_bf"])

        w_in_v = w_in_d.rearrange("(c p) n -> p c n", p=128)

        with Phase(nc) as A:
            QT = A.sb("QT", [128, 8, T], BF16)
            KT = A.sb("KT", [128, 2, T], BF16)
            qiT = A.sb("qiT", [128, 8, T], BF16)
            kiT2 = A.sb("kiT2", [128, T], BF16)
            Vt = A.sb("Vt", [128, 16, 256], BF16)
            wi_t = A.sb("wi_t", [128, 16, 16], F32)

            with Phase(nc) as P1:
                xTb = P1.sb("xTb", [128, 16, T], BF16)
                wb = [P1.sb(f"wb{i}", [128, 16, 256], BF16) for i in range(3)]
                wv = P1.sb("wv", [128, 16, 256], BF16)
                wvi = P1.sb("wvi", [128, 16, 80], BF16)
                ust = [P1.sb(f"ust{i}", [128, T], BF16) for i in range(2)]
                psm = [P1.ps(f"p1ps{i}", [128, T], F32) for i in range(2)]

                p.op("pool", lambda e: e.dma_start(out=wb[0][:, :, :], in_=w_in_v[:, :, 0:256]), writes=["wb0"], dma=True)
                for c in range(16):
                    p.op("pool", lambda e, c=c: e.dma_start(out=xTb[:, c, :], in_=xT_d[c * 128:(c + 1) * 128, :]),
                         writes=[f"xTb{c}"], dma=True)

                jobs = []
                for i in range(4):
                    jobs.append((i * 256, [("u", 2 * i), ("u", 2 * i + 1)]))
                for i in range(4):
                    jobs.append((1024 + i * 256, [("q", 2 * i), ("q", 2 * i + 1)]))
                jobs.append((2048, [("k", 0), ("k", 1)]))
                for i in range(4):
                    jobs.append((2560 + i * 256, [("qi", 2 * i), ("qi", 2 * i + 1)]))
                jobs.append((None, [("ki", 0)]))

                tile_no = 0
                for jn, (col0, dests) in enumerate(jobs):
                    if stop_after < 1 and jn >= int(stop_after * 10):
                        break
                    if col0 is None and "ki" in skip:
                        continue
                    wbuf = wb[jn % 3]
                    wname = f"wb{jn % 3}"
                    if col0 is None:
                        p.op("pool", lambda e, wbuf=wbuf: e.dma_start(out=wbuf[:, :, 0:64], in_=w_in_v[:, :, 3584:3648]),
                             writes=[wname], dma=True)
                        p.op("pool", lambda e, wbuf=wbuf: e.dma_start(out=wbuf[:, :, 64:128], in_=w_in_v[:, :, 3584:3648]),
                             appends=[wname], dma=True)
                    elif jn > 0:
                        p.op("pool", lambda e, wbuf=wbuf, col0=col0: e.dma_start(out=wbuf[:, :, :], in_=w_in_v[:, :, col0:col0 + 256]),
                             writes=[wname], dma=True)
                    for sub, (kind, idx) in enumerate(dests):
                        ps = psm[tile_no % 2]
                        psn = [f"p1ps{tile_no % 2}_{n}" for n in range(4)]
                        for kc in range(16):
                            for n in range(4):
                                p.op("pe", lambda e, ps=ps, wbuf=wbuf, kc=kc, n=n, sub=sub: e.matmul(
                                    ps[:, n * 512:(n + 1) * 512], lhsT=wbuf[:, kc, sub * 128:(sub + 1) * 128],
                                    rhs=xTb[:, kc, n * 512:(n + 1) * 512], start=(kc == 0), stop=(kc == 15)),
                                    reads=[wname, f"xTb{kc}"],
                                    writes=[psn[n]] if kc == 0 else (), appends=[psn[n]] if kc > 0 else ())
                        if kind == "u":
                            dst = ust[idx % 2]
                            dname = f"ust{idx % 2}"
                            dst_ap = dst[:, :]
                        elif kind == "q":
                            dst_ap, dname = QT[:, idx, :], f"QT{idx}"
                        elif kind == "k":
                            dst_ap, dname = KT[:, idx, :], f"KT{idx}"
                        elif kind == "qi":
                            dst_ap, dname = qiT[:, idx, :], f"qiT{idx}"
                        else:
                            dst_ap, dname = kiT2[:, :], "kiT2"
                        if tile_no % 2 == 0:
                            p.op("act", lambda e, ps=ps, dst_ap=dst_ap: e.activation(out=dst_ap, in_=ps[:, :], func=AF.Identity),
                                 reads=psn, writes=[dname])
                        else:
                            p.op("dve", lambda e, ps=ps, dst_ap=dst_ap: e.tensor_copy(out=dst_ap, in_=ps[:, :]),
                                 reads=psn, writes=[dname])
                        if kind == "u":
                            p.op("sp", lambda e, dst=dst, idx=idx: e.dma_start(out=uT_s[idx * 128:(idx + 1) * 128, :], in_=dst[:, :]),
                                 reads=[dname], writes=[f"s_uT{idx}"], dma=True)
                        tile_no += 1

                p.op("pool", lambda e: e.dma_start(out=wv[:, :, 0:256], in_=w_in_v[:, :, 2304:2560]), writes=["wv"], dma=True)
                p.op("pool", lambda e: e.dma_start(out=wvi[:, :, :], in_=w_in_v[:, :, 3584:3664]), writes=["wvi"], dma=True)
                for tt in range(16 if (stop_after >= 1 and 'tm' not in skip) else 0):
                    ps = psm[tt % 2]
                    pn = f"p1ps{tt % 2}_0"
                    pn1 = f"p1ps{tt % 2}_1"
                    for kc in range(16):
                        p.op("pe", lambda e, ps=ps, kc=kc, tt=tt: e.matmul(
                            ps[:, 0:256], lhsT=xTb[:, kc, tt * 128:(tt + 1) * 128], rhs=wv[:, kc, 0:256],
                            start=(kc == 0), stop=(kc == 15)),
                            reads=["wv", f"xTb{kc}"], writes=[pn] if kc == 0 else (), appends=[pn] if kc > 0 else ())
                    for kc in range(16):
                        p.op("pe", lambda e, ps=ps, kc=kc, tt=tt: e.matmul(
                            ps[:, 512:528], lhsT=xTb[:, kc, tt * 128:(tt + 1) * 128], rhs=wvi[:, kc, 64:80],
                            start=(kc == 0), stop=(kc == 15)),
                            reads=["wvi", f"xTb{kc}"], writes=[pn1] if kc == 0 else (), appends=[pn1] if kc > 0 else ())
                    p.op("act", lambda e, ps=ps, tt=tt: e.activation(out=Vt[:, tt, :], in_=ps[:, 0:256], func=AF.Identity),
                         reads=[pn], writes=[f"Vt{tt}"])
                    p.op("dve", lambda e, ps=ps, tt=tt: e.tensor_copy(out=wi_t[:, tt, :], in_=ps[:, 512:528]),
                         reads=[pn1], writes=[f"wi{tt}"])
            p.barrier()
            if stop_after <= 1:
                p.wait_all("sp", [])
                p.emit()
                return nc

            with Phase(nc) as P2:
                causal = P2.sb("causal", [128, 128], F32)
                alibi = P2.sb("alibi", [128, 8, 16], F32)
                sc = [P2.sb(f"sc{i}", [128, T], F32) for i in range(2)]
                rl = [P2.sb(f"rl{i}", [128, 512], F32) for i in range(3)]
                junk = P2.sb("junk", [128, T], BF16)
                mask = P2.sb("mask", [128, T], BF16)
                maskT = [P2.sb(f"maskT{i}", [128, 16, 128], BF16) for i in range(2)]
                PTb = [P2.sb(f"PT{i}", [128, 4, 128], BF16) for i in range(2)]
                PTm = [P2.sb(f"PTm{i}", [128, 4, 128], BF16) for i in range(2)]
                rden = P2.sb("rden", [128, 512], F32)
                yst = [P2.sb(f"yst{i}", [128, 4, 128], BF16) for i in range(2)]
                bs = P2.sb("bs", [128, 16], F32)
                psI = [P2.ps(f"psI{i}", [128, 512], F32) for i in range(3)]
                psS = [P2.ps(f"psS{i}", [128, 512], F32) for i in range(2)]
                psO = P2.ps("psO", [128, 512], F32)
                psD = P2.ps("psD", [128, 512], F32)
                psT = P2.ps("psT", [128, 1024], BF16)

                p.op("sp", lambda e: e.dma_start(out=causal[:], in_=causal_d), writes=["causal"], dma=True)
                p.op("sp", lambda e: e.dma_start(out=alibi[:].rearrange("p h d -> p (h d)"), in_=alibi_d), writes=["alibi"], dma=True)

                HI, LO, RW, NLO, MID, CNT, TTS, THR = range(8)
                rot = {"ii": 0, "ri": 0, "si": 0}

                def gen_front(j):
                    nk = 128 * (j + 1)
                    nch = (nk + 511) // 512
                    scj = sc[j % 2]
                    scn = f"sc{j % 2}"
                    q0 = j * 128
                    mT = maskT[j % 2]
                    mTn = f"maskT{j % 2}"
                    for h in range(16):
                        m, half = h // 2, h % 2
                        pl, ph = half * 64, half * 64 + 64
                        for n in range(nch):
                            c0 = n * 512
                            cw = min(512, nk - c0)
                            pI = psI[rot["ii"] % 3]
                            pIn = f"psI{rot['ii'] % 3}"
                            rot["ii"] += 1
                            r_ = rl[rot["ri"] % 3]
                            rn = f"rl{rot['ri'] % 3}"
                            rot["ri"] += 1
                            p.op("pe", lambda e, pI=pI, m=m, pl=pl, ph=ph, q0=q0, c0=c0, cw=cw: e.matmul(
                                pI[:, 0:cw], lhsT=qiT[pl:ph, m, q0:q0 + 128], rhs=kiT2[pl:ph, c0:c0 + cw],
                                start=True, stop=True), reads=[f"qiT{m}", "kiT2"], writes=[pIn])
                            p.op("act", lambda e, pI=pI, r_=r_, cw=cw: e.activation(out=r_[:, 0:cw], in_=pI[:, 0:cw], func=AF.Relu),
                                 reads=[pIn], writes=[rn])
                            if h == 0:
                                p.op("dve", lambda e, r_=r_, scj=scj, c0=c0, cw=cw, j=j, h=h: e.tensor_scalar(
                                    out=scj[:, c0:c0 + cw], in0=r_[:, 0:cw], scalar1=wi_t[:, j, h:h + 1], scalar2=None, op0=ALU.mult),
                                    reads=[rn, f"wi{j}"], writes=[f"{scn}_{n}"])
                            else:
                                p.op("dve", lambda e, r_=r_, scj=scj, c0=c0, cw=cw, j=j, h=h: e.scalar_tensor_tensor(
                                    out=scj[:, c0:c0 + cw], in0=r_[:, 0:cw], scalar=wi_t[:, j, h:h + 1], in1=scj[:, c0:c0 + cw],
                                    op0=ALU.mult, op1=ALU.add),
                                    reads=[rn, f"wi{j}", f"{scn}_{n}"], writes=[f"{scn}_{n}"])
                            yield
                    scall = [f"{scn}_{n}" for n in range(nch)]
                    p.op("dve", lambda e, scj=scj, q0=q0: e.tensor_tensor(out=scj[:, q0:q0 + 128], in0=scj[:, q0:q0 + 128], in1=causal[:, :], op=ALU.add),
                         reads=scall + ["causal"], writes=scall)
                    yield "BIS"
                    if j < 2:
                        p.op("dve", lambda e: e.memset(bs[:, THR:THR + 1], -1.0e29), writes=["bs"])
                    else:
                        p.op("dve", lambda e, scj=scj, nk=nk: e.tensor_reduce(out=bs[:, HI:HI + 1], in_=scj[:, 0:nk], axis=AX.X, op=ALU.max),
                             reads=scall + ["bs"], writes=["bs"])
                        p.op("dve", lambda e, scj=scj, q0=q0: e.tensor_reduce(out=bs[:, LO:LO + 1], in_=scj[:, 0:q0], axis=AX.X, op=ALU.min),
                             reads=scall + ["bs"], writes=["bs"])
                        yield
                        p.op("dve", lambda e: e.tensor_tensor(out=bs[:, RW:RW + 1], in0=bs[:, HI:HI + 1], in1=bs[:, LO:LO + 1], op=ALU.subtract),
                             reads=["bs"], writes=["bs"])
                        p.op("dve", lambda e: e.reciprocal(out=bs[:, RW:RW + 1], in_=bs[:, RW:RW + 1]), reads=["bs"], writes=["bs"])
                        p.op("dve", lambda e: e.scalar_tensor_tensor(out=bs[:, NLO:NLO + 1], in0=bs[:, LO:LO + 1], scalar=-1.0, in1=bs[:, RW:RW + 1],
                                                                    op0=ALU.mult, op1=ALU.mult), reads=["bs"], writes=["bs"])
                        yield
                        p.op("dve", lambda e, scj=scj, nk=nk: e.tensor_scalar(out=scj[:, 0:nk], in0=scj[:, 0:nk], scalar1=bs[:, RW:RW + 1], scalar2=bs[:, NLO:NLO + 1],
                                                                       op0=ALU.mult, op1=ALU.add), reads=scall + ["bs"], writes=scall)
                        p.op("dve", lambda e: e.memset(bs[:, MID:MID + 1], 0.5), reads=["bs"], writes=["bs"])
                        yield
                        for it in range(NBIS):
                            p.op("dve", lambda e, scj=scj, nk=nk: e.tensor_scalar(
                                out=junk[:, 0:nk], in0=scj[:, 0:nk], scalar1=bs[:, MID:MID + 1], scalar2=0.0,
                                op0=ALU.is_ge, op1=ALU.add, accum_out=bs[:, CNT:CNT + 1]),
                                reads=scall + ["bs"], writes=["bs", "junk"])
                            yield
                            p.op("dve", lambda e, it=it: e.tensor_scalar(
                                out=bs[:, TTS:TTS + 1], in0=bs[:, CNT:CNT + 1], scalar1=TOPK - 0.5, scalar2=0.5 ** (it + 1),
                                op0=ALU.is_ge, op1=ALU.mult), reads=["bs"], writes=["bs"])
                            yield
                            p.op("dve", lambda e, it=it: e.scalar_tensor_tensor(
                                out=bs[:, MID:MID + 1], in0=bs[:, TTS:TTS + 1], scalar=-(0.5 ** (it + 2)), in1=bs[:, MID:MID + 1],
                                op0=ALU.add, op1=ALU.add), reads=["bs"], writes=["bs"])
                            yield
                        p.op("dve", lambda e: e.tensor_scalar(out=bs[:, THR:THR + 1], in0=bs[:, MID:MID + 1], scalar1=-(0.5 ** (NBIS + 1)), scalar2=None, op0=ALU.add),
                             reads=["bs"], writes=["bs"])
                    p.op("dve", lambda e, scj=scj, nk=nk: e.tensor_scalar(
                        out=mask[:, 0:nk], in0=scj[:, 0:nk], scalar1=bs[:, THR:THR + 1], scalar2=None, op0=ALU.is_ge),
                        reads=scall + ["bs"], writes=["mask"])
                    yield
                    for g0 in range(0, j + 1, 8):
                        g1 = min(j + 1, g0 + 8)
                        for i in range(g0, g1):
                            p.op("pe", lambda e, i=i, g0=g0: e.transpose(psT[:, (i - g0) * 128:(i - g0 + 1) * 128], mask[:, i * 128:(i + 1) * 128], ident_bf[:]),
                                 reads=["mask", "ident_bf"], writes=["psT"] if i == g0 else (), appends=["psT"] if i > g0 else ())
                        p.op("act", lambda e, g0=g0, g1=g1, mT=mT: e.activation(
                            out=mT[:, g0:g1, :], in_=psT[:, 0:(g1 - g0) * 128].rearrange("p (g t) -> p g t", t=128), func=AF.Identity),
                            reads=["psT"], writes=[mTn] if g0 == 0 else (), appends=[mTn] if g0 > 0 else ())
                        yield

                def gen_attn(j):
                    q0 = j * 128
                    mT = maskT[j % 2]
                    mTn = f"maskT{j % 2}"
                    steps = [(c, i) for c in range(2) for i in range(j + 1)]
                    par = []

                    def emit_st(k):
                        c, i = steps[k]
                        k_ = rot["si"] % 2
                        rot["si"] += 1
                        par.append(k_)
                        pS, pSn = psS[k_], f"psS{k_}"
                        for hh in range(4):
                            p.op("pe", lambda e, pS=pS, c=c, i=i, hh=hh, q0=q0: e.matmul(
                                pS[:, hh * 128:(hh + 1) * 128], lhsT=KT[:, c, i * 128:(i + 1) * 128],
                                rhs=QT[:, 4 * c + hh, q0:q0 + 128], start=True, stop=True),
                                reads=[f"KT{c}", f"QT{4 * c + hh}"], writes=[pSn] if hh == 0 else (), appends=[pSn] if hh > 0 else ())

                    emit_st(0)
                    for k, (c, i) in enumerate(steps):
                        if k + 1 < len(steps):
                            emit_st(k + 1)
                        k_ = par[k]
                        pS, pSn = psS[k_], f"psS{k_}"
                        PT_, PTn = PTb[k_], f"PT{k_}"
                        PM_, PMn = PTm[k_], f"PTm{k_}"
                        for hh in range(4):
                            p.op("act", lambda e, pS=pS, PT_=PT_, c=c, i=i, hh=hh, j=j: e.activation(
                                out=PT_[:, hh, :], in_=pS[:, hh * 128:(hh + 1) * 128], func=AF.Exp,
                                bias=alibi[:, 4 * c + hh, (j - i):(j - i) + 1], scale=128.0 ** -0.5),
                                reads=[pSn, "alibi"], writes=[PTn] if hh == 0 else (), appends=[PTn] if hh > 0 else ())
                        p.op("dve", lambda e, PT_=PT_, PM_=PM_, i=i, mT=mT: e.tensor_tensor(
                            out=PM_[:, :, :], in0=PT_[:, :, :], in1=mT[:, i:i + 1, :].to_broadcast([128, 4, 128]), op=ALU.mult),
                            reads=[PTn, mTn], writes=[PMn])
                        p.op("pe", lambda e, PM_=PM_, c=c, i=i, j=j: e.matmul(
                            psO[:, :], lhsT=Vt[:, i, c * 128:(c + 1) * 128], rhs=PM_[:, :, :].rearrange("p h t -> p (h t)"),
                            start=(i == 0), stop=(i == j)),
                            reads=[PMn, f"Vt{i}"], writes=["psO"] if i == 0 else (), appends=["psO"] if i > 0 else ())
                        p.op("pe", lambda e, PM_=PM_, i=i, j=j: e.matmul(
                            psD[:, :], lhsT=ones_bf[:, :], rhs=PM_[:, :, :].rearrange("p h t -> p (h t)"),
                            start=(i == 0), stop=(i == j)),
                            reads=[PMn, "ones_bf"], writes=["psD"] if i == 0 else (), appends=["psD"] if i > 0 else ())
                        if i == j:
                            ys = yst[c]
                            ysn = f"yst{c}"
                            p.op("dve", lambda e: e.reciprocal(out=rden[:, :], in_=psD[:, :]), reads=["psD"], writes=["rden"])
                            p.op("dve", lambda e, ys=ys: e.tensor_tensor(out=ys[:, :, :].rearrange("p h t -> p (h t)"), in0=psO[:, :], in1=rden[:, :], op=ALU.mult),
                                 reads=["psO", "rden"], writes=[ysn])
                            p.op("sp", lambda e, ys=ys, c=c, q0=q0: e.dma_start(
                                out=ycatT_s[1024 + c * 512:1024 + (c + 1) * 512, q0:q0 + 128].rearrange("(h p) t -> p h t", p=128), in_=ys[:, :, :]),
                                reads=[ysn], writes=[f"s_yat{c}"], dma=True)
                        yield

                for _ in gen_front(0):
                    pass
                for j in range(16):
                    fa = gen_attn(j)
                    ff = gen_front(j + 1) if j + 1 < 16 else None
                    if ff is not None:
                        for tok in ff:
                            if tok == "BIS":
                                break
                        else:
                            ff = None
                    gens = [fa] + ([ff] if ff is not None else [])
                    while gens:
                        for g_ in list(gens):
                            try:
                                next(g_)
                            except StopIteration:
                                gens.remove(g_)
        p.barrier()
        if stop_after <= 2:
            p.wait_all("sp", [])
            p.emit()
            return nc

        with Phase(nc) as P3:
            prm = P3.sb("prm", [128, 96], F32)
            sv = P3.sb("sv", [128, 16, 32], F32)
            Bre = P3.sb("Bre", [128, 32, 128], BF16)
            Bim = P3.sb("Bim", [128, 32, 128], BF16)
            CR = P3.sb("CR", [128, 32, 128], BF16)
            NCR = P3.sb("NCR", [128, 32, 128], BF16)
            NCI = P3.sb("NCI", [128, 32, 128], BF16)
            colp = P3.sb("colp", [128, 16], F32)
            diagD = P3.sb("diagD", [128, 8, 128], BF16)
            iot = P3.sb("iot", [128, T], F32)
            hpi = P3.sb("hpi", [128, 1], F32)
            p.op("dve", lambda e: e.memset(hpi[:], 0.5 * math.pi), writes=["prm"])
            p.op("sp", lambda e: e.dma_start(out=prm[:], in_=ssm_prm_d), appends=["prm"], dma=True)
            p.op("sp", lambda e: e.dma_start(out=colp[:], in_=colp_d), writes=["colp"], dma=True)
            p.op("sp", lambda e: e.dma_start(out=iot[:], in_=iota_d), writes=["iot"], dma=True)
            p.op("pool", lambda e: e.dma_start(out=Bre[:].rearrange("p a b -> p (a b)"), in_=bpad_re_d), writes=["Bre"], dma=True)
            p.op("pool", lambda e: e.dma_start(out=Bim[:].rearrange("p a b -> p (a b)"), in_=bpad_im_d), writes=["Bim"], dma=True)

            A_RE, A_IM, LDT = prm[:, 0:32], prm[:, 32:64], prm[:, 64:96]
            (DT, RR, TH, CO, SI, FRE, FIM, T0, T1, T2, T3, DEN) = range(12)

            def svv(k):
                return sv[:, k, :]

            def d1(fn, **kw):
                p.op("dve", fn, reads=["prm", "sv"], writes=["sv"])

            def a1(fn):
                p.op("act", fn, reads=["prm", "sv"], writes=["sv"])

            a1(lambda e: e.activation(out=svv(DT), in_=LDT, func=AF.Exp))
            d1(lambda e: e.tensor_tensor(out=svv(T0), in0=svv(DT), in1=A_RE, op=ALU.mult))
            a1(lambda e: e.activation(out=svv(RR), in_=svv(T0), func=AF.Exp))
            d1(lambda e: e.tensor_tensor(out=svv(TH), in0=svv(DT), in1=A_IM, op=ALU.mult))
            d1(lambda e: e.tensor_scalar(out=svv(TH), in0=svv(TH), scalar1=1.0 / (2.0 * math.pi), scalar2=None, op0=ALU.mult))
            d1(lambda e: e.tensor_scalar(out=svv(T0), in0=svv(TH), scalar1=MAGIC, scalar2=None, op0=ALU.add))
            d1(lambda e: e.tensor_scalar(out=svv(T0), in0=svv(T0), scalar1=-MAGIC, scalar2=None, op0=ALU.add))
            d1(lambda e: e.tensor_tensor(out=svv(T1), in0=svv(TH), in1=svv(T0), op=ALU.subtract))
            a1(lambda e: e.activation(out=svv(SI), in_=svv(T1), func=AF.Sin, scale=TWO_PI_S))
            a1(lambda e: e.activation(out=svv(T2), in_=svv(T1), func=AF.Abs))
            a1(lambda e: e.activation(out=svv(CO), in_=svv(T2), func=AF.Sin, bias=hpi[:, 0:1], scale=-TWO_PI_S))
            d1(lambda e: e.tensor_tensor(out=svv(T0), in0=svv(RR), in1=svv(CO), op=ALU.mult))
            d1(lambda e: e.tensor_scalar(out=svv(T0), in0=svv(T0), scalar1=-1.0, scalar2=None, op0=ALU.add))
            d1(lambda e: e.tensor_tensor(out=svv(T1), in0=svv(RR), in1=svv(SI), op=ALU.mult))
            d1(lambda e: e.tensor_tensor(out=svv(DEN), in0=A_RE, in1=A_RE, op=ALU.mult))
            d1(lambda e: e.tensor_tensor(out=svv(T2), in0=A_IM, in1=A_IM, op=ALU.mult))
            d1(lambda e: e.tensor_tensor(out=svv(DEN), in0=svv(DEN), in1=svv(T2), op=ALU.add))
            d1(lambda e: e.reciprocal(out=svv(DEN), in_=svv(DEN)))
            d1(lambda e: e.tensor_tensor(out=svv(T2), in0=svv(T0), in1=A_RE, op=ALU.mult))
            d1(lambda e: e.tensor_tensor(out=svv(T3), in0=svv(T1), in1=A_IM, op=ALU.mult))
            d1(lambda e: e.tensor_tensor(out=svv(T2), in0=svv(T2), in1=svv(T3), op=ALU.add))
            d1(lambda e: e.tensor_tensor(out=svv(FRE), in0=svv(T2), in1=svv(DEN), op=ALU.mult))
            d1(lambda e: e.tensor_tensor(out=svv(T2), in0=svv(T1), in1=A_RE, op=ALU.mult))
            d1(lambda e: e.tensor_tensor(out=svv(T3), in0=svv(T0), in1=A_IM, op=ALU.mult))
            d1(lambda e: e.tensor_tensor(out=svv(T2), in0=svv(T2), in1=svv(T3), op=ALU.subtract))
            d1(lambda e: e.tensor_tensor(out=svv(FIM), in0=svv(T2), in1=svv(DEN), op=ALU.mult))

            with Phase(nc) as P3c:
                cre = P3c.sb("cre", [128, 32, 128], F32)
                cim = P3c.sb("cim", [128, 32, 128], F32)
                ct1 = P3c.sb("ct1", [128, 32, 128], F32)
                ct2 = P3c.sb("ct2", [128, 32, 128], F32)
                p.op("sp", lambda e: e.dma_start(out=cre[:].rearrange("p a b -> p (a b)"), in_=cpad_re_d), writes=["cre"], dma=True)
                p.op("sp", lambda e: e.dma_start(out=cim[:].rearrange("p a b -> p (a b)"), in_=cpad_im_d), writes=["cim"], dma=True)

                def bc(k):
                    return sv[:, k, :].unsqueeze(2).to_broadcast([128, 32, 128])
                p.op("dve", lambda e: e.tensor_tensor(out=ct1[:], in0=cre[:], in1=bc(FRE), op=ALU.mult), reads=["cre", "sv"], writes=["ct1"])
                p.op("dve", lambda e: e.tensor_tensor(out=ct2[:], in0=cim[:], in1=bc(FIM), op=ALU.mult), reads=["cim", "sv"], writes=["ct2"])
                p.op("dve", lambda e: e.tensor_tensor(out=CR[:], in0=ct1[:], in1=ct2[:], op=ALU.subtract), reads=["ct1", "ct2"], writes=["CR"])
                p.op("dve", lambda e: e.tensor_tensor(out=NCR[:], in0=ct2[:], in1=ct1[:], op=ALU.subtract), reads=["ct1", "ct2"], writes=["NCR"])
                p.op("dve", lambda e: e.tensor_tensor(out=ct1[:], in0=cre[:], in1=bc(FIM), op=ALU.mult), reads=["cre", "sv", "CR", "NCR"], writes=["ct1"])
                p.op("dve", lambda e: e.tensor_tensor(out=ct2[:], in0=cim[:], in1=bc(FRE), op=ALU.mult), reads=["cim", "sv", "CR", "NCR"], writes=["ct2"])
                p.op("dve", lambda e: e.tensor_tensor(out=ct1[:], in0=ct1[:], in1=ct2[:], op=ALU.add), reads=["ct1", "ct2"], writes=["ct1"])
                p.op("dve", lambda e: e.tensor_scalar(out=NCI[:], in0=ct1[:], scalar1=-1.0, scalar2=None, op0=ALU.mult), reads=["ct1"], writes=["NCI"])
            for ct in range(8):
                p.op("dve", lambda e, ct=ct: e.tensor_scalar(out=diagD[:, ct, :], in0=ident_f[:, :], scalar1=colp[:, ct:ct + 1], scalar2=None, op0=ALU.mult),
                     reads=["ident_f", "colp"], appends=["diagD"])
            p.barrier()

            with Phase(nc) as P3m:
                uTt = [P3m.sb(f"uTt{i}", [128, T], BF16) for i in range(2)]
                angA = P3m.sb("angA", [128, T], F32)
                angB = P3m.sb("angB", [128, T], F32)
                negM = P3m.sb("negM", [128, 1], F32)
                cosb = [P3m.sb(f"cosb{i}", [128, T], BF16) for i in range(2)]
                sinb = [P3m.sb(f"sinb{i}", [128, T], BF16) for i in range(2)]
                breb = [P3m.sb(f"breb{i}", [128, T], BF16) for i in range(2)]
                bimb = [P3m.sb(f"bimb{i}", [128, T], BF16) for i in range(2)]
                tmb = [P3m.sb(f"tmb{i}", [128, T], BF16) for i in range(4)]
                xreb = P3m.sb("xreb", [128, T], BF16)
                ximb = P3m.sb("ximb", [128, T], BF16)
                greb = P3m.sb("greb", [128, T], BF16)
                gimb = P3m.sb("gimb", [128, T], BF16)
                PP = [P3m.sb(f"PP{i}", [128, T], BF16) for i in range(4)]
                ygs = [P3m.sb(f"ygs{i}", [128, T], BF16) for i in range(2)]
                psB = P3m.ps("psBu", [128, T], F32)
                psY = P3m.ps("psY", [128, T], F32)
                psYn = [f"psY{n}" for n in range(4)]
                p.op("dve", lambda e: e.memset(negM[:], -MAGIC), writes=["negM"])

                def tab_A(q):
                    p.op("dve", lambda e, q=q: e.tensor_scalar(out=angA[:, :], in0=iot[:, :], scalar1=sv[:, TH, q:q + 1], scalar2=MAGIC, op0=ALU.mult, op1=ALU.add),
                         reads=["iot", "sv"], writes=["angA"])

                def tab_B(q):
                    p.op("act", lambda e: e.activation(out=angB[:, :], in_=angA[:, :], func=AF.Identity, bias=negM[:, 0:1], scale=1.0),
                         reads=["angA", "negM"], writes=["angB"])

                def tab_C(q):
                    p.op("dve", lambda e, q=q: e.scalar_tensor_tensor(out=angA[:, :], in0=iot[:, :], scalar=sv[:, TH, q:q + 1], in1=angB[:, :], op0=ALU.mult, op1=ALU.subtract),
                         reads=["iot", "sv", "angB"], writes=["angA"])

                def tab_D(q):
                    cb, cbn = cosb[q % 2], f"cosb{q % 2}"
                    sb_, sbn = sinb[q % 2], f"sinb{q % 2}"
                    p.op("act", lambda e, sb_=sb_: e.activation(out=sb_[:, :], in_=angA[:, :], func=AF.Sin, scale=TWO_PI_S), reads=["angA"], writes=[sbn])
                    p.op("act", lambda e: e.activation(out=angB[:, :], in_=angA[:, :], func=AF.Abs), reads=["angA"], writes=["angB"])
                    p.op("act", lambda e, cb=cb: e.activation(out=cb[:, :], in_=angB[:, :], func=AF.Sin, bias=hpi[:, 0:1], scale=-TWO_PI_S), reads=["angB", "hpi"], writes=[cbn])

                def load_u(ct):
                    p.op("sp", lambda e, ct=ct: e.dma_start(out=uTt[ct % 2][:, :], in_=uT_s[ct * 128:(ct + 1) * 128, :]),
                         reads=[f"s_uT{ct}"], writes=[f"uTt{ct % 2}"], dma=True)

                def emit_dterm(ct):
                    ut, utn = uTt[ct % 2], f"uTt{ct % 2}"
                    for n in range(4):
                        p.op("pe", lambda e, ut=ut, ct=ct, n=n: e.matmul(psY[:, n * 512:(n + 1) * 512], lhsT=diagD[:, ct, :],
                                                                     rhs=ut[:, n * 512:(n + 1) * 512], start=True, stop=False),
                             reads=[utn, "diagD"], writes=[psYn[n]])

                def emit_bu(q):
                    ct = q // 4
                    ut, utn = uTt[ct % 2], f"uTt{ct % 2}"
                    br, brn = breb[q % 2], f"breb{q % 2}"
                    bi, bin_ = bimb[q % 2], f"bimb{q % 2}"
                    for hf in range(2):
                        t0 = hf * 1024
                        pbn = ["psB0", "psB1", "psB2", "psB3"]
                        for part, Bw, bwn in ((0, Bre, "Bre"), (1, Bim, "Bim")):
                            for n in range(2):
                                p.op("pe", lambda e, Bw=Bw, q=q, ut=ut, part=part, n=n, t0=t0: e.matmul(
                                    psB[:, part * 1024 + n * 512:part * 1024 + (n + 1) * 512], lhsT=Bw[:, q, :],
                                    rhs=ut[:, t0 + n * 512:t0 + (n + 1) * 512], start=True, stop=True),
                                    reads=[bwn, utn], writes=[pbn[part * 2 + n]])
                        sl = slice(t0, t0 + 1024)
                        p.op("act", lambda e, sl=sl, br=br: e.activation(out=br[:, sl], in_=psB[:, 0:1024], func=AF.Identity),
                             reads=["psB0", "psB1"], writes=[brn] if hf == 0 else (), appends=[brn] if hf else ())
                        p.op("act", lambda e, sl=sl, bi=bi: e.activation(out=bi[:, sl], in_=psB[:, 1024:2048], func=AF.Identity),
                             reads=["psB2", "psB3"], writes=[bin_] if hf == 0 else (), appends=[bin_] if hf else ())

                load_u(0)
                tab_A(0); tab_B(0); tab_C(0); tab_D(0)
                emit_bu(0)
                for q in range(32):
                    ct, pq = divmod(q, 4)
                    cb, cbn = cosb[q % 2], f"cosb{q % 2}"
                    sb_, sbn = sinb[q % 2], f"sinb{q % 2}"
                    br, brn = breb[q % 2], f"breb{q % 2}"
                    bi, bin_ = bimb[q % 2], f"bimb{q % 2}"
                    if pq == 0:
                        if ct + 1 < 8:
                            load_u(ct + 1)
                        emit_dterm(ct)
                    if q + 1 < 32:
                        tab_A(q + 1)
                        tab_B(q + 1)
                        emit_bu(q + 1)
                    p.op("dve", lambda e, cb=cb, br=br: e.tensor_tensor(out=tmb[0][:, :], in0=br[:, :], in1=cb[:, :], op=ALU.mult), reads=[brn, cbn], writes=["tmb0"])
                    p.op("dve", lambda e, sb_=sb_, bi=bi: e.tensor_tensor(out=tmb[1][:, :], in0=bi[:, :], in1=sb_[:, :], op=ALU.mult), reads=[bin_, sbn], writes=["tmb1"])
                    p.op("dve", lambda e, cb=cb, bi=bi: e.tensor_tensor(out=tmb[2][:, :], in0=bi[:, :], in1=cb[:, :], op=ALU.mult), reads=[bin_, cbn], writes=["tmb2"])
                    p.op("dve", lambda e, sb_=sb_, br=br: e.tensor_tensor(out=tmb[3][:, :], in0=br[:, :], in1=sb_[:, :], op=ALU.mult), reads=[brn, sbn], writes=["tmb3"])
                    p.op("dve", lambda e: e.tensor_tensor(out=xreb[:, :], in0=tmb[0][:, :], in1=tmb[1][:, :], op=ALU.add), reads=["tmb0", "tmb1"], writes=["xreb"])
                    p.op("dve", lambda e: e.tensor_tensor(out=ximb[:, :], in0=tmb[2][:, :], in1=tmb[3][:, :], op=ALU.subtract), reads=["tmb2", "tmb3"], writes=["ximb"])
                    if q + 1 < 32:
                        tab_C(q + 1)
                        tab_D(q + 1)
                    p.op("dve", lambda e, q=q: e.tensor_tensor_scan(out=greb[:, :], data0=sv[:, RR, q:q + 1].to_broadcast([128, T]), data1=xreb[:, :], initial=0.0, op0=ALU.mult, op1=ALU.add),
                         reads=["sv", "xreb"], writes=["greb"])
                    p.op("dve", lambda e, q=q: e.tensor_tensor_scan(out=gimb[:, :], data0=sv[:, RR, q:q + 1].to_broadcast([128, T]), data1=ximb[:, :], initial=0.0, op0=ALU.mult, op1=ALU.add),
                         reads=["sv", "ximb"], writes=["gimb"])
                    p.op("dve", lambda e, cb=cb: e.tensor_tensor(out=PP[0][:, :], in0=greb[:, :], in1=cb[:, :], op=ALU.mult), reads=["greb", cbn], writes=["PP0"])
                    p.op("dve", lambda e, sb_=sb_: e.tensor_tensor(out=PP[1][:, :], in0=gimb[:, :], in1=sb_[:, :], op=ALU.mult), reads=["gimb", sbn], writes=["PP1"])
                    p.op("dve", lambda e, sb_=sb_: e.tensor_tensor(out=PP[2][:, :], in0=greb[:, :], in1=sb_[:, :], op=ALU.mult), reads=["greb", sbn], writes=["PP2"])
                    p.op("dve", lambda e, cb=cb: e.tensor_tensor(out=PP[3][:, :], in0=gimb[:, :], in1=cb[:, :], op=ALU.mult), reads=["gimb", cbn], writes=["PP3"])
                    for k, (Cw, cwn) in enumerate(((CR, "CR"), (NCR, "NCR"), (NCI, "NCI"), (NCI, "NCI"))):
                        for n in range(4):
                            last = (pq == 3 and k == 3)
                            p.op("pe", lambda e, Cw=Cw, q=q, k=k, n=n, last=last: e.matmul(
                                psY[:, n * 512:(n + 1) * 512], lhsT=Cw[:, q, :], rhs=PP[k][:, n * 512:(n + 1) * 512],
                                start=False, stop=last), reads=[cwn, f"PP{k}"], appends=[psYn[n]])
                    if pq == 3:
                        yg = ygs[ct % 2]
                        ygn = f"ygs{ct % 2}"
                        p.op("act", lambda e, yg=yg: e.activation(out=yg[:, :], in_=psY[:, :], func=AF.Gelu), reads=psYn, writes=[ygn])
                        p.op("sp", lambda e, yg=yg, ct=ct: e.dma_start(out=yT_s[ct * 128:(ct + 1) * 128, :], in_=yg[:, :]),
                             reads=[ygn], writes=["s_yT"] if ct == 0 else (), appends=["s_yT"] if ct > 0 else (), dma=True)
        p.barrier()
        if stop_after <= 3:
            p.wait_all("sp", [])
            p.emit()
            return nc

        P34 = Phase(nc)
        P34.__enter__()
        wo = P34.sb("wo", [128, 16, D], BF16)
        for kc in range(16):
            p.op("pool", lambda e, kc=kc: e.dma_start(out=wo[:, kc, :], in_=w_out_d[kc * 128:(kc + 1) * 128, :]), appends=["wo"], dma=True)
        with Phase(nc) as P3b:
            yTs = P3b.sb("yTs", [128, 8, T], BF16)
            wgl = P3b.sb("wgl", [128, 8, 1024], BF16)
            colp2 = P3b.sb("colp2", [128, 16], F32)
            sg = [P3b.sb(f"sg{i}", [128, T], F32) for i in range(2)]
            yso = [P3b.sb(f"yso{i}", [128, T], BF16) for i in range(2)]
            psZ = [P3b.ps(f"psZ{i}", [128, T], F32) for i in range(2)]
            p.op("sp", lambda e: e.dma_start(out=colp2[:], in_=colp_d), writes=["colp2"], dma=True)
            p.op("sp", lambda e: e.dma_start(out=yTs[:, :, :], in_=yT_s.rearrange("(c p) t -> p c t", p=128)), writes=["yTs"], dma=True)
            p.op("pool", lambda e: e.dma_start(out=wgl[:, :, :], in_=w_glu_d.rearrange("(c p) n -> p c n", p=128)), writes=["wgl"], dma=True)
            for mo in range(8):
                pz = psZ[mo % 2]
                pzn = [f"psZ{mo % 2}_{n}" for n in range(4)]
                for kc in range(8):
                    for n in range(4):
                        p.op("pe", lambda e, pz=pz, mo=mo, kc=kc, n=n: e.matmul(
                            pz[:, n * 512:(n + 1) * 512], lhsT=wgl[:, kc, mo * 128:(mo + 1) * 128], rhs=yTs[:, kc, n * 512:(n + 1) * 512],
                            start=(kc == 0), stop=(kc == 7)), reads=["wgl", "yTs"],
                            writes=[pzn[n]] if kc == 0 else (), appends=[pzn[n]] if kc > 0 else ())
                s_ = sg[mo % 2]
                sn = f"sg{mo % 2}"
                yo = yso[mo % 2]
                yon = f"yso{mo % 2}"
                p.op("act", lambda e, pz=pz, s_=s_, mo=mo: e.activation(out=s_[:, :], in_=pz[:, :], func=AF.Sigmoid, bias=colp2[:, 8 + mo:9 + mo], scale=1.0),
                     reads=pzn + ["colp2"], writes=[sn])
                p.op("dve", lambda e, s_=s_, yo=yo, mo=mo: e.tensor_tensor(out=yo[:, :], in0=yTs[:, mo, :], in1=s_[:, :], op=ALU.mult),
                     reads=[sn, "yTs"], writes=[yon])
                p.op("sp", lambda e, yo=yo, mo=mo: e.dma_start(out=ycatT_s[mo * 128:(mo + 1) * 128, :], in_=yo[:, :]),
                     reads=[yon], writes=["s_yssm"] if mo == 0 else (), appends=["s_yssm"] if mo > 0 else (), dma=True)
        p.barrier()
        if stop_after <= 4:
            p.wait_all("sp", [])
            p.emit()
            return nc

        def layer_norm(pre_ap, prn, gam, bet, gnames, stt, sttn, lnj):
            p.op("dve", lambda e: e.memset(stt[:, 0:2], 0.0), writes=[sttn])
            p.op("act", lambda e: e.activation(out=lnj[:, :], in_=pre_ap, func=AF.Identity, accum_out=stt[:, 0:1]),
                 reads=[prn, sttn], writes=["lnj", sttn])
            p.op("act", lambda e: e.activation(out=lnj[:, :], in_=pre_ap, func=AF.Square, accum_out=stt[:, 1:2]),
                 reads=[prn, sttn], writes=["lnj", sttn])
            p.op("dve", lambda e: e.tensor_scalar(out=stt[:, 2:4], in0=stt[:, 0:2], scalar1=1.0 / D, scalar2=None, op0=ALU.mult), reads=[sttn], writes=[sttn])
            p.op("dve", lambda e: e.tensor_tensor(out=stt[:, 4:5], in0=stt[:, 2:3], in1=stt[:, 2:3], op=ALU.mult), reads=[sttn], writes=[sttn])
            p.op("dve", lambda e: e.tensor_tensor(out=stt[:, 5:6], in0=stt[:, 3:4], in1=stt[:, 4:5], op=ALU.subtract), reads=[sttn], writes=[sttn])
            p.op("dve", lambda e: e.tensor_scalar(out=stt[:, 5:6], in0=stt[:, 5:6], scalar1=EPS, scalar2=None, op0=ALU.add), reads=[sttn], writes=[sttn])
            p.op("act", lambda e: e.activation(out=stt[:, 7:8], in_=stt[:, 5:6], func=AF.Sqrt), reads=[sttn], writes=[sttn])
            p.op("dve", lambda e: e.reciprocal(out=stt[:, 5:6], in_=stt[:, 7:8]), reads=[sttn], writes=[sttn])
            p.op("dve", lambda e: e.scalar_tensor_tensor(out=stt[:, 6:7], in0=stt[:, 2:3], scalar=-1.0, in1=stt[:, 5:6], op0=ALU.mult, op1=ALU.mult), reads=[sttn], writes=[sttn])
            p.op("dve", lambda e: e.tensor_scalar(out=pre_ap, in0=pre_ap, scalar1=stt[:, 5:6], scalar2=stt[:, 6:7], op0=ALU.mult, op1=ALU.add),
                 reads=[prn, sttn], writes=[prn])
            p.op("dve", lambda e: e.tensor_tensor(out=pre_ap, in0=pre_ap, in1=gam[:, :], op=ALU.mult), reads=[prn] + gnames, writes=[prn])
            p.op("dve", lambda e: e.tensor_tensor(out=pre_ap, in0=pre_ap, in1=bet[:, :], op=ALU.add), reads=[prn] + gnames, writes=[prn])

        with Phase(nc) as P4:
            yct = [P4.sb(f"yct{i}", [128, 16, 128], BF16) for i in range(2)]
            xt = [P4.sb(f"xt{i}", [128, D], F32) for i in range(2)]
            lnj = P4.sb("lnj", [128, D], BF16)
            gam = P4.sb("gam", [128, D], F32)
            bet = P4.sb("bet", [128, D], F32)
            stt = P4.sb("stt", [128, 8], F32)
            h1b = P4.sb("h1b", [128, D], BF16)
            hTs = [P4.sb(f"hTs{i}", [128, 16, 128], BF16) for i in range(2)]
            psM = P4.ps("psM", [128, D], F32)
            psTr = P4.ps("psTr", [128, D], BF16)
            psMn = [f"psM{n}" for n in range(4)]
            p.op("sp", lambda e: e.dma_start(out=gam[:], in_=ln_d[0]), writes=["gam"], dma=True)
            p.op("sp", lambda e: e.dma_start(out=bet[:], in_=ln_d[1]), writes=["bet"], dma=True)
            ycat_v = ycatT_s.rearrange("(c p) t -> p c t", p=128)
            h1T_v = h1T_s.rearrange("(c p) t -> p c t", p=128)

            def p4_load(tt):
                p.op("sp", lambda e, tt=tt: e.dma_start(out=yct[tt % 2][:, :, :], in_=ycat_v[:, :, tt * 128:(tt + 1) * 128]),
                     reads=["s_yssm", "s_yat0", "s_yat1"], writes=[f"yct{tt % 2}"], dma=True)
                p.op("sp", lambda e, tt=tt: e.dma_start(out=xt[tt % 2][:, :], in_=x_d[tt * 128:(tt + 1) * 128, :]),
                     writes=[f"xt{tt % 2}"], dma=True)
            def p4_mm(tt):
                yc, ycn = yct[tt % 2], f"yct{tt % 2}"
                for kc in range(16):
                    for n in range(4):
                        p.op("pe", lambda e, yc=yc, kc=kc, n=n: e.matmul(psM[:, n * 512:(n + 1) * 512], lhsT=yc[:, kc, :],
                                                                      rhs=wo[:, kc, n * 512:(n + 1) * 512], start=(kc == 0), stop=(kc == 15)),
                             reads=[ycn, "wo"], writes=[psMn[n]] if kc == 0 else (), appends=[psMn[n]] if kc > 0 else ())
            p4_load(0)
            p4_load(1)
            p4_mm(0)
            for tt in range(16):
                x_, xn = xt[tt % 2], f"xt{tt % 2}"
                p.op("dve", lambda e, x_=x_: e.scalar_tensor_tensor(out=x_[:, :], in0=x_[:, :], scalar=ALPHA, in1=psM[:, :], op0=ALU.mult, op1=ALU.add),
                     reads=psMn + [xn], writes=[xn])
                if tt + 1 < 16:
                    p4_mm(tt + 1)
                if debug:
                    p.op("sp", lambda e, x_=x_, tt=tt: e.dma_start(out=out_d[tt * 128:(tt + 1) * 128, :], in_=x_[:, :]),
                         reads=[xn], writes=["out_dbg"], dma=True)
                layer_norm(x_[:, :], xn, gam, bet, ["gam", "bet"], stt, "stt", lnj)
                p.op("sp", lambda e, x_=x_, tt=tt: e.dma_start(out=h1_s[tt * 128:(tt + 1) * 128, :], in_=x_[:, :]),
                     reads=[xn], writes=["s_h1"] if tt == 0 else (), appends=["s_h1"] if tt > 0 else (), dma=True)
                p.op("act", lambda e, x_=x_: e.activation(out=h1b[:, :], in_=x_[:, :], func=AF.Identity), reads=[xn], writes=["h1b"])
                for dc in range(16):
                    p.op("pe", lambda e, dc=dc: e.transpose(psTr[:, dc * 128:(dc + 1) * 128], h1b[:, dc * 128:(dc + 1) * 128], ident_bf[:]),
                         reads=["h1b", "ident_bf"], writes=["psTr"] if dc == 0 else (), appends=["psTr"] if dc > 0 else ())
                hs, hsn = hTs[tt % 2], f"hTs{tt % 2}"
                p.op("dve", lambda e, hs=hs: e.tensor_copy(out=hs[:, :, :].rearrange("p c t -> p (c t)"), in_=psTr[:, :]), reads=["psTr"], writes=[hsn])
                p.op("sp", lambda e, hs=hs, tt=tt: e.dma_start(out=h1T_v[:, :, tt * 128:(tt + 1) * 128], in_=hs[:, :, :]),
                     reads=[hsn], writes=["s_h1T"] if tt == 0 else (), appends=["s_h1T"] if tt > 0 else (), dma=True)
                if tt + 2 < 16:
                    p4_load(tt + 2)
        p.barrier()
        P34.__exit__(None, None, None)
        if stop_after <= 5:
            p.wait_all("sp", [])
            p.emit()
            return nc

        with Phase(nc) as P5:
            hT = [P5.sb(f"hT{i}", [128, 16, 512], BF16) for i in range(2)]
            gT = P5.sb("gT", [128, NFC, 512], BF16)
            wu = [P5.sb(f"wu{i}", [128, 16, 256], BF16) for i in range(3)]
            wg = [P5.sb(f"wg{i}", [128, 16, 256], BF16) for i in range(3)]
            wd = [P5.sb(f"wd{i}", [128, 4, 512], BF16) for i in range(4)]
            pre2 = P5.sb("pre2", [128, 4, D], F32)
            lnj5 = P5.sb("lnj5", [128, D], BF16)
            gam5 = P5.sb("gam5", [128, D], F32)
            bet5 = P5.sb("bet5", [128, D], F32)
            stt5 = P5.sb("stt5", [128, 8], F32)
            cvp = P5.sb("cvp", [128, NFC, 4], F32)
            hprev = P5.sb("hprev", [128, NFC, 2], F32)
            hup = [P5.sb(f"hup{i}", [128, 514], F32) for i in range(2)]
            cv = [P5.sb(f"cv{i}", [128, 512], F32) for i in range(2)]
            ge = [P5.sb(f"ge{i}", [128, 512], F32) for i in range(2)]
            psU = [P5.ps(f"psU{i}", [128, 512], F32) for i in range(2)]
            psG = [P5.ps(f"psG{i}", [128, 512], F32) for i in range(2)]
            psA = [P5.ps(f"psAcc{i}", [128, 512], F32) for i in range(4)]
            p.op("sp", lambda e: e.dma_start(out=gam5[:], in_=ln_d[2]), writes=["gam5"], dma=True)
            p.op("sp", lambda e: e.dma_start(out=bet5[:], in_=ln_d[3]), writes=["bet5"], dma=True)
            p.op("sp", lambda e: e.dma_start(out=cvp[:].rearrange("p a b -> p (a b)"), in_=convp_d), writes=["cvp"], dma=True)
            p.op("dve", lambda e: e.memset(hprev[:], 0.0), writes=["hprev"])
            h1T_v5 = h1T_s.rearrange("(c p) t -> p c t", p=128)
            wup_v = w_up_d.rearrange("(c p) f -> p c f", p=128)
            wgt_v = w_gate_d.rearrange("(c p) f -> p c f", p=128)
            wdn_v = w_down_d.rearrange("(k p) d -> p k d", p=128)

            def lnj_fix():
                return None
            wl = 0
            dl = 0
            ev = 0
            for ti in range(4):
                tk0 = ti * 512
                hT_, hTn = hT[ti % 2], f"hT{ti % 2}"
                if ti == 0:
                    p.op("sp", lambda e, hT_=hT_, tk0=tk0: e.dma_start(out=hT_[:, :, :], in_=h1T_v5[:, :, tk0:tk0 + 512]),
                         reads=["s_h1T"], writes=[hTn], dma=True)
                p.op("sp", lambda e, tk0=tk0: e.dma_start(out=pre2[:, :, :], in_=h1_s[tk0:tk0 + 512, :].rearrange("(s p) d -> p s d", p=128)),
                     reads=["s_h1"], writes=["pre2"], dma=True)
                for f2 in range(NFC // 2):
                    wu_, wun = wu[wl % 3], f"wu{wl % 3}"
                    wg_, wgn = wg[wl % 3], f"wg{wl % 3}"
                    wl += 1
                    p.op("pool", lambda e, wu_=wu_, f2=f2: e.dma_start(out=wu_[:, :, :], in_=wup_v[:, :, f2 * 256:(f2 + 1) * 256]), writes=[wun], dma=True)
                    p.op("pool", lambda e, wg_=wg_, f2=f2: e.dma_start(out=wg_[:, :, :], in_=wgt_v[:, :, f2 * 256:(f2 + 1) * 256]), writes=[wgn], dma=True)
                    for sub in range(2):
                        fc = f2 * 2 + sub
                        pU, pUn = psU[ev % 2], f"psU{ev % 2}"
                        pG, pGn = psG[ev % 2], f"psG{ev % 2}"
                        hu, hun = hup[ev % 2], f"hup{ev % 2}"
                        cv_, cvn = cv[ev % 2], f"cv{ev % 2}"
                        ge_, gen = ge[ev % 2], f"ge{ev % 2}"
                        ev += 1
                        for kc in range(16):
                            p.op("pe", lambda e, pU=pU, wu_=wu_, kc=kc, sub=sub, hT_=hT_: e.matmul(
                                pU[:, :], lhsT=wu_[:, kc, sub * 128:(sub + 1) * 128], rhs=hT_[:, kc, :], start=(kc == 0), stop=(kc == 15)),
                                reads=[wun, hTn], writes=[pUn] if kc == 0 else (), appends=[pUn] if kc > 0 else ())
                        for kc in range(16):
                            p.op("pe", lambda e, pG=pG, wg_=wg_, kc=kc, sub=sub, hT_=hT_: e.matmul(
                                pG[:, :], lhsT=wg_[:, kc, sub * 128:(sub + 1) * 128], rhs=hT_[:, kc, :], start=(kc == 0), stop=(kc == 15)),
                                reads=[wgn, hTn], writes=[pGn] if kc == 0 else (), appends=[pGn] if kc > 0 else ())
                        p.op("act", lambda e, hu=hu, pU=pU: e.activation(out=hu[:, 2:514], in_=pU[:, :], func=AF.Identity), reads=[pUn], writes=[hun])
                        p.op("act", lambda e, hu=hu, fc=fc: e.activation(out=hu[:, 0:2], in_=hprev[:, fc, :], func=AF.Identity), reads=["hprev"], appends=[hun])
                        p.op("act", lambda e, hu=hu, fc=fc: e.activation(out=hprev[:, fc, :], in_=hu[:, 512:514], func=AF.Identity), reads=[hun], writes=["hprev"])
                        p.op("dve", lambda e, hu=hu, cv_=cv_, fc=fc: e.tensor_scalar(out=cv_[:, :], in0=hu[:, 2:514], scalar1=cvp[:, fc, 2:3], scalar2=cvp[:, fc, 3:4], op0=ALU.mult, op1=ALU.add),
                             reads=[hun, "cvp"], writes=[cvn])
                        p.op("dve", lambda e, hu=hu, cv_=cv_, fc=fc: e.scalar_tensor_tensor(out=cv_[:, :], in0=hu[:, 1:513], scalar=cvp[:, fc, 1:2], in1=cv_[:, :], op0=ALU.mult, op1=ALU.add),
                             reads=[hun, "cvp", cvn], writes=[cvn])
                        p.op("dve", lambda e, hu=hu, cv_=cv_, fc=fc: e.scalar_tensor_tensor(out=cv_[:, :], in0=hu[:, 0:512], scalar=cvp[:, fc, 0:1], in1=cv_[:, :], op0=ALU.mult, op1=ALU.add),
                             reads=[hun, "cvp", cvn], writes=[cvn])
                        p.op("act", lambda e, cv_=cv_, ge_=ge_: e.activation(out=ge_[:, :], in_=cv_[:, :], func=AF.Gelu), reads=[cvn], writes=[gen])
                        p.op("dve", lambda e, ge_=ge_, pG=pG, fc=fc: e.tensor_tensor(out=gT[:, fc, :], in0=ge_[:, :], in1=pG[:, :], op=ALU.mult),
                             reads=[gen, pGn], writes=[f"gT{fc}"])
                if ti + 1 < 4:
                    p.op("sp", lambda e, ti=ti: e.dma_start(out=hT[(ti + 1) % 2][:, :, :], in_=h1T_v5[:, :, (ti + 1) * 512:(ti + 2) * 512]),
                         reads=["s_h1T"], writes=[f"hT{(ti + 1) % 2}"], dma=True)
                for dg in range(4):
                    for k4 in range(NFC // 4):
                        wd_, wdn = wd[dl % 4], f"wd{dl % 4}"
                        dl += 1
                        p.op("pool", lambda e, wd_=wd_, k4=k4, dg=dg: e.dma_start(out=wd_[:, :, :], in_=wdn_v[:, k4 * 4:(k4 + 1) * 4, dg * 512:(dg + 1) * 512]),
                             writes=[wdn], dma=True)
                        for kk in range(4):
                            k = k4 * 4 + kk
                            for st_ in range(4):
                                p.op("pe", lambda e, wd_=wd_, kk=kk, k=k, st_=st_: e.matmul(
                                    psA[st_][:, :], lhsT=gT[:, k, st_ * 128:(st_ + 1) * 128], rhs=wd_[:, kk, :], start=(k == 0), stop=(k == NFC - 1)),
                                    reads=[wdn, f"gT{k}"], writes=[f"psAcc{st_}"] if k == 0 else (), appends=[f"psAcc{st_}"] if k > 0 else ())
                    for st_ in range(4):
                        p.op("dve", lambda e, st_=st_, dg=dg: e.scalar_tensor_tensor(
                            out=pre2[:, st_, dg * 512:(dg + 1) * 512], in0=pre2[:, st_, dg * 512:(dg + 1) * 512], scalar=ALPHA, in1=psA[st_][:, :], op0=ALU.mult, op1=ALU.add),
                            reads=[f"psAcc{st_}", "pre2"], writes=["pre2"])
                if debug and ti == 0:
                    for fc in range(8):
                        p.op("sp", lambda e, fc=fc: e.dma_start(out=uT_s[fc * 128:(fc + 1) * 128, 0:512], in_=gT[:, fc, :]),
                             reads=[f"gT{fc}"], writes=[f"dbg_g{fc}"], dma=True)
                for st_ in range(4):
                    if not debug:
                        layer_norm(pre2[:, st_, :], "pre2", gam5, bet5, ["gam5", "bet5"], stt5, "stt5", lnj5)
                    p.op("sp", lambda e, st_=st_, tk0=tk0: e.dma_start(out=out_d[tk0 + st_ * 128:tk0 + (st_ + 1) * 128, :], in_=pre2[:, st_, :]),
                         reads=["pre2"], writes=["out"] if (ti == 0 and st_ == 0) else (), appends=["out"] if not (ti == 0 and st_ == 0) else (), dma=True)
            p.wait_all("sp", ["out"])
        p.emit()
    return nc


def _consts():
    ident = np.eye(128, dtype=np.float32)
    pp = np.arange(128)[:, None]
    jj = np.arange(128)[None, :]
    causal = np.where(jj <= pp, 0.0, -1.0e30).astype(np.float32)
    slopes = np.exp2(-8.0 * np.arange(1, 9, dtype=np.float64) / 8.0)
    dd = np.arange(16, dtype=np.float64)
    alibi = (slopes[None, :, None] * (np.arange(128, dtype=np.float64)[:, None, None] - 128.0 * dd[None, None, :])).astype(np.float32)
    iota = np.ascontiguousarray(np.broadcast_to(np.arange(T, dtype=np.float32)[None, :], (128, T)))
    return ident, causal, np.ascontiguousarray(alibi.reshape(128, 128)), iota


def _shared_inputs(inp):
    f32 = np.float32
    G, P, HG = 64, 64, 16
    g = np.arange(G)

    def state_major(a):
        return np.ascontiguousarray(a.reshape(32, 2, P).transpose(1, 2, 0).reshape(128, 32))
    a_re = state_major(inp["ssm_a_re"][0])
    a_im = state_major(inp["ssm_a_im"][0])
    ldt = state_major(np.repeat(inp["ssm_log_dt"][0][:, None], P, axis=1))
    ssm_prm = np.ascontiguousarray(np.concatenate([a_re, a_im, ldt], axis=1).astype(f32))

    def bpad(b):
        o = np.zeros((128, 32, 128), f32)
        for gi in range(G):
            q, g2 = gi // 2, gi % 2
            r0 = (q % 4) * 32 + g2 * 16
            o[r0:r0 + 16, q, g2 * 64:(g2 + 1) * 64] = b[gi].T
        return o.reshape(128, 32 * 128)

    def cpad(c):
        o = np.zeros((128, 32, 128), f32)
        for gi in range(G):
            q, g2 = gi // 2, gi % 2
            c0 = (q % 4) * 32 + g2 * 16
            o[g2 * 64:(g2 + 1) * 64, q, c0:c0 + 16] = c[gi].T
        return o.reshape(128, 32 * 128)

    colp = np.concatenate([inp["ssm_d"][0].reshape(8, 128).T, inp["b_glu"][0].reshape(8, 128).T], axis=1).astype(f32)
    cw = inp["conv_w"][0]
    cb = inp["conv_b"][0]
    convp = np.stack([cw[0], cw[1], cw[2], cb], axis=1).reshape(NFC, 128, 4).transpose(1, 0, 2).reshape(128, NFC * 4)
    ln = np.stack([np.broadcast_to(inp[k][0][None, :], (128, D)) for k in ("ln1_g", "ln1_b", "ln2_g", "ln2_b")], axis=0)
    ident, causal, alibi, iota = _consts()
    return {
        "w_in": np.ascontiguousarray(inp["w_in"][0]),
        "w_glu": np.ascontiguousarray(inp["w_glu"][0]),
        "w_out": np.ascontiguousarray(inp["w_out"][0]),
        "w_up": np.ascontiguousarray(inp["w_up"][0]),
        "w_gate": np.ascontiguousarray(inp["w_gate"][0]),
        "w_down": np.ascontiguousarray(inp["w_down"][0]),
        "ssm_prm": ssm_prm,
        "bpad_re": bpad(inp["ssm_b_re"][0]), "bpad_im": bpad(inp["ssm_b_im"][0]),
        "cpad_re": cpad(inp["ssm_c_re"][0]), "cpad_im": cpad(inp["ssm_c_im"][0]),
        "colp": np.ascontiguousarray(colp),
        "convp": np.ascontiguousarray(convp.astype(f32)),
        "ln": np.ascontiguousarray(ln.astype(f32)),
        "ident": ident, "causal": causal, "alibi": alibi, "iota": iota,
    }


def kernel(**inputs):
    inp = {k: np.asarray(v, dtype=np.float32) for k, v in inputs.items()}
    x = inp["x"]
    nb = x.shape[0]
    shared = _shared_inputs(inp)
    in_maps = []
    for b in range(nb):
        m = dict(shared)
        m["x"] = np.ascontiguousarray(x[b])
        m["xT"] = np.ascontiguousarray(x[b].T)
        in_maps.append(m)
    nc = build_program()
    res = run_bass_kernel_spmd(nc, in_maps, core_ids=list(range(nb)))
    out = np.stack([np.asarray(r["out"], dtype=np.float32) for r in res.results], axis=0)
    return out
```
